# Optimizing a Trainium2 kernel written in Bass

```python
import math
import jax, jax.numpy as jnp
from jax import lax
import numpy as np


D_MODEL = 2048
BATCH = 4
SEQ = 8192
DEPTH = 1

GRID_W = 64
CTX_LEN = 256
N_HEADS_A = 8
HEAD_DIM_A = 64
V_DIM_A = 2 * HEAD_DIM_A
QK_WIDTH_A = N_HEADS_A * 2 * HEAD_DIM_A
WIDTH_A = N_HEADS_A * V_DIM_A
N_HEADS_B = 8
HEAD_DIM_B = 128
WIDTH_B = N_HEADS_B * HEAD_DIM_B
WIN_H = 8
WIN_W = 16
N_EXPERTS = 16
EXPERT_FF = 2048
CAPACITY_FACTOR = 2
Q_BLOCK = 128
ROPE_BASE = 10000.0
EPS = 1e-6
NEG_INF = -1e30

OFF_QA = 0
OFF_QB = OFF_QA + QK_WIDTH_A
OFF_GATE = OFF_QB + WIDTH_B
OFF_KA = OFF_GATE + 2 * D_MODEL
OFF_VA = OFF_KA + QK_WIDTH_A
OFF_KB = OFF_VA + WIDTH_A
OFF_VB = OFF_KB + WIDTH_B
PROJ_DIM = OFF_VB + WIDTH_B

kernel_name = 'hybrid_diffattn_natten_ecmoe_dit_block'


def rms_norm(x, g):
    xf = x.astype(jnp.float32)
    y = xf * lax.rsqrt(jnp.mean(xf * xf, axis=-1, keepdims=True) + EPS)
    return (y * g.astype(jnp.float32)).astype(x.dtype)


def modulate(x, shift, scale):
    return x * (1.0 + scale) + shift


def heads(t, n_heads):
    b, n, _ = t.shape
    return t.reshape(b, n, n_heads, -1).transpose(0, 2, 1, 3)


def merge_heads(t):
    b, h, n, dh = t.shape
    return t.transpose(0, 2, 1, 3).reshape(b, n, h * dh)


def axial_rope_tables(n_tokens):
    t = jnp.arange(n_tokens, dtype=jnp.int32)
    row = (t // GRID_W).astype(jnp.float32)
    col = (t % GRID_W).astype(jnp.float32)
    half = HEAD_DIM_A // 2
    inv_freq = ROPE_BASE ** (-jnp.arange(0, half, 2, dtype=jnp.float32) / half)

    def tab(pos):
        ang = pos[:, None] * inv_freq[None, :]
        ang = jnp.concatenate([ang, ang], axis=-1)
        return jnp.cos(ang), jnp.sin(ang)

    cr, sr = tab(row)
    cc, sc = tab(col)
    return jnp.concatenate([cr, cc], axis=-1), jnp.concatenate([sr, sc], axis=-1)


def rotate_half(x):
    x1, x2 = jnp.split(x, 2, axis=-1)
    return jnp.concatenate([-x2, x1], axis=-1)


def apply_axial_rope(x, cos, sin):
    xr, xc = jnp.split(x, 2, axis=-1)
    rot = jnp.concatenate([rotate_half(xr), rotate_half(xc)], axis=-1)
    return (x * cos + rot * sin).astype(x.dtype)


def split_kv(t):
    k_a = heads(t[..., 0:OFF_VA - OFF_KA], N_HEADS_A)
    v_a = heads(t[..., OFF_VA - OFF_KA:OFF_KB - OFF_KA], N_HEADS_A)
    k_b = heads(t[..., OFF_KB - OFF_KA:OFF_VB - OFF_KA], N_HEADS_B)
    v_b = heads(t[..., OFF_VB - OFF_KA:PROJ_DIM - OFF_KA], N_HEADS_B)
    return k_a, v_a, k_b, v_b


def diff_pair(t, gain):
    return rms_norm(t[..., :HEAD_DIM_A], gain), rms_norm(t[..., HEAD_DIM_A:], gain)


def diff_attend(q1, q2, k1, k2, v, lam):
    scale = HEAD_DIM_A ** -0.5
    p1 = jax.nn.softmax(jnp.einsum('bhqd,bhkd->bhqk', q1, k1).astype(jnp.float32) * scale, axis=-1)
    p2 = jax.nn.softmax(jnp.einsum('bhqd,bhkd->bhqk', q2, k2).astype(jnp.float32) * scale, axis=-1)
    att = (p1 - lam * p2).astype(v.dtype)
    return jnp.einsum('bhqk,bhkd->bhqd', att, v)


def diff_attention_blocks(q1, q2, k1_all, k2_all, v_all, lam):
    b, h, s, d = q1.shape
    nb = s // Q_BLOCK

    def blk(a):
        return a.reshape(b, h, nb, Q_BLOCK, d).transpose(2, 0, 1, 3, 4)

    out = lax.map(lambda qs: diff_attend(qs[0], qs[1], k1_all, k2_all, v_all, lam), (blk(q1), blk(q2)))
    return out.transpose(1, 2, 0, 3, 4).reshape(b, h, s, -1)


def dense_attend(q, k, v):
    scale = q.shape[-1] ** -0.5
    p = jax.nn.softmax(jnp.einsum('bhqd,bhkd->bhqk', q, k).astype(jnp.float32) * scale, axis=-1)
    return jnp.einsum('bhqk,bhkd->bhqd', p.astype(v.dtype), v)


def neighbourhood_attention(q, k, v, k_ctx, v_ctx, rpb, rows):
    b, h, s, dh = q.shape
    n_ctx = k_ctx.shape[2]
    kh = min(WIN_H, rows)
    k_grid = k.reshape(b, h, rows, GRID_W, dh)
    v_grid = v.reshape(b, h, rows, GRID_W, dh)
    q_rows = q.reshape(b, h, rows, GRID_W, dh).transpose(2, 0, 1, 3, 4)
    col = jnp.arange(GRID_W, dtype=jnp.int32)
    col_start = jnp.clip(col - WIN_W // 2, 0, GRID_W - WIN_W)
    in_win = (col[None, :] >= col_start[:, None]) & (col[None, :] < col_start[:, None] + WIN_W)
    col_mask = jnp.tile(in_win, (1, kh))
    idx_c = jnp.clip(col[None, :] - col[:, None] + WIN_W - 1, 0, 2 * WIN_W - 2)
    scale = dh ** -0.5

    def row_step(args):
        r, q_r = args
        r_start = jnp.clip(r - kh // 2, 0, rows - kh)
        k_blk = lax.dynamic_slice_in_dim(k_grid, r_start, kh, axis=2).reshape(b, h, kh * GRID_W, dh)
        v_blk = lax.dynamic_slice_in_dim(v_grid, r_start, kh, axis=2).reshape(b, h, kh * GRID_W, dh)
        idx_r = r_start + jnp.arange(kh, dtype=jnp.int32) - r + WIN_H - 1
        bias = rpb[:, idx_r[:, None, None], idx_c[None, :, :]]
        bias = bias.transpose(0, 2, 1, 3).reshape(h, GRID_W, kh * GRID_W).astype(jnp.float32)
        s_lat = jnp.einsum('bhqd,bhkd->bhqk', q_r, k_blk).astype(jnp.float32) * scale + bias[None]
        s_lat = jnp.where(col_mask, s_lat, NEG_INF)
        s_ctx = jnp.einsum('bhqd,bhkd->bhqk', q_r, k_ctx).astype(jnp.float32) * scale
        p = jax.nn.softmax(jnp.concatenate([s_ctx, s_lat], axis=-1), axis=-1).astype(v.dtype)
        return (jnp.einsum('bhqk,bhkd->bhqd', p[..., :n_ctx], v_ctx)
                + jnp.einsum('bhqk,bhkd->bhqd', p[..., n_ctx:], v_blk))

    out = lax.map(row_step, (jnp.arange(rows, dtype=jnp.int32), q_rows))
    return out.transpose(1, 2, 0, 3, 4).reshape(b, h, s, dh)


def gated_merge(gate_logits, y_a, y_b, w_a, w_b, w_o):
    g_a = jax.nn.sigmoid(gate_logits[..., :D_MODEL])
    g_b = jax.nn.sigmoid(gate_logits[..., D_MODEL:])
    return (g_a * (y_a @ w_a) + g_b * (y_b @ w_b)) @ w_o


def expert_choice_ffn(h, w_router, w_gate, w_up, w_down):
    b, n, d = h.shape
    cap = CAPACITY_FACTOR * n // N_EXPERTS
    aff = jax.nn.softmax((h @ w_router).astype(jnp.float32), axis=-1)
    g, idx = lax.top_k(aff.transpose(0, 2, 1), cap)
    xe = jax.vmap(lambda hb, ib: hb[ib])(h, idx)
    a = jnp.einsum('becd,edf->becf', xe, w_gate)
    u = jnp.einsum('becd,edf->becf', xe, w_up)
    y = jnp.einsum('becf,efd->becd', jax.nn.silu(a) * u, w_down)
    y = y * g[..., None].astype(y.dtype)

    def combine(yb, ib):
        return jnp.zeros((n, d), h.dtype).at[ib.reshape(-1)].add(yb.reshape(-1, d))

    return jax.vmap(combine)(y, idx)


def setup_inputs(seed: int = 0) -> dict:
    key = jax.random.key(seed)
    ks = jax.random.split(key, 32)
    d = D_MODEL

    def nrm(k, shape, scale):
        return jax.random.normal(k, shape, jnp.float32) * scale

    return {
        'x': nrm(ks[0], (BATCH, SEQ, d), 1.0),
        'c': nrm(ks[1], (BATCH, d), 1.0),
        'ctx': nrm(ks[2], (BATCH, CTX_LEN, d), 1.0),
        'c_ctx': nrm(ks[3], (d,), 1.0),
        'w_mod': nrm(ks[4], (DEPTH, d, 6 * d), 0.5 * d ** -0.5),
        'b_mod': nrm(ks[5], (DEPTH, 6 * d), 0.02),
        'g_norm1': 1.0 + nrm(ks[6], (DEPTH, d), 0.05),
        'g_norm2': 1.0 + nrm(ks[7], (DEPTH, d), 0.05),
        'w_in': nrm(ks[8], (DEPTH, d, PROJ_DIM), d ** -0.5),
        'q_gain_a': 1.0 + nrm(ks[9], (DEPTH, HEAD_DIM_A), 0.05),
        'k_gain_a': 1.0 + nrm(ks[10], (DEPTH, HEAD_DIM_A), 0.05),
        'lam_q1': nrm(ks[11], (DEPTH, HEAD_DIM_A), 0.1),
        'lam_k1': nrm(ks[12], (DEPTH, HEAD_DIM_A), 0.1),
        'lam_q2': nrm(ks[13], (DEPTH, HEAD_DIM_A), 0.1),
        'lam_k2': nrm(ks[14], (DEPTH, HEAD_DIM_A), 0.1),
        'subln_gain': 1.0 + nrm(ks[15], (DEPTH, V_DIM_A), 0.05),
        'q_gain_b': 1.0 + nrm(ks[16], (DEPTH, HEAD_DIM_B), 0.05),
        'k_gain_b': 1.0 + nrm(ks[17], (DEPTH, HEAD_DIM_B), 0.05),
        'rel_pos_bias': nrm(ks[18], (DEPTH, N_HEADS_B, 2 * WIN_H - 1, 2 * WIN_W - 1), 0.1),
        'w_branch_a': nrm(ks[19], (DEPTH, WIDTH_A, d), WIDTH_A ** -0.5),
        'w_branch_b': nrm(ks[20], (DEPTH, WIDTH_B, d), WIDTH_B ** -0.5),
        'w_out': nrm(ks[21], (DEPTH, d, d), d ** -0.5),
        'w_router': nrm(ks[22], (DEPTH, d, N_EXPERTS), d ** -0.5),
        'w_exp_gate': nrm(ks[23], (DEPTH, N_EXPERTS, d, EXPERT_FF), d ** -0.5),
        'w_exp_up': nrm(ks[24], (DEPTH, N_EXPERTS, d, EXPERT_FF), d ** -0.5),
        'w_exp_down': nrm(ks[25], (DEPTH, N_EXPERTS, EXPERT_FF, d), EXPERT_FF ** -0.5),
    }


def reference(x, c, ctx, c_ctx, w_mod, b_mod, g_norm1, g_norm2, w_in, q_gain_a, k_gain_a,
              lam_q1, lam_k1, lam_q2, lam_k2, subln_gain, q_gain_b, k_gain_b, rel_pos_bias,
              w_branch_a, w_branch_b, w_out, w_router, w_exp_gate, w_exp_up, w_exp_down):
    s = x.shape[1]
    rows = s // GRID_W
    cos, sin = axial_rope_tables(s)
    for l in range(DEPTH):
        update_ctx = l < DEPTH - 1
        lam_init = 0.8 - 0.6 * math.exp(-0.3 * l)
        lam = (jnp.exp(jnp.sum(lam_q1[l].astype(jnp.float32) * lam_k1[l].astype(jnp.float32)))
               - jnp.exp(jnp.sum(lam_q2[l].astype(jnp.float32) * lam_k2[l].astype(jnp.float32)))
               + lam_init)

        mod = jax.nn.silu(c) @ w_mod[l] + b_mod[l]
        mod_c = jax.nn.silu(c_ctx) @ w_mod[l] + b_mod[l]
        sh1, sc1, ga1, sh2, sc2, ga2 = jnp.split(mod[:, None, :], 6, axis=-1)
        csh1, csc1, cga1, csh2, csc2, cga2 = jnp.split(mod_c, 6, axis=-1)

        h = modulate(rms_norm(x, g_norm1[l]), sh1, sc1)
        hc = modulate(rms_norm(ctx, g_norm1[l]), csh1, csc1)
        proj = h @ w_in[l]
        if update_ctx:
            projc = hc @ w_in[l]
            projc_kv = projc[..., OFF_KA:]
        else:
            projc_kv = hc @ w_in[l][:, OFF_KA:]

        k_a_c, v_a_c, k_b_c, v_b_c = split_kv(projc_kv)
        k1c, k2c = diff_pair(k_a_c, k_gain_a[l])
        k_b_c = rms_norm(k_b_c, k_gain_b[l])

        k_a, v_a, k_b, v_b = split_kv(proj[..., OFF_KA:])
        q1, q2 = diff_pair(heads(proj[..., OFF_QA:OFF_QB], N_HEADS_A), q_gain_a[l])
        q1, q2 = apply_axial_rope(q1, cos, sin), apply_axial_rope(q2, cos, sin)
        k1, k2 = diff_pair(k_a, k_gain_a[l])
        k1, k2 = apply_axial_rope(k1, cos, sin), apply_axial_rope(k2, cos, sin)

        k1_all = jnp.concatenate([k1c, k1], axis=2)
        k2_all = jnp.concatenate([k2c, k2], axis=2)
        v_a_all = jnp.concatenate([v_a_c, v_a], axis=2)
        y_a = diff_attention_blocks(q1, q2, k1_all, k2_all, v_a_all, lam)
        y_a = merge_heads(rms_norm(y_a, subln_gain[l]) * (1.0 - lam_init))

        q_b = rms_norm(heads(proj[..., OFF_QB:OFF_GATE], N_HEADS_B), q_gain_b[l])
        k_b = rms_norm(k_b, k_gain_b[l])
        y_b = merge_heads(neighbourhood_attention(q_b, k_b, v_b, k_b_c, v_b_c, rel_pos_bias[l], rows))

        mix = gated_merge(proj[..., OFF_GATE:OFF_KA], y_a, y_b, w_branch_a[l], w_branch_b[l], w_out[l])
        x_new = x + ga1 * mix
        h2 = modulate(rms_norm(x_new, g_norm2[l]), sh2, sc2)
        x_new = x_new + ga2 * expert_choice_ffn(h2, w_router[l], w_exp_gate[l], w_exp_up[l], w_exp_down[l])

        if update_ctx:
            q1c, q2c = diff_pair(heads(projc[..., OFF_QA:OFF_QB], N_HEADS_A), q_gain_a[l])
            y_a_c = diff_attend(q1c, q2c, k1c, k2c, v_a_c, lam)
            y_a_c = merge_heads(rms_norm(y_a_c, subln_gain[l]) * (1.0 - lam_init))
            q_b_c = rms_norm(heads(projc[..., OFF_QB:OFF_GATE], N_HEADS_B), q_gain_b[l])
            y_b_c = merge_heads(dense_attend(q_b_c, k_b_c, v_b_c))
            mix_c = gated_merge(projc[..., OFF_GATE:OFF_KA], y_a_c, y_b_c, w_branch_a[l], w_branch_b[l], w_out[l])
            ctx_new = ctx + cga1 * mix_c
            h2c = modulate(rms_norm(ctx_new, g_norm2[l]), csh2, csc2)
            ctx = ctx_new + cga2 * expert_choice_ffn(h2c, w_router[l], w_exp_gate[l], w_exp_up[l], w_exp_down[l])
        x = x_new
    return x
```

```python
import math
from contextlib import ExitStack

import numpy as np
import concourse.bass as bass
import concourse.mybir as mybir
from concourse.bass_utils import run_bass_kernel_spmd

F32 = mybir.dt.float32
BF16 = mybir.dt.bfloat16
I32 = mybir.dt.int32
AF = mybir.ActivationFunctionType
ALU = mybir.AluOpType
AX = mybir.AxisListType

D = 2048
S = 8192
L = 256
NK = S + L
NT = S // 128
KT = D // 128
GRID_W = 64
PROJ = 10240
OFF_QA, OFF_QB, OFF_GATE, OFF_KA, OFF_VA, OFF_KB, OFF_VB = 0, 1024, 2048, 6144, 7168, 8192, 9216
NE = 16
CAP = 1024
EPS = 1e-6
LAM_INIT = 0.8 - 0.6 * math.exp(0.0)
N_CORES = 8


class Ev:
    __slots__ = ("sem", "name", "val")

    def __init__(self, sem, name, val):
        self.sem, self.name, self.val = sem, name, val


class Prog:
    def __init__(self, nc, es):
        self.nc = nc
        self.eng = {"pe": nc.tensor, "act": nc.scalar, "dve": nc.vector, "pool": nc.gpsimd, "sp": nc.sync}
        self.esem = {}
        self.ecnt = {}
        for e in ("pe", "act", "dve", "pool"):
            self.esem[e] = es.enter_context(nc.semaphore("s_" + e))
            self.ecnt[e] = 0
        self.rings = {}
        self.rpos = {}
        for e, n in (("sp", 12), ("pool", 12), ("act", 8)):
            self.rings[e] = [[es.enter_context(nc.semaphore("d_%s%d" % (e, i))), "d_%s%d" % (e, i), 0] for i in range(n)]
            self.rpos[e] = 0
        self.waited = {e: {} for e in self.eng}
        self.lastw = {}
        self.readers = {}
        self.nins = 0

    def wait(self, eng, ev):
        if ev is None:
            return
        if eng == "pe" and ev.name == "s_pe":
            return
        w = self.waited[eng]
        if w.get(ev.name, 0) >= ev.val:
            return
        self.eng[eng].wait_ge(ev.sem, ev.val)
        w[ev.name] = ev.val
        self.nins += 1

    def _hazards(self, eng, reads, writes):
        for k in reads:
            self.wait(eng, self.lastw.get(k))
        for k in writes:
            self.wait(eng, self.lastw.get(k))
            rd = self.readers.get(k)
            if rd:
                for ev in rd.values():
                    self.wait(eng, ev)

    def _record(self, ev, reads, writes):
        for k in reads:
            self.readers.setdefault(k, {})[ev.name] = ev
        for k in writes:
            self.lastw[k] = ev
            self.readers[k] = {}

    def op(self, eng, fn, reads=(), writes=()):
        self._hazards(eng, reads, writes)
        ins = fn(self.eng[eng])
        self.ecnt[eng] += 1
        ins.then_inc(self.esem[eng], 1)
        ev = Ev(self.esem[eng], "s_" + eng, self.ecnt[eng])
        self._record(ev, reads, writes)
        self.nins += 1
        return ev

    def dma(self, eng, fn, reads=(), writes=()):
        ring = self.rings[eng]
        i = self.rpos[eng]
        self.rpos[eng] = (i + 1) % len(ring)
        sem, name, cnt = ring[i]
        if cnt:
            self.wait(eng, Ev(sem, name, cnt))
        self._hazards(eng, reads, writes)
        ins = fn(self.eng[eng])
        ins.then_inc(sem, 16)
        ring[i][2] = cnt + 16
        ev = Ev(sem, name, cnt + 16)
        self._record(ev, reads, writes)
        self.nins += 1
        return ev

    def barrier(self, pool_ring=False):
        evs = [Ev(self.esem[e], "s_" + e, self.ecnt[e]) for e in self.esem if self.ecnt[e]]
        for e in self.rings:
            if e == "pool" and not pool_ring:
                continue
            for sem, name, cnt in self.rings[e]:
                if cnt:
                    evs.append(Ev(sem, name, cnt))
        for e in self.eng:
            for ev in evs:
                self.wait(e, ev)
        keep = {k: v for k, v in self.lastw.items() if "wbf" in repr(k)}
        self.lastw = keep
        self.readers = {}


def _consts():
    t = np.arange(S)
    row = (t // GRID_W).astype(np.float32)
    col = (t % GRID_W).astype(np.float32)
    half = 32
    inv_freq = (10000.0 ** (-np.arange(0, half, 2, dtype=np.float32) / half)).astype(np.float32)

    def tab(pos):
        ang = pos[:, None] * inv_freq[None, :]
        ang = np.concatenate([ang, ang], axis=-1)
        return np.cos(ang), np.sin(ang)

    cr, sr = tab(row)
    cc, sc = tab(col)
    cos = np.concatenate([cr, cc], -1).astype(np.float32)
    sin = np.concatenate([sr, sc], -1).astype(np.float32)
    ss = sin.reshape(S, 2, 2, 16).copy()
    ss[:, :, 0, :] *= -1.0
    ss = ss.reshape(S, 64)
    rope = np.stack([cos, ss], axis=1)
    rope = np.ascontiguousarray(rope.reshape(NT, 128, 2, 64).transpose(1, 0, 2, 3))
    q = np.arange(64)
    cs = np.clip(q - 8, 0, 48)
    wk = np.arange(64)
    inwin = (wk[None, :] >= cs[:, None]) & (wk[None, :] < cs[:, None] + 16)
    m = np.zeros((128, 4, 64), np.float32)
    for i in range(8):
        m[(i % 2) * 64:(i % 2) * 64 + 64, i // 2, :] = inwin.T.astype(np.float32)
    return rope.astype(np.float32), m


def _rpb_expand(rpb):
    H = rpb.shape[0]
    q = np.arange(64)
    wk = np.arange(64)
    idx_c = np.clip(wk[:, None] - q[None, :] + 15, 0, 30)
    out = np.zeros((H, 128, 8, 4, 64), np.float32)
    for v in range(8):
        for i in range(8):
            idx_r = 7 - v + i
            out[:, (i % 2) * 64:(i % 2) * 64 + 64, v, i // 2, :] = rpb[:, idx_r][:, idx_c]
    return out


def build(debug=False, upto=99, lim=None):
    lim = lim or {}
    nc = bass.Bass("TRN2", target_bir_lowering=False)

    def din(name, shape, dt=F32):
        return nc.dram_tensor(name, list(shape), dt, kind="ExternalInput").ap()

    def dscr(name, shape, dt, dbg=False):
        kind = "ExternalOutput" if (debug and dbg) else "Internal"
        return nc.dram_tensor(name, list(shape), dt, kind=kind).ap()

    x_d = din("x", [S, D])
    ctx_d = din("ctx", [L, D])
    cc_d = din("cc", [128, KT, 2])
    wmod_d = din("w_mod", [D, 6 * D])
    bmod_d = din("b_mod", [1, 6 * D])
    g12_d = din("g12", [2, D])
    win_d = din("w_in", [D, PROJ])
    smallp_d = din("smallp", [1, 768])
    rpbx_d = din("rpbx", [8, 128, 8, 4, 64])
    namask_d = din("namask", [128, 4, 64])
    rope_d = din("rope", [128, NT, 2, 64])
    wa_d = din("w_a", [1024, D])
    wb_d = din("w_b", [1024, D])
    wo_d = din("w_o", [D, D])
    wr_d = din("w_r", [D, NE])
    weg_d = din("w_eg", [NE, D, D])
    weu_d = din("w_eu", [NE, D, D])
    wed_d = din("w_ed", [NE, D, D])
    out_d = nc.dram_tensor("out", [S, D], F32, kind="ExternalOutput").ap()

    qaT_d = dscr("qaT", [8, 128, S], BF16, True)
    kaT_d = dscr("kaT", [8, 128, NK], BF16, True)
    va_d = dscr("va", [NK, 1024], BF16, True)
    qbT_d = dscr("qbT", [8, 128, S], BF16, True)
    kbT_d = dscr("kbT", [8, 128, NK], BF16, True)
    vb_d = dscr("vb", [NK, 1024], BF16, True)
    gT_d = dscr("gT", [4096, S], BF16, True)
    yaT_d = dscr("yaT", [8, 128, S], BF16, True)
    ybT_d = dscr("ybT", [8, 128, S], BF16, True)
    mT_d = dscr("mT", [KT, 128, S], BF16, True)
    h2_d = dscr("h2", [S, D], BF16, True)
    modsave_d = dscr("modsave", [4, 128, D], F32, True)
    dbg_d = dscr("dbg", [128, 4096], F32, True)
    wbf_in = dscr("wbf_in", [D, PROJ], BF16)
    wbf_a = dscr("wbf_a", [1024, D], BF16)
    wbf_b = dscr("wbf_b", [1024, D], BF16)
    wbf_o = dscr("wbf_o", [D, D], BF16)
    wbf_eg = dscr("wbf_eg", [NE, D, D], BF16)
    wbf_eu = dscr("wbf_eu", [NE, D, D], BF16)
    wbf_ed = dscr("wbf_ed", [NE, D, D], BF16)

    with ExitStack() as es:
        P = Prog(nc, es)

        uniq = [0]

        def sb(name, shape, dt, stack=es):
            uniq[0] += 1
            return stack.enter_context(nc.sbuf_tensor("sb%d_%s" % (uniq[0], name), list(shape), dt))

        def ps(name, shape, dt, stack=es):
            uniq[0] += 1
            return stack.enter_context(nc.psum_tensor("ps%d_%s" % (uniq[0], name), list(shape), dt))

        ident_f = sb("ident_f", [128, 128], F32)
        ident_b = sb("ident_b", [128, 128], BF16)
        ones_b = sb("ones_b", [128, 128], BF16)
        rstd_all = sb("rstd_all", [128, NT + 2], F32)
        aff_all = sb("aff_all", [128, NT, NE], F32)
        neg_lam = sb("neg_lam", [128, 1], F32)
        gains = sb("gains", [128, 768], F32)
        iota_p = sb("iota_p", [128, 1], F32)
        ones_f = sb("ones_f", [128, 128], F32)
        subln_col = sb("subln_col", [128, 1], F32)
        eps_col = sb("eps_col", [128, 1], F32)
        iota_row = sb("iota_row", [128, 128], F32)

        def convert(src, dst, rows, cols, key):
            for r0 in range(0, rows, 512):
                for c0 in range(0, cols, 2048):
                    P.dma("pool", lambda e, r0=r0, c0=c0: e.dma_start(
                        out=dst[r0:r0 + 512, c0:c0 + 2048], in_=src[r0:r0 + 512, c0:c0 + 2048]),
                        writes=[(key, r0 // 512, c0 // 2048)])

        ph01 = es.enter_context(ExitStack())
        gs1row = sb("gs1row", [128, 2, D], F32, ph01)
        sh1rep = sb("sh1rep", [128, 2, KT, 128], BF16, ph01)
        with ExitStack() as ph:
            P.op("pool", lambda e: e.iota(iota_row[:], pattern=[[1, 128]], base=0, channel_multiplier=0,
                                          allow_small_or_imprecise_dtypes=True), writes=["iota_row"])
            P.op("pool", lambda e: e.iota(iota_p[:], pattern=[[0, 1]], base=0, channel_multiplier=1,
                                          allow_small_or_imprecise_dtypes=True), writes=["iota_p"])
            convert(win_d, wbf_in, D, PROJ, "wbf_in")
            convert(wa_d, wbf_a, 1024, D, "wbf_a")
            convert(wb_d, wbf_b, 1024, D, "wbf_b")
            convert(wo_d, wbf_o, D, D, "wbf_o")
            for e_ in range(NE if upto >= 4 else 0):
                convert(weg_d[e_], wbf_eg[e_], D, D, ("wbf_eg", e_))
                convert(weu_d[e_], wbf_eu[e_], D, D, ("wbf_eu", e_))
                convert(wed_d[e_], wbf_ed[e_], D, D, ("wbf_ed", e_))

            P.op("dve", lambda e: e.tensor_scalar(out=ident_f[:], in0=iota_row[:], scalar1=iota_p[:, 0:1], scalar2=None,
                                                  op0=ALU.is_equal), reads=["iota_row", "iota_p"], writes=["ident_f"])
            P.op("dve", lambda e: e.tensor_copy(out=ident_b[:], in_=ident_f[:]), reads=["ident_f"], writes=["ident_b"])
            P.op("dve", lambda e: e.memset(ones_b[:], 1.0), writes=["ones_b"])
            P.op("dve", lambda e: e.memset(ones_f[:], 1.0), writes=["ones_f"])
            P.op("dve", lambda e: e.memset(eps_col[:], EPS), writes=["eps_col"])

            cc = sb("cc", [128, KT, 2], F32, ph)
            csl = sb("csl", [128, KT, 2], F32, ph)
            crep = sb("crep", [128, KT, 2, 128], F32, ph)
            P.dma("sp", lambda e: e.dma_start(out=cc[:], in_=cc_d[:, :, :]), writes=["cc"])
            P.op("act", lambda e: e.activation(out=csl[:], in_=cc[:], func=AF.Silu), reads=["cc"], writes=["csl"])
            P.op("dve", lambda e: e.tensor_copy(out=crep[:], in_=csl[:].unsqueeze(3).to_broadcast([128, KT, 2, 128])),
                 reads=["csl"], writes=["crep"])
            modrow = sb("modrow", [128, 6 * D], F32, ph)
            modrow_c = sb("modrow_c", [128, 2 * D], F32, ph)
            MB = 512
            wmb = [sb("wmb%d" % i, [128, KT, MB], F32, ph) for i in range(2)]
            bmb = [sb("bmb%d" % i, [128, MB], F32, ph) for i in range(2)]
            psm = [ps("psm%d" % i, [128, MB], F32, ph) for i in range(4)]
            npm = 0
            nblk = 6 * D // MB
            for blk in range(nblk):
                i = blk % 2
                P.dma("sp", lambda e: e.dma_start(out=wmb[i][:], in_=wmod_d[:, blk * MB:(blk + 1) * MB].rearrange("(k p) c -> p k c", p=128)),
                      writes=[("wmb", i)])
                P.dma("sp", lambda e: e.dma_start(out=bmb[i][:], in_=bmod_d[0:1, blk * MB:(blk + 1) * MB].partition_broadcast(128)),
                      writes=[("bmb", i)])
                for j in range(2 if blk < 2 * D // MB else 1):
                    pt = psm[npm % 4]
                    pk = ("psm", npm % 4)
                    npm += 1
                    for k in range(KT):
                        P.op("pe", lambda e: e.matmul(pt[:], lhsT=crep[:, k, j, :], rhs=wmb[i][:, k, :], start=(k == 0), stop=(k == KT - 1)),
                             reads=["crep", ("wmb", i)], writes=[pk])
                    dst = modrow if j == 0 else modrow_c
                    P.op("dve", lambda e: e.tensor_tensor(out=dst[:, blk * MB:(blk + 1) * MB], in0=pt[:], in1=bmb[i][:], op=ALU.add),
                         reads=[pk, ("bmb", i)], writes=[("modrow", j, blk)])
            g12 = sb("g12", [128, 2, D], F32, ph)
            P.dma("sp", lambda e: e.dma_start(out=g12[:, 0, :], in_=g12_d[0:1, :].partition_broadcast(128)), writes=["g12a"])
            P.dma("sp", lambda e: e.dma_start(out=g12[:, 1, :], in_=g12_d[1:2, :].partition_broadcast(128)), writes=["g12b"])
            mr_all = [("modrow", 0, b_) for b_ in range(nblk)]
            mrc_all = [("modrow", 1, b_) for b_ in range(2 * D // MB)]
            P.op("dve", lambda e: e.scalar_tensor_tensor(out=gs1row[:, 0, :], in0=modrow[:, D:2 * D], scalar=1.0, in1=g12[:, 0, :],
                                                         op0=ALU.add, op1=ALU.mult), reads=mr_all + ["g12a"], writes=["gs1row0"])
            P.op("dve", lambda e: e.scalar_tensor_tensor(out=gs1row[:, 1, :], in0=modrow_c[:, D:2 * D], scalar=1.0, in1=g12[:, 0, :],
                                                         op0=ALU.add, op1=ALU.mult), reads=mrc_all + ["g12a"], writes=["gs1row1"])
            P.op("dve", lambda e: e.scalar_tensor_tensor(out=modrow[:, 4 * D:5 * D], in0=modrow[:, 4 * D:5 * D], scalar=1.0, in1=g12[:, 1, :],
                                                         op0=ALU.add, op1=ALU.mult), reads=mr_all + ["g12b"], writes=mr_all)
            for j in range(4):
                P.dma("sp", lambda e: e.dma_start(out=modsave_d[j], in_=modrow[:, (2 + j) * D:(3 + j) * D]), reads=mr_all, writes=[("modsave", j)])
            dtmp = sb("dtmp", [128, KT, 128], F32, ph)
            shc = sb("shc", [128, 2, KT], F32, ph)
            for j in range(2):
                src = modrow if j == 0 else modrow_c
                P.op("dve", lambda e: e.tensor_tensor(out=dtmp[:], in0=src[:, 0:D].rearrange("p (k m) -> p k m", m=128),
                                                      in1=ident_f[:].unsqueeze(1).to_broadcast([128, KT, 128]), op=ALU.mult),
                     reads=(mr_all if j == 0 else mrc_all) + ["ident_f"], writes=["dtmp"])
                P.op("dve", lambda e: e.tensor_reduce(out=shc[:, j, :], in_=dtmp[:], axis=AX.X, op=ALU.add), reads=["dtmp"], writes=[("shc", j)])
                P.op("dve", lambda e: e.tensor_copy(out=sh1rep[:, j, :, :], in_=shc[:, j, :].unsqueeze(2).to_broadcast([128, KT, 128])),
                     reads=[("shc", j)], writes=[("sh1rep", j)])
            P.dma("sp", lambda e: e.dma_start(out=gains[:], in_=smallp_d[0:1, :].partition_broadcast(128)), writes=["gains"])
            lt = sb("lt", [128, 2, 64], F32, ph)
            ls = sb("ls", [128, 4], F32, ph)
            P.op("dve", lambda e: e.tensor_tensor(out=lt[:, 0, :], in0=gains[:, 128:192], in1=gains[:, 192:256], op=ALU.mult), reads=["gains"], writes=["lt0"])
            P.op("dve", lambda e: e.tensor_tensor(out=lt[:, 1, :], in0=gains[:, 256:320], in1=gains[:, 320:384], op=ALU.mult), reads=["gains"], writes=["lt1"])
            P.op("dve", lambda e: e.tensor_reduce(out=ls[:, 0:2], in_=lt[:], axis=AX.X, op=ALU.add), reads=["lt0", "lt1"], writes=["ls01"])
            P.op("act", lambda e: e.activation(out=ls[:, 2:4], in_=ls[:, 0:2], func=AF.Exp), reads=["ls01"], writes=["ls23"])
            P.op("dve", lambda e: e.tensor_tensor(out=ls[:, 0:1], in0=ls[:, 3:4], in1=ls[:, 2:3], op=ALU.subtract), reads=["ls23"], writes=["ls0"])
            P.op("dve", lambda e: e.tensor_scalar(out=neg_lam[:], in0=ls[:, 0:1], scalar1=-LAM_INIT, scalar2=None, op0=ALU.add),
                 reads=["ls0"], writes=["neg_lam"])
            P.op("dve", lambda e: e.tensor_scalar(out=gains[:, 0:64], in0=gains[:, 0:64], scalar1=0.125, scalar2=None, op0=ALU.mult),
                 reads=["gains", "lt0", "lt1"], writes=["gains"])
            P.op("dve", lambda e: e.tensor_scalar(out=gains[:, 384:512], in0=gains[:, 384:512], scalar1=1.0 - LAM_INIT, scalar2=None, op0=ALU.mult),
                 reads=["gains"], writes=["gains"])
            P.op("dve", lambda e: e.tensor_scalar(out=gains[:, 512:640], in0=gains[:, 512:640], scalar1=128.0 ** -0.5, scalar2=None, op0=ALU.mult),
                 reads=["gains"], writes=["gains"])
            sdt = sb("sdt", [128, 128], F32, ph)
            P.op("dve", lambda e: e.tensor_tensor(out=sdt[:], in0=gains[:, 384:512], in1=ident_f[:], op=ALU.mult), reads=["gains", "ident_f"], writes=["sdt"])
            P.op("dve", lambda e: e.tensor_reduce(out=subln_col[:], in_=sdt[:], axis=AX.X, op=ALU.add), reads=["sdt"], writes=["subln_col"])
            if debug:
                P.dma("sp", lambda e: e.dma_start(out=dbg_d[:, 0:768], in_=gains[:]), reads=["gains"], writes=["dbg0"])
                P.dma("sp", lambda e: e.dma_start(out=dbg_d[:, 768:769], in_=neg_lam[:], allow_slow_non_contiguous=True), reads=["neg_lam"], writes=["dbg1"])
                P.dma("sp", lambda e: e.dma_start(out=dbg_d[:, 1024:1024 + 2 * KT], in_=shc[:].rearrange("p a k -> p (a k)")),
                      reads=[("shc", 0), ("shc", 1)], writes=["dbg2"])
            P.barrier()
        if upto < 1:
            P.barrier(pool_ring=True)
            return nc, P

        GT = 8
        with ExitStack() as ph:
            xT = sb("xT", [128, KT, GT * 128], BF16, ph)
            wbuf = [sb("wbuf%d" % i, [128, KT, 512], BF16, ph) for i in range(2)]
            ropet = sb("ropet", [128, GT, 2, 64], F32, ph)
            xt = [sb("xt%d" % i, [128, D], F32, ph) for i in range(2)]
            xb = [sb("xb%d" % i, [128, D], BF16, ph) for i in range(2)]
            junk = sb("junk", [128, D], BF16, ph)
            ssq = sb("ssq", [128, 2], F32, ph)
            shwb = [sb("shwb%d" % i, [128, 512], F32, ph) for i in range(2)]
            NB = 3
            pv = [sb("pv%d" % i, [128, 512], F32, ph) for i in range(NB)]
            sq = [sb("sq%d" % i, [128, 512], F32, ph) for i in range(NB)]
            qn = [sb("qn%d" % i, [128, 512], F32, ph) for i in range(NB)]
            t1 = [sb("t1%d" % i, [128, 512], F32, ph) for i in range(NB)]
            t2 = [sb("t2%d" % i, [128, 512], F32, ph) for i in range(NB)]
            s8 = [sb("s8%d" % i, [128, 8], F32, ph) for i in range(NB)]
            qo = [sb("qo%d" % i, [128, 512], BF16, ph) for i in range(NB)]
            stage = [sb("stage%d" % i, [128, 4, GT * 128], BF16, ph) for i in range(2)]
            pT = [ps("pT%d" % i, [128, 4, 128], BF16, ph) for i in range(2)]
            pM = [ps("pM%d" % i, [128, 512], F32, ph) for i in range(3)]
            pW = ps("pW", [128, 512], F32, ph)
            cnt = {"pT": 0, "pM": 0, "pp": 0, "stage": 0, "w": 0}

            blocks = []
            for cb in range(20):
                c0 = cb * 512
                if c0 < OFF_QB:
                    blocks.append(("qa", cb, c0 // 128))
                elif c0 < OFF_GATE:
                    blocks.append(("qb", cb, (c0 - OFF_QB) // 128))
                elif c0 < OFF_KA:
                    blocks.append(("gate", cb, (c0 - OFF_GATE) // 128))
                elif c0 < OFF_VA:
                    blocks.append(("ka", cb, (c0 - OFF_KA) // 128))
                elif c0 < OFF_KB:
                    blocks.append(("va", cb, (c0 - OFF_VA)))
                elif c0 < OFF_VB:
                    blocks.append(("kb", cb, (c0 - OFF_KB) // 128))
                else:
                    blocks.append(("vb", cb, (c0 - OFF_VB)))

            groups = [("lat", g) for g in range(NT // GT)] + [("ctx", 0)]
            for gkind, g in groups:
                is_ctx = gkind == "ctx"
                ntile = 2 if is_ctx else GT
                mj = 1 if is_ctx else 0
                src_d = ctx_d if is_ctx else x_d
                tok0 = 0 if is_ctx else g * GT * 128
                key0 = 0 if is_ctx else L + g * GT * 128
                if not is_ctx:
                    P.dma("sp", lambda e: e.dma_start(out=ropet[:], in_=rope_d[:, g * GT:(g + 1) * GT, :, :]), writes=["ropet"])
                for tt in range(ntile):
                    i = tt % 2
                    rcol = (NT + tt) if is_ctx else (g * GT + tt)
                    P.dma("sp", lambda e: e.dma_start(out=xt[i][:], in_=src_d[tok0 + tt * 128: tok0 + (tt + 1) * 128, :]), writes=[("xt", i)])
                    P.op("act", lambda e: e.activation(out=junk[:], in_=xt[i][:], func=AF.Square, accum_out=ssq[:, 0:1]),
                         reads=[("xt", i)], writes=["junk", "ssq0"])
                    P.op("act", lambda e: e.activation(out=ssq[:, 1:2], in_=ssq[:, 0:1], func=AF.Sqrt, scale=1.0 / D, bias=EPS),
                         reads=["ssq0"], writes=["ssq1"])
                    P.op("dve", lambda e: e.reciprocal(out=rstd_all[:, rcol:rcol + 1], in_=ssq[:, 1:2]), reads=["ssq1"], writes=[("rstd", rcol)])
                    P.op("dve", lambda e: e.tensor_tensor(out=xb[i][:], in0=xt[i][:], in1=gs1row[:, mj, :], op=ALU.mult),
                         reads=[("xt", i), "gs1row%d" % mj], writes=[("xb", i)])
                    for j4 in range(4):
                        pi = cnt["pT"] % 2
                        cnt["pT"] += 1
                        for jj in range(4):
                            k = j4 * 4 + jj
                            P.op("pe", lambda e: e.transpose(out=pT[pi][:, jj, :], in_=xb[i][:, k * 128:(k + 1) * 128], identity=ident_b[:]),
                                 reads=[("xb", i)], writes=[("pT", pi)])
                        eng = "act" if j4 % 2 == 0 else "dve"
                        if eng == "act":
                            P.op("act", lambda e: e.copy(out=xT[:, j4 * 4:(j4 + 1) * 4, tt * 128:(tt + 1) * 128], in_=pT[pi][:]),
                                 reads=[("pT", pi)], writes=[("xT", tt)])
                        else:
                            P.op("dve", lambda e: e.tensor_copy(out=xT[:, j4 * 4:(j4 + 1) * 4, tt * 128:(tt + 1) * 128], in_=pT[pi][:]),
                                 reads=[("pT", pi)], writes=[("xT", tt)])
                for kind, cb, hoff in blocks:
                    if is_ctx and kind in ("qa", "qb", "gate"):
                        continue
                    wi = cnt["w"] % 2
                    cnt["w"] += 1
                    wkeys = [("wbf_in", r_, (cb * 512) // 2048) for r_ in range(4)]
                    P.dma("sp", lambda e: e.dma_start(out=wbuf[wi][:], in_=wbf_in[:, cb * 512:(cb + 1) * 512].rearrange("(k p) c -> p k c", p=128)),
                          reads=wkeys, writes=[("wbuf", wi)])
                    for k in range(KT):
                        P.op("pe", lambda e: e.matmul(pW[:], lhsT=sh1rep[:, mj, k, :], rhs=wbuf[wi][:, k, :], start=(k == 0), stop=(k == KT - 1)),
                             reads=[("sh1rep", mj), ("wbuf", wi)], writes=["pW"])
                    P.op("act", lambda e: e.copy(out=shwb[wi][:], in_=pW[:]), reads=["pW"], writes=[("shwb", wi)])
                    need_stage = kind in ("qa", "qb", "ka", "kb", "gate")
                    if need_stage:
                        si = cnt["stage"] % 2
                        cnt["stage"] += 1
                    pending = []
                    for tt in range(ntile):
                        rcol = (NT + tt) if is_ctx else (g * GT + tt)
                        mi = cnt["pM"] % 3
                        cnt["pM"] += 1
                        for k in range(KT):
                            P.op("pe", lambda e: e.matmul(pM[mi][:], lhsT=xT[:, k, tt * 128:(tt + 1) * 128], rhs=wbuf[wi][:, k, :],
                                                          start=(k == 0), stop=(k == KT - 1)),
                                 reads=[("xT", tt), ("wbuf", wi)], writes=[("pM", mi)])
                        while len(pending) > 1:
                            pending.pop(0)()
                        bi = cnt["pp"] % NB
                        cnt["pp"] += 1
                        if kind in ("va", "vb"):
                            P.op("dve", lambda e: e.scalar_tensor_tensor(out=qo[bi][:], in0=pM[mi][:], scalar=rstd_all[:, rcol:rcol + 1],
                                                                         in1=shwb[wi][:], op0=ALU.mult, op1=ALU.add),
                                 reads=[("pM", mi), ("rstd", rcol), ("shwb", wi)], writes=[("qo", bi)])
                            dst = va_d if kind == "va" else vb_d
                            P.dma("act", lambda e: e.dma_start(out=dst[key0 + tt * 128:key0 + (tt + 1) * 128, hoff:hoff + 512], in_=qo[bi][:]),
                                  reads=[("qo", bi)], writes=[(kind, key0 + tt * 128, hoff)])
                            continue
                        P.op("dve", lambda e: e.scalar_tensor_tensor(out=pv[bi][:], in0=pM[mi][:], scalar=rstd_all[:, rcol:rcol + 1],
                                                                     in1=shwb[wi][:], op0=ALU.mult, op1=ALU.add),
                             reads=[("pM", mi), ("rstd", rcol), ("shwb", wi)], writes=[("pv", bi)])
                        if kind == "gate":
                            P.op("act", lambda e: e.activation(out=qo[bi][:], in_=pv[bi][:], func=AF.Sigmoid), reads=[("pv", bi)], writes=[("qo", bi)])
                        else:
                            npc, wdt = (8, 64) if kind in ("qa", "ka") else (4, 128)
                            goff = {"qa": 0, "ka": 64, "qb": 512, "kb": 640}[kind]
                            P.op("act", lambda e: e.activation(out=sq[bi][:], in_=pv[bi][:], func=AF.Square), reads=[("pv", bi)], writes=[("sq", bi)])
                            P.op("dve", lambda e: e.tensor_reduce(out=s8[bi][:, 0:npc], in_=sq[bi][:].rearrange("p (a b) -> p a b", b=wdt),
                                                                  axis=AX.X, op=ALU.add), reads=[("sq", bi)], writes=[("s8", bi)])
                            P.op("act", lambda e: e.activation(out=s8[bi][:, 0:npc], in_=s8[bi][:, 0:npc], func=AF.Sqrt, scale=1.0 / wdt, bias=EPS),
                                 reads=[("s8", bi)], writes=[("s8", bi)])
                            P.op("dve", lambda e: e.reciprocal(out=s8[bi][:, 0:npc], in_=s8[bi][:, 0:npc]), reads=[("s8", bi)], writes=[("s8", bi)])
                            P.op("dve", lambda e: e.tensor_tensor(out=qn[bi][:].rearrange("p (a b) -> p a b", b=wdt),
                                                                  in0=pv[bi][:].rearrange("p (a b) -> p a b", b=wdt),
                                                                  in1=s8[bi][:, 0:npc].unsqueeze(2).to_broadcast([128, npc, wdt]), op=ALU.mult),
                                 reads=[("pv", bi), ("s8", bi)], writes=[("qn", bi)])
                            rope_on = kind in ("qa", "ka") and not is_ctx
                            gdst = qn[bi] if rope_on else qo[bi]
                            P.op("dve", lambda e: e.tensor_tensor(out=gdst[:].rearrange("p (a b) -> p a b", b=wdt),
                                                                  in0=qn[bi][:].rearrange("p (a b) -> p a b", b=wdt),
                                                                  in1=gains[:, goff:goff + wdt].unsqueeze(1).to_broadcast([128, npc, wdt]), op=ALU.mult),
                                 reads=[("qn", bi), "gains"], writes=[("qn", bi) if rope_on else ("qo", bi)])
                            if rope_on:
                                q5 = qn[bi][:].rearrange("p (a r h w) -> p a r h w", a=8, r=2, h=2, w=16)
                                t5 = t2[bi][:].rearrange("p (a r h w) -> p a r h w", a=8, r=2, h=2, w=16)
                                cosb = ropet[:, tt, 0, :].unsqueeze(1).to_broadcast([128, 8, 64])
                                ss4 = ropet[:, tt, 1, :].rearrange("p (r h w) -> p r h w", r=2, h=2, w=16)
                                P.op("dve", lambda e: e.tensor_tensor(out=t1[bi][:].rearrange("p (a b) -> p a b", b=64),
                                                                      in0=qn[bi][:].rearrange("p (a b) -> p a b", b=64), in1=cosb, op=ALU.mult),
                                     reads=[("qn", bi), "ropet"], writes=[("t1", bi)])
                                for r_ in range(2):
                                    for h_ in range(2):
                                        P.op("dve", lambda e: e.tensor_tensor(out=t5[:, :, r_, h_, :], in0=q5[:, :, r_, 1 - h_, :],
                                                                              in1=ss4[:, r_, h_, :].unsqueeze(1).to_broadcast([128, 8, 16]), op=ALU.mult),
                                             reads=[("qn", bi), "ropet"], writes=[("t2", bi, r_, h_)])
                                P.op("dve", lambda e: e.tensor_tensor(out=qo[bi][:], in0=t1[bi][:], in1=t2[bi][:], op=ALU.add),
                                     reads=[("t1", bi)] + [("t2", bi, r_, h_) for r_ in range(2) for h_ in range(2)], writes=[("qo", bi)])
                        def do_tr(bi=bi, si=si, tt=tt):
                            pi = cnt["pT"] % 2
                            cnt["pT"] += 1
                            for jj in range(4):
                                P.op("pe", lambda e: e.transpose(out=pT[pi][:, jj, :], in_=qo[bi][:, jj * 128:(jj + 1) * 128], identity=ident_b[:]),
                                     reads=[("qo", bi)], writes=[("pT", pi)])
                            P.op("act", lambda e: e.copy(out=stage[si][:, :, tt * 128:(tt + 1) * 128], in_=pT[pi][:]),
                                 reads=[("pT", pi)], writes=[("stage", si)])
                        pending.append(do_tr)
                    while pending:
                        pending.pop(0)()
                    if need_stage:
                        nt_ = ntile * 128
                        if kind == "gate":
                            r0 = hoff * 128
                            P.dma("act", lambda e: e.dma_start(out=gT_d[r0:r0 + 512, tok0:tok0 + nt_].rearrange("(j p) t -> p j t", p=128),
                                                              in_=stage[si][:, :, 0:nt_]), reads=[("stage", si)], writes=[("gT", cb, g)])
                        else:
                            dst = {"qa": qaT_d, "ka": kaT_d, "qb": qbT_d, "kb": kbT_d}[kind]
                            o0 = tok0 if kind in ("qa", "qb") else key0
                            P.dma("act", lambda e: e.dma_start(out=dst[hoff:hoff + 4, :, o0:o0 + nt_].rearrange("h p t -> p h t"),
                                                              in_=stage[si][:, :, 0:nt_]), reads=[("stage", si)], writes=[(kind, cb, g, gkind)])
            P.barrier()
        ph01.close()
        if upto < 2:
            P.barrier(pool_ring=True)
            return nc, P

        NKC = NK // 128
        with ExitStack() as ph:
            qT = [sb("aqT%d" % i, [128, S], BF16, ph) for i in range(2)]
            kT = [sb("akT%d" % i, [128, NK], BF16, ph) for i in range(2)]
            vv = [sb("avv%d" % i, [128, NKC, 128], BF16, ph) for i in range(2)]
            pTb = [sb("apT%d" % i, [128, 2, 512], BF16, ph) for i in range(3)]
            accS = sb("aaccS", [128, 2, 512], F32, ph)
            accP = sb("aaccP", [128, 512], F32, ph)
            rb = [sb("arb%d" % i, [128, 512], F32, ph) for i in range(2)]
            tta = sb("atta", [128, 512], F32, ph)
            yy = sb("ayy", [128, 512], F32, ph)
            ysq = sb("aysq", [128, 512], F32, ph)
            rstdb = sb("arstdb", [128, 512], F32, ph)
            yo = [sb("ayo%d" % i, [128, 512], BF16, ph) for i in range(2)]
            psS = [ps("apsS%d" % i, [128, 2, 512], F32, ph) for i in range(2)]
            psO = [ps("apsO%d" % i, [128, 512], F32, ph) for i in range(2)]
            psF = [ps("apsF%d" % i, [128, 512], F32, ph) for i in range(2)]
            nhA = lim.get("headsA", 8)

            def load_head_a(h):
                hi = h % 2
                P.dma("sp", lambda e: e.dma_start(out=qT[hi][:], in_=qaT_d[h]), writes=[("qT", hi)])
                P.dma("sp", lambda e: e.dma_start(out=kT[hi][:], in_=kaT_d[h]), writes=[("kT", hi)])
                for c0 in range(0, NKC, 22):
                    P.dma("sp", lambda e: e.dma_start(out=vv[hi][:, c0:c0 + 22, :],
                                                      in_=va_d[c0 * 128:(c0 + 22) * 128, h * 128:(h + 1) * 128].rearrange("(c p) d -> p c d", p=128)),
                          writes=[("vv", hi, c0)])

            nqbA = lim.get("qbA", 16)
            stepsA = [(h, qb, kc) for h in range(nhA) for qb in range(nqbA) for kc in range(NKC)]

            def qk_a(s):
                h, qb, kc = stepsA[s]
                hi = h % 2
                si = s % 2
                for sm in range(2):
                    P.op("pe", lambda e: e.matmul(psS[si][:, sm, :], lhsT=kT[hi][sm * 64:(sm + 1) * 64, kc * 128:(kc + 1) * 128],
                                                  rhs=qT[hi][sm * 64:(sm + 1) * 64, qb * 512:(qb + 1) * 512], start=True, stop=True),
                         reads=[("kT", hi), ("qT", hi)], writes=[("psS", si, sm)])

            load_head_a(0)
            qk_a(0)
            for s, (h, qb, kc) in enumerate(stepsA):
                    hi = h % 2
                    vkeys = [("vv", hi, c0) for c0 in range(0, NKC, 22)]
                    if qb == 1 and kc == 0 and h + 1 < nhA:
                        load_head_a(h + 1)
                    si = s % 2
                    pi = s % 3
                    if s + 1 < len(stepsA):
                        qk_a(s + 1)
                    for sm in range(2):
                        P.op("act", lambda e: e.activation(out=pTb[pi][:, sm, :], in_=psS[si][:, sm, :], func=AF.Exp),
                             reads=[("psS", si, sm)], writes=[("pTb", pi, sm)])
                    if kc == 0:
                        P.op("dve", lambda e: e.tensor_copy(out=accS[:, 0, :], in_=pTb[pi][:, 0, :]), reads=[("pTb", pi, 0)], writes=["accS"])
                    else:
                        P.op("dve", lambda e: e.tensor_tensor(out=accS[:, 0, :], in0=accS[:, 0, :], in1=pTb[pi][:, 0, :], op=ALU.add),
                             reads=[("pTb", pi, 0), "accS"], writes=["accS"])
                    if kc % 2 == 0:
                        if kc == 0:
                            P.op("dve", lambda e: e.tensor_copy(out=accS[:, 1, :], in_=pTb[pi][:, 1, :]), reads=[("pTb", pi, 1)], writes=["accS1"])
                        else:
                            P.op("dve", lambda e: e.tensor_tensor(out=accS[:, 1, :], in0=accS[:, 1, :], in1=pTb[pi][:, 1, :], op=ALU.add),
                                 reads=[("pTb", pi, 1), "accS1"], writes=["accS1"])
                    else:
                        if kc == 1:
                            P.op("pool", lambda e: e.tensor_copy(out=accP[:], in_=pTb[pi][:, 1, :]), reads=[("pTb", pi, 1)], writes=["accP"])
                        else:
                            P.op("pool", lambda e: e.tensor_tensor(out=accP[:], in0=accP[:], in1=pTb[pi][:, 1, :], op=ALU.add),
                                 reads=[("pTb", pi, 1), "accP"], writes=["accP"])
                    for sm in range(2):
                        P.op("pe", lambda e: e.matmul(psO[sm][:], lhsT=vv[hi][:, kc, :], rhs=pTb[pi][:, sm, :], start=(kc == 0), stop=(kc == NKC - 1)),
                             reads=[("pTb", pi, sm)] + vkeys, writes=[("psO", sm)])
                    if kc != NKC - 1:
                        continue
                    yi = qb % 2
                    P.op("dve", lambda e: e.tensor_tensor(out=accS[:, 1, :], in0=accS[:, 1, :], in1=accP[:], op=ALU.add),
                         reads=["accS1", "accP"], writes=["accS1"])
                    for sm in range(2):
                        P.op("pe", lambda e: e.matmul(psF[sm][:], lhsT=ones_f[:], rhs=accS[:, sm, :], start=True, stop=True),
                             reads=["accS" if sm == 0 else "accS1", "ones_f"], writes=[("psF", sm)])
                        P.op("dve", lambda e: e.reciprocal(out=rb[sm][:], in_=psF[sm][:]), reads=[("psF", sm)], writes=[("rb", sm)])
                    P.op("dve", lambda e: e.tensor_scalar(out=rb[1][:], in0=rb[1][:], scalar1=neg_lam[:, 0:1], scalar2=None, op0=ALU.mult),
                         reads=[("rb", 1), "neg_lam"], writes=[("rb", 1)])
                    P.op("dve", lambda e: e.tensor_tensor(out=tta[:], in0=psO[1][:], in1=rb[1][:], op=ALU.mult), reads=[("psO", 1), ("rb", 1)], writes=["tta"])
                    P.op("dve", lambda e: e.tensor_tensor(out=yy[:], in0=psO[0][:], in1=rb[0][:], op=ALU.mult), reads=[("psO", 0), ("rb", 0)], writes=["yy"])
                    P.op("dve", lambda e: e.tensor_tensor(out=yy[:], in0=yy[:], in1=tta[:], op=ALU.add), reads=["yy", "tta"], writes=["yy"])
                    P.op("dve", lambda e: e.tensor_tensor(out=ysq[:], in0=yy[:], in1=yy[:], op=ALU.mult), reads=["yy"], writes=["ysq"])
                    P.op("pe", lambda e: e.matmul(psF[0][:], lhsT=ones_f[:], rhs=ysq[:], start=True, stop=True), reads=["ysq", "ones_f"], writes=[("psF", 0)])
                    P.op("act", lambda e: e.activation(out=rstdb[:], in_=psF[0][:], func=AF.Ln, scale=1.0 / 128, bias=eps_col[:, 0:1]),
                         reads=[("psF", 0), "eps_col"], writes=["rstdb"])
                    P.op("act", lambda e: e.activation(out=rstdb[:], in_=rstdb[:], func=AF.Exp, scale=-0.5), reads=["rstdb"], writes=["rstdb"])
                    P.op("dve", lambda e: e.scalar_tensor_tensor(out=yo[yi][:], in0=yy[:], scalar=subln_col[:, 0:1], in1=rstdb[:], op0=ALU.mult, op1=ALU.mult),
                         reads=["yy", "rstdb", "subln_col"], writes=[("yo", yi)])
                    P.dma("sp", lambda e: e.dma_start(out=yaT_d[h][:, qb * 512:(qb + 1) * 512], in_=yo[yi][:]),
                          reads=[("yo", yi)], writes=[("yaT", h, qb)])
            P.barrier()
        if upto < 3:
            P.barrier(pool_ring=True)
            return nc, P

        with ExitStack() as ph:
            qT = [sb("bqT%d" % i, [128, S], BF16, ph) for i in range(2)]
            kT = [sb("bkT%d" % i, [128, NK], BF16, ph) for i in range(2)]
            vE = [sb("bvE%d" % i, [128, NKC, 129], BF16, ph) for i in range(2)]
            vO = [sb("bvO%d" % i, [128, NKC - 1, 129], BF16, ph) for i in range(2)]
            rpbt = sb("brpbt", [128, 8, 4, 64], F32, ph)
            nam = sb("bnam", [128, 4, 64], F32, ph)
            expB = [sb("bexpB%d" % i, [128, 8, 4, 64], BF16, ph) for i in range(2)]
            Pb = [sb("bPb%d" % i, [128, 6, 64], BF16, ph) for i in range(3)]
            rec = [sb("brec%d" % i, [128, 1], F32, ph) for i in range(2)]
            yb = [sb("byb%d" % i, [128, 128], BF16, ph) for i in range(2)]
            ybst = [sb("bybst%d" % i, [128, 2048], BF16, ph) for i in range(2)]
            psS = [ps("bpsS%d" % i, [128, 6, 64], F32, ph) for i in range(3)]
            accO = [ps("baccO%d" % i, [128, 129], F32, ph) for i in range(2)]
            pTr = ps("bpTr", [128, 128], BF16, ph)
            P.dma("sp", lambda e: e.dma_start(out=nam[:], in_=namask_d[:, :, :]), writes=["nam"])
            for i in range(2):
                P.op("dve", lambda e: e.memset(vE[i][:, :, 128:129], 1.0), writes=[("vE1", i)])
                P.op("dve", lambda e: e.memset(vO[i][:, :, 128:129], 1.0), writes=[("vO1", i)])
            step = 0
            nhB = lim.get("headsB", 8)

            def load_head_b(h):
                hi = h % 2
                P.dma("sp", lambda e: e.dma_start(out=qT[hi][:], in_=qbT_d[h]), writes=[("qT", hi)])
                P.dma("sp", lambda e: e.dma_start(out=kT[hi][:], in_=kbT_d[h]), writes=[("kT", hi)])
                for c0 in range(0, NKC, 22):
                    P.dma("sp", lambda e: e.dma_start(out=vE[hi][:, c0:c0 + 22, 0:128],
                                                      in_=vb_d[c0 * 128:(c0 + 22) * 128, h * 128:(h + 1) * 128].rearrange("(c p) d -> p c d", p=128)),
                          writes=[("vE", hi, c0)])
                for c0, n_ in ((0, 22), (22, 22), (44, 21)):
                    P.dma("sp", lambda e: e.dma_start(out=vO[hi][:, c0:c0 + n_, 0:128],
                                                      in_=vb_d[64 + c0 * 128:64 + (c0 + n_) * 128, h * 128:(h + 1) * 128].rearrange("(c p) d -> p c d", p=128)),
                          writes=[("vO", hi, c0)])

            npB = lim.get("pairsB", 64)
            stepsB = [(h, j, r2) for h in range(nhB) for j in range(npB) for r2 in range(2)]

            def rowinfo(j, r2):
                r = 2 * j + r2
                r_start = min(max(r - 4, 0), 120)
                return r, r_start, r - r_start, L + r_start * 64

            def qk_b(s):
                h, j, r2 = stepsB[s]
                hi = h % 2
                si = s % 3
                r, r_start, v, key0 = rowinfo(j, r2)
                for c in range(6):
                    ko = c * 128 if c < 2 else key0 + (c - 2) * 128
                    P.op("pe", lambda e: e.matmul(psS[si][:, c, :], lhsT=kT[hi][:, ko:ko + 128], rhs=qT[hi][:, r * 64:(r + 1) * 64],
                                                  start=True, stop=True, skip_group_check=True),
                         reads=[("kT", hi), ("qT", hi)], writes=[("psS", si)])

            def load_bias(h):
                hi = h % 2
                P.dma("sp", lambda e: e.dma_start(out=rpbt[:], in_=rpbx_d[h]), writes=["rpbt"])
                P.op("act", lambda e: e.activation(out=rpbt[:], in_=rpbt[:], func=AF.Exp), reads=["rpbt"], writes=["rpbt"])
                P.op("dve", lambda e: e.tensor_tensor(out=expB[hi][:], in0=rpbt[:], in1=nam[:].unsqueeze(1).to_broadcast([128, 8, 4, 64]), op=ALU.mult),
                     reads=["rpbt", "nam"], writes=[("expB", hi)])

            pendB = []
            load_head_b(0)
            load_bias(0)
            qk_b(0)
            for s, (h, j, r2) in enumerate(stepsB):
                hi = h % 2
                si = s % 3
                ai = j % 2
                vEk = [("vE", hi, c0) for c0 in range(0, NKC, 22)] + [("vE1", hi)]
                vOk = [("vO", hi, c0) for c0 in (0, 22, 44)] + [("vO1", hi)]
                if j == 8 and r2 == 0 and h + 1 < nhB:
                    load_head_b(h + 1)
                    load_bias(h + 1)
                r, r_start, v, key0 = rowinfo(j, r2)
                if s + 1 < len(stepsB):
                    qk_b(s + 1)
                P.op("act", lambda e: e.activation(out=Pb[si][:], in_=psS[si][:], func=AF.Exp), reads=[("psS", si)], writes=[("Pb", si)])
                P.op("dve", lambda e: e.tensor_tensor(out=Pb[si][:, 2:6, :], in0=Pb[si][:, 2:6, :], in1=expB[hi][:, v, :, :], op=ALU.mult),
                     reads=[("Pb", si), ("expB", hi)], writes=[("Pb", si)])
                for c in range(6):
                    if c < 2:
                        rhs = vE[hi][:, c, :]
                    elif r_start % 2 == 0:
                        rhs = vE[hi][:, 2 + r_start // 2 + (c - 2), :]
                    else:
                        rhs = vO[hi][:, (192 + r_start * 64) // 128 + (c - 2), :]
                    P.op("pe", lambda e: e.matmul(accO[ai][r2 * 64:(r2 + 1) * 64, :], lhsT=Pb[si][:, c, :], rhs=rhs, start=(c == 0), stop=(c == 5),
                                                  skip_group_check=True),
                         reads=[("Pb", si)] + vEk + vOk, writes=[("accO", ai)])
                while pendB:
                    pendB.pop(0)()
                if r2 == 0:
                    continue
                P.op("dve", lambda e: e.reciprocal(out=rec[ai][:], in_=accO[ai][:, 128:129]), reads=[("accO", ai)], writes=[("rec", ai)])
                P.op("dve", lambda e: e.tensor_scalar(out=yb[ai][:], in0=accO[ai][:, 0:128], scalar1=rec[ai][:, 0:1], scalar2=None, op0=ALU.mult),
                     reads=[("accO", ai), ("rec", ai)], writes=[("yb", ai)])

                def fin_b(ai=ai, j=j, h=h):
                    P.op("pe", lambda e: e.transpose(out=pTr[:], in_=yb[ai][:], identity=ident_b[:]), reads=[("yb", ai)], writes=["pTr"])
                    yi = (j // 16) % 2
                    P.op("act", lambda e: e.copy(out=ybst[yi][:, (j % 16) * 128:(j % 16 + 1) * 128], in_=pTr[:]), reads=["pTr"], writes=[("ybst", yi)])
                    if j % 16 == 15:
                        P.dma("sp", lambda e: e.dma_start(out=ybT_d[h][:, (j // 16) * 2048:(j // 16 + 1) * 2048], in_=ybst[yi][:]),
                              reads=[("ybst", yi)], writes=[("ybT", h, j // 16)])
                pendB.append(fin_b)
            while pendB:
                pendB.pop(0)()
            P.barrier()
        if upto < 4:
            P.barrier(pool_ring=True)
            return nc, P

        with ExitStack() as ph:
            wa = sb("cwa", [128, 8, D], BF16, ph)
            wb = sb("cwb", [128, 8, D], BF16, ph)
            P.dma("sp", lambda e: e.dma_start(out=wa[:], in_=wbf_a.rearrange("(k p) c -> p k c", p=128)),
                  reads=[("wbf_a", r_, 0) for r_ in range(2)], writes=["wa"])
            P.dma("sp", lambda e: e.dma_start(out=wb[:], in_=wbf_b.rearrange("(k p) c -> p k c", p=128)),
                  reads=[("wbf_b", r_, 0) for r_ in range(2)], writes=["wb"])
            yaTg = [sb("cya%d" % i, [128, 8, 512], BF16, ph) for i in range(2)]
            ybTg = [sb("cyb%d" % i, [128, 8, 512], BF16, ph) for i in range(2)]
            gt = [sb("cgt%d" % i, [128, 2, 512], BF16, ph) for i in range(4)]
            t1 = [sb("ct1%d" % i, [128, 512], F32, ph) for i in range(2)]
            t2 = [sb("ct2%d" % i, [128, 512], F32, ph) for i in range(2)]
            mst = [sb("cmst%d" % i, [128, 512], BF16, ph) for i in range(2)]
            psA = [ps("cpsA%d" % i, [128, 512], F32, ph) for i in range(2)]
            psB = [ps("cpsB%d" % i, [128, 512], F32, ph) for i in range(2)]
            n3 = 0
            for tg in range(lim.get("tg3a", S // 512)):
                gi = tg % 2
                tsl = slice(tg * 512, (tg + 1) * 512)
                P.dma("sp", lambda e: e.dma_start(out=yaTg[gi][:], in_=yaT_d[:, :, tsl].rearrange("h p t -> p h t")), writes=[("yaTg", gi)])
                P.dma("sp", lambda e: e.dma_start(out=ybTg[gi][:], in_=ybT_d[:, :, tsl].rearrange("h p t -> p h t")), writes=[("ybTg", gi)])
                for dc in range(KT):
                    pi = n3 % 2
                    g4 = n3 % 4
                    n3 += 1
                    P.dma("sp", lambda e: e.dma_start(out=gt[g4][:, 0, :], in_=gT_d[dc * 128:(dc + 1) * 128, tsl]), writes=[("gt", g4, 0)])
                    P.dma("sp", lambda e: e.dma_start(out=gt[g4][:, 1, :], in_=gT_d[D + dc * 128:D + (dc + 1) * 128, tsl]), writes=[("gt", g4, 1)])
                    for k in range(8):
                        P.op("pe", lambda e: e.matmul(psA[pi][:], lhsT=wa[:, k, dc * 128:(dc + 1) * 128], rhs=yaTg[gi][:, k, :], start=(k == 0), stop=(k == 7)),
                             reads=["wa", ("yaTg", gi)], writes=[("psA", pi)])
                    for k in range(8):
                        P.op("pe", lambda e: e.matmul(psB[pi][:], lhsT=wb[:, k, dc * 128:(dc + 1) * 128], rhs=ybTg[gi][:, k, :], start=(k == 0), stop=(k == 7)),
                             reads=["wb", ("ybTg", gi)], writes=[("psB", pi)])
                    P.op("dve", lambda e: e.tensor_tensor(out=t1[pi][:], in0=psA[pi][:], in1=gt[g4][:, 0, :], op=ALU.mult),
                         reads=[("psA", pi), ("gt", g4, 0)], writes=[("t1", pi)])
                    P.op("dve", lambda e: e.tensor_tensor(out=t2[pi][:], in0=psB[pi][:], in1=gt[g4][:, 1, :], op=ALU.mult),
                         reads=[("psB", pi), ("gt", g4, 1)], writes=[("t2", pi)])
                    P.op("dve", lambda e: e.tensor_tensor(out=mst[pi][:], in0=t1[pi][:], in1=t2[pi][:], op=ALU.add),
                         reads=[("t1", pi), ("t2", pi)], writes=[("mst", pi)])
                    P.dma("act", lambda e: e.dma_start(out=mT_d[dc][:, tsl], in_=mst[pi][:]), reads=[("mst", pi)], writes=[("mT", dc, tg)])
            P.barrier()

        with ExitStack() as ph:
            wo = sb("dwo", [128, KT, D], BF16, ph)
            P.dma("sp", lambda e: e.dma_start(out=wo[:], in_=wbf_o.rearrange("(k p) c -> p k c", p=128)),
                  reads=[("wbf_o", r_, 0) for r_ in range(4)], writes=["wo"])
            wr = sb("dwr", [128, KT, NE], F32, ph)
            P.dma("sp", lambda e: e.dma_start(out=wr[:], in_=wr_d.rearrange("(k p) e -> p k e", p=128)), writes=["wr"])
            rows = sb("drows", [128, 3, D], F32, ph)
            for j in range(3):
                P.dma("sp", lambda e: e.dma_start(out=rows[:, j, :], in_=modsave_d[j]), writes=[("rows", j)])
            mTt = [sb("dmT%d" % i, [128, KT, 128], BF16, ph) for i in range(2)]
            xt = [sb("dxt%d" % i, [128, D], F32, ph) for i in range(2)]
            xn = [sb("dxn%d" % i, [128, D], F32, ph) for i in range(2)]
            h2 = sb("dh2", [128, D], F32, ph)
            h2b = [sb("dh2b%d" % i, [128, D], BF16, ph) for i in range(2)]
            h2T = sb("dh2T", [128, KT, 128], F32, ph)
            junk = sb("djunk", [128, D], BF16, ph)
            sm = [sb("dsm%d" % i, [128, 8], F32, ph) for i in range(2)]
            ex = [sb("dex%d" % i, [128, NE], F32, ph) for i in range(2)]
            pmix = [ps("dpmix%d" % i, [128, 512], F32, ph) for i in range(2)]
            pTf = [ps("dpTf%d" % i, [128, 4, 128], F32, ph) for i in range(2)]
            pR = ps("dpR", [128, NE], F32, ph)
            n3 = 0
            ntf = 0
            for t in range(lim.get("t3b", NT)):
                i = t % 2
                rsl = slice(t * 128, (t + 1) * 128)
                P.dma("sp", lambda e: e.dma_start(out=mTt[i][:], in_=mT_d[:, :, rsl].rearrange("k p t -> p k t")), writes=[("mTt", i)])
                P.dma("sp", lambda e: e.dma_start(out=xt[i][:], in_=x_d[rsl, :]), writes=[("xt", i)])
                for cb in range(4):
                    pi = n3 % 2
                    n3 += 1
                    csl = slice(cb * 512, (cb + 1) * 512)
                    for k in range(KT):
                        P.op("pe", lambda e: e.matmul(pmix[pi][:], lhsT=mTt[i][:, k, :], rhs=wo[:, k, csl], start=(k == 0), stop=(k == KT - 1)),
                             reads=[("mTt", i), "wo"], writes=[("pmix", pi)])
                    P.op("dve", lambda e: e.tensor_tensor(out=xn[i][:, csl], in0=pmix[pi][:], in1=rows[:, 0, csl], op=ALU.mult),
                         reads=[("pmix", pi), ("rows", 0)], writes=[("xn", i, cb)])
                    P.op("dve", lambda e: e.tensor_tensor(out=xn[i][:, csl], in0=xn[i][:, csl], in1=xt[i][:, csl], op=ALU.add),
                         reads=[("xn", i, cb), ("xt", i)], writes=[("xn", i, cb)])
                xnk = [("xn", i, cb) for cb in range(4)]
                P.dma("act", lambda e: e.dma_start(out=out_d[rsl, :], in_=xn[i][:]), reads=xnk, writes=[("out", t)])
                P.op("act", lambda e: e.activation(out=junk[:], in_=xn[i][:], func=AF.Square, accum_out=sm[i][:, 0:1]), reads=xnk, writes=["junk", ("sm", i, 0)])
                P.op("act", lambda e: e.activation(out=sm[i][:, 1:2], in_=sm[i][:, 0:1], func=AF.Sqrt, scale=1.0 / D, bias=EPS),
                     reads=[("sm", i, 0)], writes=[("sm", i, 1)])
                P.op("dve", lambda e: e.reciprocal(out=sm[i][:, 1:2], in_=sm[i][:, 1:2]), reads=[("sm", i, 1)], writes=[("sm", i, 1)])
                P.op("dve", lambda e: e.scalar_tensor_tensor(out=h2[:], in0=xn[i][:], scalar=sm[i][:, 1:2], in1=rows[:, 2, :], op0=ALU.mult, op1=ALU.mult),
                     reads=xnk + [("sm", i, 1), ("rows", 2)], writes=["h2"])
                P.op("dve", lambda e: e.tensor_tensor(out=h2[:], in0=h2[:], in1=rows[:, 1, :], op=ALU.add), reads=["h2", ("rows", 1)], writes=["h2"])
                P.op("act", lambda e: e.copy(out=h2b[i][:], in_=h2[:]), reads=["h2"], writes=[("h2b", i)])
                P.dma("act", lambda e: e.dma_start(out=h2_d[rsl, :], in_=h2b[i][:]), reads=[("h2b", i)], writes=[("h2d", t)])
                for j4 in range(4):
                    ti = ntf % 2
                    ntf += 1
                    for jj in range(4):
                        k = j4 * 4 + jj
                        P.op("pe", lambda e: e.transpose(out=pTf[ti][:, jj, :], in_=h2[:, k * 128:(k + 1) * 128], identity=ident_f[:]),
                             reads=["h2"], writes=[("pTf", ti)])
                    if j4 % 2 == 0:
                        P.op("act", lambda e: e.copy(out=h2T[:, j4 * 4:(j4 + 1) * 4, :], in_=pTf[ti][:]), reads=[("pTf", ti)], writes=[("h2T", j4)])
                    else:
                        P.op("dve", lambda e: e.tensor_copy(out=h2T[:, j4 * 4:(j4 + 1) * 4, :], in_=pTf[ti][:]), reads=[("pTf", ti)], writes=[("h2T", j4)])
                for k in range(KT):
                    P.op("pe", lambda e: e.matmul(pR[:], lhsT=h2T[:, k, :], rhs=wr[:, k, :], start=(k == 0), stop=(k == KT - 1)),
                         reads=[("h2T", k // 4), "wr"], writes=["pR"])
                P.op("dve", lambda e: e.tensor_reduce(out=sm[i][:, 2:3], in_=pR[:], axis=AX.X, op=ALU.max), reads=["pR"], writes=[("sm", i, 2)])
                P.op("dve", lambda e: e.tensor_scalar(out=sm[i][:, 3:4], in0=sm[i][:, 2:3], scalar1=-1.0, scalar2=None, op0=ALU.mult),
                     reads=[("sm", i, 2)], writes=[("sm", i, 3)])
                P.op("act", lambda e: e.activation(out=ex[i][:], in_=pR[:], func=AF.Exp, bias=sm[i][:, 3:4], accum_out=sm[i][:, 4:5]),
                     reads=["pR", ("sm", i, 3)], writes=[("ex", i), ("sm", i, 4)])
                P.op("dve", lambda e: e.reciprocal(out=sm[i][:, 5:6], in_=sm[i][:, 4:5]), reads=[("sm", i, 4)], writes=[("sm", i, 5)])
                P.op("dve", lambda e: e.tensor_scalar(out=aff_all[:, t, :], in0=ex[i][:], scalar1=sm[i][:, 5:6], scalar2=None, op0=ALU.mult),
                     reads=[("ex", i), ("sm", i, 5)], writes=[("aff", t)])
            if debug:
                P.dma("sp", lambda e: e.dma_start(out=dbg_d[:, 2048:2048 + NT * NE], in_=aff_all[:].rearrange("p j e -> p (j e)")),
                      reads=[("aff", t_) for t_ in range(lim.get("t3b", NT))], writes=["dbg3"])
            P.barrier()
        if upto < 5:
            P.barrier(pool_ring=True)
            return nc, P

        with ExitStack() as ph:
            meta = sb("emeta", [128, NE, 8, 4], F32, ph)
            idx32 = sb("eidx32", [128, NE, 8], I32, ph)
            idx4 = sb("eidx4", [128, NE, 8, 4], I32, ph)
            with ExitStack() as ph4:
                lo = sb("elo", [128, NE], F32, ph4)
                mid = sb("emid", [128, NE], F32, ph4)
                cmpb = sb("ecmp", [128, NT, NE], BF16, ph4)
                cntp = sb("ecntp", [128, NE], F32, ph4)

                gsel = sb("egsel", [128, NE], F32, ph4)
                pC = ps("epC", [128, NE], F32, ph4)
                P.op("dve", lambda e: e.memset(lo[:], 0.0), writes=["lo"])
                for it in range(30):
                    ci = 2.0 ** -(it + 1)
                    P.op("dve", lambda e: e.tensor_scalar(out=mid[:], in0=lo[:], scalar1=ci, scalar2=None, op0=ALU.add), reads=["lo"], writes=["mid"])
                    P.op("dve", lambda e: e.tensor_tensor(out=cmpb[:], in0=aff_all[:], in1=mid[:].unsqueeze(1).to_broadcast([128, NT, NE]), op=ALU.is_ge),
                         reads=["mid"], writes=["cmpb"])
                    P.op("dve", lambda e: e.tensor_reduce(out=cntp[:], in_=cmpb[:].rearrange("p j e -> p e j"), axis=AX.X, op=ALU.add),
                         reads=["cmpb"], writes=["cntp"])
                    P.op("pe", lambda e: e.matmul(pC[:], lhsT=ones_f[:], rhs=cntp[:], start=True, stop=True), reads=["cntp", "ones_f"], writes=["pC"])
                    P.op("dve", lambda e: e.tensor_scalar(out=gsel[:], in0=pC[:], scalar1=CAP - 0.5, scalar2=ci, op0=ALU.is_ge, op1=ALU.mult),
                         reads=["pC"], writes=["gsel"])
                    P.op("dve", lambda e: e.tensor_tensor(out=lo[:], in0=lo[:], in1=gsel[:], op=ALU.add), reads=["lo", "gsel"], writes=["lo"])
                msk = sb("emsk", [128, NT, NE], F32, ph4)
                mskb = sb("emskb", [128, NT, NE], BF16, ph4)
                U = sb("eU", [128, 128], BF16, ph4)
                tot = sb("etot", [128, NE, NT], F32, ph4)
                incl = sb("eincl", [128, NE, NT], F32, ph4)
                ones64 = sb("eones64", [128, NT], F32, ph4)
                pos = sb("epos", [128, NT, NE], F32, ph4)
                posm = sb("eposm", [128, NT, NE], F32, ph4)
                vals = sb("evals", [128, NT, NE, 5], BF16, ph4)
                rres = sb("erres", [128, NT, NE], F32, ph4)
                iota_j = sb("eiotaj", [128, NT], F32, ph4)
                iota_s = sb("eiotas", [128, CAP], F32, ph4)
                oh = [sb("eoh%d" % i, [128, CAP], BF16, ph4) for i in range(3)]
                pP = [ps("epP%d" % i, [128, 512], F32, ph4) for i in range(2)]
                pTt = [ps("epTt%d" % i, [128, 512], F32, ph4) for i in range(2)]
                pM = [ps("epM%d" % i, [128, 8, 8, 8], F32, ph4) for i in range(2)]
                P.op("pool", lambda e: e.iota(iota_j[:], pattern=[[1, NT]], base=0, channel_multiplier=0, allow_small_or_imprecise_dtypes=True),
                     writes=["iota_j"])
                P.op("pool", lambda e: e.iota(iota_s[:], pattern=[[1, CAP]], base=0, channel_multiplier=0, allow_small_or_imprecise_dtypes=True),
                     writes=["iota_s"])
                P.op("dve", lambda e: e.tensor_tensor(out=msk[:], in0=aff_all[:], in1=lo[:].unsqueeze(1).to_broadcast([128, NT, NE]), op=ALU.is_ge),
                     reads=["lo"], writes=["msk"])
                P.op("dve", lambda e: e.tensor_copy(out=mskb[:], in_=msk[:]), reads=["msk"], writes=["mskb"])
                P.op("dve", lambda e: e.tensor_scalar(out=U[:], in0=iota_row[:], scalar1=iota_p[:, 0:1], scalar2=None, op0=ALU.is_gt), writes=["U"])
                P.op("dve", lambda e: e.memset(ones64[:], 1.0), writes=["ones64"])
                mflat = mskb[:].rearrange("p j e -> p (j e)")
                for hf in range(2):
                    P.op("pe", lambda e: e.matmul(pP[hf][:], lhsT=U[:], rhs=mflat[:, hf * 512:(hf + 1) * 512], start=True, stop=True),
                         reads=["U", "mskb"], writes=[("pP", hf)])
                    P.op("pe", lambda e: e.matmul(pTt[hf][:], lhsT=ones_b[:], rhs=mflat[:, hf * 512:(hf + 1) * 512], start=True, stop=True),
                         reads=["mskb"], writes=[("pTt", hf)])
                    P.op("dve", lambda e: e.tensor_copy(out=tot[:, :, hf * 32:(hf + 1) * 32], in_=pTt[hf][:].rearrange("p (j e) -> p e j", e=NE)),
                         reads=[("pTt", hf)], writes=[("tot", hf)])
                for e_ in range(NE):
                    P.op("dve", lambda e: e.tensor_tensor_scan(out=incl[:, e_, :], data0=ones64[:], data1=tot[:, e_, :], initial=0.0, op0=ALU.mult, op1=ALU.add),
                         reads=[("tot", 0), ("tot", 1), "ones64"], writes=[("incl", e_)])
                inck = [("incl", e_) for e_ in range(NE)]
                P.op("dve", lambda e: e.tensor_tensor(out=incl[:], in0=incl[:], in1=tot[:], op=ALU.subtract), reads=inck + [("tot", 0), ("tot", 1)], writes=inck)
                for hf in range(2):
                    P.op("dve", lambda e: e.tensor_tensor(out=pos[:, hf * 32:(hf + 1) * 32, :], in0=pP[hf][:].rearrange("p (j e) -> p j e", e=NE),
                                                          in1=incl[:, :, hf * 32:(hf + 1) * 32].rearrange("p e j -> p j e"), op=ALU.add),
                         reads=[("pP", hf)] + inck, writes=[("pos", hf)])
                P.op("dve", lambda e: e.scalar_tensor_tensor(out=posm[:], in0=pos[:], scalar=1.0, in1=msk[:], op0=ALU.add, op1=ALU.mult),
                     reads=[("pos", 0), ("pos", 1), "msk"], writes=["posm"])
                P.op("dve", lambda e: e.tensor_scalar(out=posm[:], in0=posm[:], scalar1=-1.0, scalar2=None, op0=ALU.add), reads=["posm"], writes=["posm"])
                P.op("dve", lambda e: e.tensor_copy(out=vals[:, :, :, 0], in_=aff_all[:]), writes=["v0"])
                P.op("dve", lambda e: e.tensor_tensor(out=rres[:], in0=aff_all[:], in1=vals[:, :, :, 0], op=ALU.subtract), reads=["v0"], writes=["rres"])
                P.op("dve", lambda e: e.tensor_copy(out=vals[:, :, :, 1], in_=rres[:]), reads=["rres"], writes=["v1"])
                P.op("dve", lambda e: e.tensor_tensor(out=rres[:], in0=rres[:], in1=vals[:, :, :, 1], op=ALU.subtract), reads=["rres", "v1"], writes=["rres"])
                P.op("dve", lambda e: e.tensor_copy(out=vals[:, :, :, 2], in_=rres[:]), reads=["rres"], writes=["v2"])
                P.op("dve", lambda e: e.tensor_copy(out=vals[:, :, :, 3], in_=iota_p[:, 0:1].unsqueeze(2).to_broadcast([128, NT, NE])), writes=["v3"])
                P.op("dve", lambda e: e.tensor_copy(out=vals[:, :, :, 4], in_=iota_j[:].unsqueeze(2).to_broadcast([128, NT, NE])),
                     reads=["iota_j"], writes=["v4"])
                if debug:
                    P.dma("sp", lambda e: e.dma_start(out=dbg_d[:, 3072:3072 + NT * NE], in_=posm[:].rearrange("p j e -> p (j e)")),
                          reads=["posm"], writes=["dbg4"])
                    P.dma("sp", lambda e: e.dma_start(out=dbg_d[:, 1100:1100 + NE], in_=lo[:]), reads=["lo"], writes=["dbg5"])
                noh = 0
                for e_ in range(NE):
                    for j in range(NT):
                        oi = noh % 3
                        noh += 1
                        P.op("dve", lambda e: e.tensor_scalar(out=oh[oi][:], in0=iota_s[:], scalar1=posm[:, j, e_:e_ + 1], scalar2=None, op0=ALU.is_equal),
                             reads=["iota_s", "posm"], writes=[("oh", oi)])
                        for st in range(8):
                            P.op("pe", lambda e: e.matmul(pM[e_ // 8][:, e_ % 8, st, 0:5], lhsT=oh[oi][:, st * 128:(st + 1) * 128], rhs=vals[:, j, e_, :],
                                                          start=(e_ % 8 == 0 and j == 0 and st == 0), stop=(j == NT - 1), skip_group_check=True),
                                 reads=[("oh", oi), "v0", "v1", "v2", "v3", "v4"], writes=[("pM", e_ // 8)])
                for hf in range(2):
                    esl = slice(hf * 8, hf * 8 + 8)
                    P.op("dve", lambda e: e.tensor_tensor(out=meta[:, esl, :, 0], in0=pM[hf][:, :, :, 0], in1=pM[hf][:, :, :, 1], op=ALU.add) if False else
                         e.tensor_copy(out=meta[:, esl, :, 0:3], in_=pM[hf][:, :, :, 2:5]), reads=[("pM", hf)], writes=[("metaA", hf)])
                    P.op("dve", lambda e: e.tensor_tensor(out=meta[:, esl, :, 0], in0=meta[:, esl, :, 0], in1=pM[hf][:, :, :, 1], op=ALU.add),
                         reads=[("pM", hf), ("metaA", hf)], writes=[("metaA", hf)])
                    P.op("dve", lambda e: e.tensor_tensor(out=meta[:, esl, :, 0], in0=meta[:, esl, :, 0], in1=pM[hf][:, :, :, 0], op=ALU.add),
                         reads=[("pM", hf), ("metaA", hf)], writes=[("metaA", hf)])
                P.op("dve", lambda e: e.tensor_copy(out=meta[:, :, :, 3:4], in_=meta[:, :, :, 3:4]), reads=[("metaA", 0), ("metaA", 1)], writes=["meta"])
                tokf = sb("etokf", [128, NE, 8], F32, ph4)
                tok4 = sb("etok4", [128, NE, 8, 4], F32, ph4)
                P.op("dve", lambda e: e.scalar_tensor_tensor(out=tokf[:], in0=meta[:, :, :, 2], scalar=128.0, in1=meta[:, :, :, 1], op0=ALU.mult, op1=ALU.add),
                     reads=["meta"], writes=["tokf"])
                P.op("dve", lambda e: e.tensor_copy(out=idx32[:], in_=tokf[:]), reads=["tokf"], writes=["idx32"])
                for db in range(4):
                    P.op("dve", lambda e: e.tensor_scalar(out=tok4[:, :, :, db], in0=tokf[:], scalar1=4.0, scalar2=float(db), op0=ALU.mult, op1=ALU.add),
                         reads=["tokf"], writes=[("tok4", db)])
                P.op("dve", lambda e: e.tensor_copy(out=idx4[:], in_=tok4[:]), reads=[("tok4", db) for db in range(4)], writes=["idx4"])
                if debug:
                    P.dma("sp", lambda e: e.dma_start(out=dbg_d[:, 1200:1200 + NE * 8 * 4], in_=meta[:].rearrange("p a b c -> p (a b c)")),
                          reads=["meta"], writes=["dbg6"])
                P.barrier()
            with ExitStack() as ph5:
                ga2row = sb("fga2", [128, D], F32, ph5)
                P.dma("sp", lambda e: e.dma_start(out=ga2row[:], in_=modsave_d[3]), writes=["ga2row"])
                xeT = sb("fxeT", [128, KT, CAP], BF16, ph5)
                actT = sb("factT", [128, KT, CAP], BF16, ph5)
                wring = [sb("fw%d" % i, [128, KT, 512], BF16, ph5) for i in range(4)]
                xg = [sb("fxg%d" % i, [128, D], BF16, ph5) for i in range(2)]
                sa = [sb("fsa%d" % i, [128, 512], F32, ph5) for i in range(2)]
                ysc = [sb("fysc%d" % i, [128, 512], F32, ph5) for i in range(4)]
                psA = [ps("fpsA%d" % i, [128, 512], F32, ph5) for i in range(2)]
                psU = [ps("fpsU%d" % i, [128, 512], F32, ph5) for i in range(2)]
                psY = [ps("fpsY%d" % i, [128, 512], F32, ph5) for i in range(2)]
                pTx = [ps("fpTx%d" % i, [128, 4, 128], BF16, ph5) for i in range(2)]
                out4 = out_d.rearrange("t (q c) -> (t q) c", c=512)
                c5 = {"w": 0, "xg": 0, "tx": 0, "au": 0, "y": 0, "ysc": 0}
                nexp = lim.get("experts", NE)

                def load_w(srcw, keyname, e_, blk):
                    wi = c5["w"] % 4
                    c5["w"] += 1
                    P.dma("sp", lambda e: e.dma_start(out=wring[wi][:], in_=srcw[e_][:, blk * 512:(blk + 1) * 512].rearrange("(k p) c -> p k c", p=128)),
                          reads=[((keyname, e_), r_, 0) for r_ in range(4)], writes=[("wring", wi)])
                    return wi

                def gather(e_):
                    for st in range(8):
                        gi = c5["xg"] % 2
                        c5["xg"] += 1
                        P.dma("pool", lambda e: e.indirect_dma_start(out=xg[gi][:], out_offset=None, in_=h2_d[:, :],
                                                                     in_offset=bass.IndirectOffsetOnAxis(ap=idx32[:, e_, st:st + 1], axis=0)),
                              reads=["idx32"], writes=[("xg", gi)])
                        for j4 in range(4):
                            ti = c5["tx"] % 2
                            c5["tx"] += 1
                            for jj in range(4):
                                k = j4 * 4 + jj
                                P.op("pe", lambda e: e.transpose(out=pTx[ti][:, jj, :], in_=xg[gi][:, k * 128:(k + 1) * 128], identity=ident_b[:]),
                                     reads=[("xg", gi)], writes=[("pTx", ti)])
                            if j4 % 2 == 0:
                                P.op("act", lambda e: e.copy(out=xeT[:, j4 * 4:(j4 + 1) * 4, st * 128:(st + 1) * 128], in_=pTx[ti][:]),
                                     reads=[("pTx", ti)], writes=[("xeT", st)])
                            else:
                                P.op("dve", lambda e: e.tensor_copy(out=xeT[:, j4 * 4:(j4 + 1) * 4, st * 128:(st + 1) * 128], in_=pTx[ti][:]),
                                     reads=[("pTx", ti)], writes=[("xeT", st)])

                prev_sc = []
                gather(0)
                for e_ in range(nexp):
                    for fb in range(4):
                        wg = load_w(wbf_eg, "wbf_eg", e_, fb)
                        wu = load_w(wbf_eu, "wbf_eu", e_, fb)
                        for fc in range(4):
                            for sh in range(2):
                                ai = c5["au"] % 2
                                c5["au"] += 1
                                xk = [("xeT", s_) for s_ in range(sh * 4, sh * 4 + 4)]
                                for k in range(KT):
                                    P.op("pe", lambda e: e.matmul(psA[ai][:], lhsT=wring[wg][:, k, fc * 128:(fc + 1) * 128], rhs=xeT[:, k, sh * 512:(sh + 1) * 512],
                                                                  start=(k == 0), stop=(k == KT - 1)), reads=[("wring", wg)] + xk, writes=[("psA", ai)])
                                for k in range(KT):
                                    P.op("pe", lambda e: e.matmul(psU[ai][:], lhsT=wring[wu][:, k, fc * 128:(fc + 1) * 128], rhs=xeT[:, k, sh * 512:(sh + 1) * 512],
                                                                  start=(k == 0), stop=(k == KT - 1)), reads=[("wring", wu)] + xk, writes=[("psU", ai)])
                                P.op("act", lambda e: e.activation(out=sa[ai][:], in_=psA[ai][:], func=AF.Silu), reads=[("psA", ai)], writes=[("sa", ai)])
                                P.op("dve", lambda e: e.tensor_tensor(out=actT[:, fb * 4 + fc, sh * 512:(sh + 1) * 512], in0=psU[ai][:], in1=sa[ai][:], op=ALU.mult),
                                     reads=[("psU", ai), ("sa", ai)], writes=[("actT", fb * 4 + fc, sh)])
                    if e_ + 1 < nexp:
                        gather(e_ + 1)
                    cur_sc = []
                    ak = [("actT", f_, s_) for f_ in range(KT) for s_ in range(2)]
                    for db in range(4):
                        wd = load_w(wbf_ed, "wbf_ed", e_, db)
                        for st in range(8):
                            yi = c5["y"] % 2
                            c5["y"] += 1
                            for k in range(KT):
                                P.op("pe", lambda e: e.matmul(psY[yi][:], lhsT=actT[:, k, st * 128:(st + 1) * 128], rhs=wring[wd][:, k, :],
                                                              start=(k == 0), stop=(k == KT - 1)),
                                     reads=[("wring", wd)] + [("actT", f_, st // 4) for f_ in range(KT)], writes=[("psY", yi)])
                            si = c5["ysc"] % 4
                            c5["ysc"] += 1
                            P.op("dve", lambda e: e.scalar_tensor_tensor(out=ysc[si][:], in0=psY[yi][:], scalar=meta[:, e_, st, 0:1],
                                                                         in1=ga2row[:, db * 512:(db + 1) * 512], op0=ALU.mult, op1=ALU.mult),
                                 reads=[("psY", yi), "meta", "ga2row"], writes=[("ysc", si)])
                            for ev in prev_sc:
                                P.wait("pool", ev)
                            ev = P.dma("pool", lambda e: e.indirect_dma_start(out=out4[:, :],
                                                                             out_offset=bass.IndirectOffsetOnAxis(ap=idx4[:, e_, st, db:db + 1], axis=0),
                                                                             in_=ysc[si][:], in_offset=None, compute_op=ALU.add),
                                       reads=[("ysc", si), "idx4"], writes=[])
                            cur_sc.append(ev)
                    prev_sc = cur_sc
                P.barrier(pool_ring=True)
        P.barrier(pool_ring=True)
        return nc, P


_CONST_CACHE = {}


def _prep_inputs(inputs):
    if "c" not in _CONST_CACHE:
        _CONST_CACHE["c"] = _consts()
    rope, namask = _CONST_CACHE["c"]
    f = lambda a: np.ascontiguousarray(np.asarray(a, dtype=np.float32))
    x = f(inputs["x"]); ctx = f(inputs["ctx"]); c = f(inputs["c"]); c_ctx = f(inputs["c_ctx"])
    smallp = np.concatenate([f(inputs[k])[0] for k in ("q_gain_a", "k_gain_a", "lam_q1", "lam_k1", "lam_q2", "lam_k2",
                                                       "subln_gain", "q_gain_b", "k_gain_b")]).reshape(1, 768)
    shared = {
        "w_mod": f(inputs["w_mod"])[0], "b_mod": f(inputs["b_mod"])[0].reshape(1, -1),
        "g12": np.stack([f(inputs["g_norm1"])[0], f(inputs["g_norm2"])[0]]),
        "w_in": f(inputs["w_in"])[0], "smallp": smallp,
        "rpbx": _rpb_expand(f(inputs["rel_pos_bias"])[0]), "namask": namask, "rope": rope,
        "w_a": f(inputs["w_branch_a"])[0], "w_b": f(inputs["w_branch_b"])[0], "w_o": f(inputs["w_out"])[0],
        "w_r": f(inputs["w_router"])[0], "w_eg": f(inputs["w_exp_gate"])[0], "w_eu": f(inputs["w_exp_up"])[0],
        "w_ed": f(inputs["w_exp_down"])[0],
    }
    maps = []
    for core in range(N_CORES):
        b = core % 4
        cc = np.stack([c[b].reshape(KT, 128).T, c_ctx.reshape(KT, 128).T], axis=-1)
        m = dict(shared)
        m.update({"x": x[b], "ctx": ctx[b], "cc": np.ascontiguousarray(cc)})
        maps.append(m)
    return maps


def kernel(**inputs):
    maps = _prep_inputs(inputs)
    nc, _ = build()
    res = run_bass_kernel_spmd(nc, maps, core_ids=list(range(N_CORES)))
    out = np.stack([res.results[b]["out"] for b in range(4)], axis=0)
    return out.astype(np.float32)
```

```python
import math
from contextlib import ExitStack

import numpy as np
import concourse.bass as bass
import concourse.mybir as mybir
from concourse.bass_utils import run_bass_kernel_spmd

F32 = mybir.dt.float32
BF16 = mybir.dt.bfloat16
I32 = mybir.dt.int32
AF = mybir.ActivationFunctionType
ALU = mybir.AluOpType
AX = mybir.AxisListType

D = 2048
S = 8192
L = 256
NK = S + L
NT = S // 128
KT = D // 128
GRID_W = 64
PROJ = 10240
OFF_QA, OFF_QB, OFF_GATE, OFF_KA, OFF_VA, OFF_KB, OFF_VB = 0, 1024, 2048, 6144, 7168, 8192, 9216
NE = 16
CAP = 1024
EPS = 1e-6
LAM_INIT = 0.8 - 0.6 * math.exp(0.0)
N_CORES = 8


class Ev:
    __slots__ = ("sem", "name", "val")

    def __init__(self, sem, name, val):
        self.sem, self.name, self.val = sem, name, val


class Prog:
    def __init__(self, nc, es):
        self.nc = nc
        self.eng = {"pe": nc.tensor, "act": nc.scalar, "dve": nc.vector, "pool": nc.gpsimd, "sp": nc.sync}
        self.esem = {}
        self.ecnt = {}
        for e in ("pe", "act", "dve", "pool"):
            self.esem[e] = es.enter_context(nc.semaphore("s_" + e))
            self.ecnt[e] = 0
        self.rings = {}
        self.rpos = {}
        for e, n in (("sp", 12), ("pool", 12), ("act", 8)):
            self.rings[e] = [[es.enter_context(nc.semaphore("d_%s%d" % (e, i))), "d_%s%d" % (e, i), 0] for i in range(n)]
            self.rpos[e] = 0
        self.waited = {e: {} for e in self.eng}
        self.lastw = {}
        self.readers = {}
        self.nins = 0

    def wait(self, eng, ev):
        if ev is None:
            return
        if eng == "pe" and ev.name == "s_pe":
            return
        w = self.waited[eng]
        if w.get(ev.name, 0) >= ev.val:
            return
        self.eng[eng].wait_ge(ev.sem, ev.val)
        w[ev.name] = ev.val
        self.nins += 1

    def _hazards(self, eng, reads, writes):
        for k in reads:
            self.wait(eng, self.lastw.get(k))
        for k in writes:
            self.wait(eng, self.lastw.get(k))
            rd = self.readers.get(k)
            if rd:
                for ev in rd.values():
                    self.wait(eng, ev)

    def _record(self, ev, reads, writes):
        for k in reads:
            self.readers.setdefault(k, {})[ev.name] = ev
        for k in writes:
            self.lastw[k] = ev
            self.readers[k] = {}

    def op(self, eng, fn, reads=(), writes=()):
        self._hazards(eng, reads, writes)
        ins = fn(self.eng[eng])
        self.ecnt[eng] += 1
        ins.then_inc(self.esem[eng], 1)
        ev = Ev(self.esem[eng], "s_" + eng, self.ecnt[eng])
        self._record(ev, reads, writes)
        self.nins += 1
        return ev

    def dma(self, eng, fn, reads=(), writes=()):
        ring = self.rings[eng]
        i = self.rpos[eng]
        self.rpos[eng] = (i + 1) % len(ring)
        sem, name, cnt = ring[i]
        if cnt:
            self.wait(eng, Ev(sem, name, cnt))
        self._hazards(eng, reads, writes)
        ins = fn(self.eng[eng])
        ins.then_inc(sem, 16)
        ring[i][2] = cnt + 16
        ev = Ev(sem, name, cnt + 16)
        self._record(ev, reads, writes)
        self.nins += 1
        return ev

    def barrier(self, pool_ring=False):
        evs = [Ev(self.esem[e], "s_" + e, self.ecnt[e]) for e in self.esem if self.ecnt[e]]
        for e in self.rings:
            if e == "pool" and not pool_ring:
                continue
            for sem, name, cnt in self.rings[e]:
                if cnt:
                    evs.append(Ev(sem, name, cnt))
        for e in self.eng:
            for ev in evs:
                self.wait(e, ev)
        keep = {k: v for k, v in self.lastw.items() if "wbf" in repr(k)}
        self.lastw = keep
        self.readers = {}


def _consts():
    t = np.arange(S)
    row = (t // GRID_W).astype(np.float32)
    col = (t % GRID_W).astype(np.float32)
    half = 32
    inv_freq = (10000.0 ** (-np.arange(0, half, 2, dtype=np.float32) / half)).astype(np.float32)

    def tab(pos):
        ang = pos[:, None] * inv_freq[None, :]
        ang = np.concatenate([ang, ang], axis=-1)
        return np.cos(ang), np.sin(ang)

    cr, sr = tab(row)
    cc, sc = tab(col)
    cos = np.concatenate([cr, cc], -1).astype(np.float32)
    sin = np.concatenate([sr, sc], -1).astype(np.float32)
    ss = sin.reshape(S, 2, 2, 16).copy()
    ss[:, :, 0, :] *= -1.0
    ss = ss.reshape(S, 64)
    rope = np.stack([cos, ss], axis=1)
    rope = np.ascontiguousarray(rope.reshape(NT, 128, 2, 64).transpose(1, 0, 2, 3))
    q = np.arange(64)
    cs = np.clip(q - 8, 0, 48)
    wk = np.arange(64)
    inwin = (wk[None, :] >= cs[:, None]) & (wk[None, :] < cs[:, None] + 16)
    m = np.zeros((128, 4, 64), np.float32)
    for i in range(8):
        m[(i % 2) * 64:(i % 2) * 64 + 64, i // 2, :] = inwin.T.astype(np.float32)
    return rope.astype(np.float32), m


def _rpb_expand(rpb):
    H = rpb.shape[0]
    q = np.arange(64)
    wk = np.arange(64)
    idx_c = np.clip(wk[:, None] - q[None, :] + 15, 0, 30)
    out = np.zeros((H, 128, 8, 4, 64), np.float32)
    for v in range(8):
        for i in range(8):
            idx_r = 7 - v + i
            out[:, (i % 2) * 64:(i % 2) * 64 + 64, v, i // 2, :] = rpb[:, idx_r][:, idx_c]
    return out


def build(debug=False, upto=99, lim=None):
    lim = lim or {}
    nc = bass.Bass("TRN2", target_bir_lowering=False)

    def din(name, shape, dt=F32):
        return nc.dram_tensor(name, list(shape), dt, kind="ExternalInput").ap()

    def dscr(name, shape, dt, dbg=False):
        kind = "ExternalOutput" if (debug and dbg) else "Internal"
        return nc.dram_tensor(name, list(shape), dt, kind=kind).ap()

    x_d = din("x", [S, D])
    ctx_d = din("ctx", [L, D])
    cc_d = din("cc", [128, KT, 2])
    wmod_d = din("w_mod", [D, 6 * D])
    bmod_d = din("b_mod", [1, 6 * D])
    g12_d = din("g12", [2, D])
    win_d = din("w_in", [D, PROJ])
    smallp_d = din("smallp", [1, 768])
    rpbx_d = din("rpbx", [8, 128, 8, 4, 64])
    namask_d = din("namask", [128, 4, 64])
    rope_d = din("rope", [128, NT, 2, 64])
    wa_d = din("w_a", [1024, D])
    wb_d = din("w_b", [1024, D])
    wo_d = din("w_o", [D, D])
    wr_d = din("w_r", [D, NE])
    weg_d = din("w_eg", [NE, D, D])
    weu_d = din("w_eu", [NE, D, D])
    wed_d = din("w_ed", [NE, D, D])
    out_d = nc.dram_tensor("out", [S, D], F32, kind="ExternalOutput").ap()

    qaT_d = dscr("qaT", [8, 128, S], BF16, True)
    kaT_d = dscr("kaT", [8, 128, NK], BF16, True)
    va_d = dscr("va", [NK, 1024], BF16, True)
    qbT_d = dscr("qbT", [8, 128, S], BF16, True)
    kbT_d = dscr("kbT", [8, 128, NK], BF16, True)
    vb_d = dscr("vb", [NK, 1024], BF16, True)
    gT_d = dscr("gT", [4096, S], BF16, True)
    yaT_d = dscr("yaT", [8, 128, S], BF16, True)
    ybT_d = dscr("ybT", [8, 128, S], BF16, True)
    mT_d = dscr("mT", [KT, 128, S], BF16, True)
    h2_d = dscr("h2", [S, D], BF16, True)
    modsave_d = dscr("modsave", [4, 128, D], F32, True)
    dbg_d = dscr("dbg", [128, 4096], F32, True)
    wbf_in = dscr("wbf_in", [D, PROJ], BF16)
    wbf_a = dscr("wbf_a", [1024, D], BF16)
    wbf_b = dscr("wbf_b", [1024, D], BF16)
    wbf_o = dscr("wbf_o", [D, D], BF16)
    wbf_eg = dscr("wbf_eg", [NE, D, D], BF16)
    wbf_eu = dscr("wbf_eu", [NE, D, D], BF16)
    wbf_ed = dscr("wbf_ed", [NE, D, D], BF16)

    with ExitStack() as es:
        P = Prog(nc, es)

        uniq = [0]

        def sb(name, shape, dt, stack=es):
            uniq[0] += 1
            return stack.enter_context(nc.sbuf_tensor("sb%d_%s" % (uniq[0], name), list(shape), dt))

        def ps(name, shape, dt, stack=es):
            uniq[0] += 1
            return stack.enter_context(nc.psum_tensor("ps%d_%s" % (uniq[0], name), list(shape), dt))

        ident_f = sb("ident_f", [128, 128], F32)
        ident_b = sb("ident_b", [128, 128], BF16)
        ones_b = sb("ones_b", [128, 128], BF16)
        rstd_all = sb("rstd_all", [128, NT + 2], F32)
        aff_all = sb("aff_all", [128, NT, NE], F32)
        neg_lam = sb("neg_lam", [128, 1], F32)
        gains = sb("gains", [128, 768], F32)
        iota_p = sb("iota_p", [128, 1], F32)
        ones_f = sb("ones_f", [128, 128], F32)
        subln_col = sb("subln_col", [128, 1], F32)
        eps_col = sb("eps_col", [128, 1], F32)
        iota_row = sb("iota_row", [128, 128], F32)

        def convert(src, dst, rows, cols, key):
            for r0 in range(0, rows, 512):
                for c0 in range(0, cols, 2048):
                    P.dma("pool", lambda e, r0=r0, c0=c0: e.dma_start(
                        out=dst[r0:r0 + 512, c0:c0 + 2048], in_=src[r0:r0 + 512, c0:c0 + 2048]),
                        writes=[(key, r0 // 512, c0 // 2048)])

        ph01 = es.enter_context(ExitStack())
        gs1row = sb("gs1row", [128, 2, D], F32, ph01)
        sh1rep = sb("sh1rep", [128, 2, KT, 128], BF16, ph01)
        with ExitStack() as ph:
            P.op("pool", lambda e: e.iota(iota_row[:], pattern=[[1, 128]], base=0, channel_multiplier=0,
                                          allow_small_or_imprecise_dtypes=True), writes=["iota_row"])
            P.op("pool", lambda e: e.iota(iota_p[:], pattern=[[0, 1]], base=0, channel_multiplier=1,
                                          allow_small_or_imprecise_dtypes=True), writes=["iota_p"])
            convert(win_d, wbf_in, D, PROJ, "wbf_in")
            convert(wa_d, wbf_a, 1024, D, "wbf_a")
            convert(wb_d, wbf_b, 1024, D, "wbf_b")
            convert(wo_d, wbf_o, D, D, "wbf_o")
            for e_ in range(NE if upto >= 4 else 0):
                convert(weg_d[e_], wbf_eg[e_], D, D, ("wbf_eg", e_))
                convert(weu_d[e_], wbf_eu[e_], D, D, ("wbf_eu", e_))
                convert(wed_d[e_], wbf_ed[e_], D, D, ("wbf_ed", e_))

            P.op("dve", lambda e: e.tensor_scalar(out=ident_f[:], in0=iota_row[:], scalar1=iota_p[:, 0:1], scalar2=None,
                                                  op0=ALU.is_equal), reads=["iota_row", "iota_p"], writes=["ident_f"])
            P.op("dve", lambda e: e.tensor_copy(out=ident_b[:], in_=ident_f[:]), reads=["ident_f"], writes=["ident_b"])
            P.op("dve", lambda e: e.memset(ones_b[:], 1.0), writes=["ones_b"])
            P.op("dve", lambda e: e.memset(ones_f[:], 1.0), writes=["ones_f"])
            P.op("dve", lambda e: e.memset(eps_col[:], EPS), writes=["eps_col"])

            cc = sb("cc", [128, KT, 2], F32, ph)
            csl = sb("csl", [128, KT, 2], F32, ph)
            crep = sb("crep", [128, KT, 2, 128], F32, ph)
            P.dma("sp", lambda e: e.dma_start(out=cc[:], in_=cc_d[:, :, :]), writes=["cc"])
            P.op("act", lambda e: e.activation(out=csl[:], in_=cc[:], func=AF.Silu), reads=["cc"], writes=["csl"])
            P.op("dve", lambda e: e.tensor_copy(out=crep[:], in_=csl[:].unsqueeze(3).to_broadcast([128, KT, 2, 128])),
                 reads=["csl"], writes=["crep"])
            modrow = sb("modrow", [128, 6 * D], F32, ph)
            modrow_c = sb("modrow_c", [128, 2 * D], F32, ph)
            MB = 512
            wmb = [sb("wmb%d" % i, [128, KT, MB], F32, ph) for i in range(2)]
            bmb = [sb("bmb%d" % i, [128, MB], F32, ph) for i in range(2)]
            psm = [ps("psm%d" % i, [128, MB], F32, ph) for i in range(4)]
            npm = 0
            nblk = 6 * D // MB
            for blk in range(nblk):
                i = blk % 2
                P.dma("sp", lambda e: e.dma_start(out=wmb[i][:], in_=wmod_d[:, blk * MB:(blk + 1) * MB].rearrange("(k p) c -> p k c", p=128)),
                      writes=[("wmb", i)])
                P.dma("sp", lambda e: e.dma_start(out=bmb[i][:], in_=bmod_d[0:1, blk * MB:(blk + 1) * MB].partition_broadcast(128)),
                      writes=[("bmb", i)])
                for j in range(2 if blk < 2 * D // MB else 1):
                    pt = psm[npm % 4]
                    pk = ("psm", npm % 4)
                    npm += 1
                    for k in range(KT):
                        P.op("pe", lambda e: e.matmul(pt[:], lhsT=crep[:, k, j, :], rhs=wmb[i][:, k, :], start=(k == 0), stop=(k == KT - 1)),
                             reads=["crep", ("wmb", i)], writes=[pk])
                    dst = modrow if j == 0 else modrow_c
                    P.op("dve", lambda e: e.tensor_tensor(out=dst[:, blk * MB:(blk + 1) * MB], in0=pt[:], in1=bmb[i][:], op=ALU.add),
                         reads=[pk, ("bmb", i)], writes=[("modrow", j, blk)])
            g12 = sb("g12", [128, 2, D], F32, ph)
            P.dma("sp", lambda e: e.dma_start(out=g12[:, 0, :], in_=g12_d[0:1, :].partition_broadcast(128)), writes=["g12a"])
            P.dma("sp", lambda e: e.dma_start(out=g12[:, 1, :], in_=g12_d[1:2, :].partition_broadcast(128)), writes=["g12b"])
            mr_all = [("modrow", 0, b_) for b_ in range(nblk)]
            mrc_all = [("modrow", 1, b_) for b_ in range(2 * D // MB)]
            P.op("dve", lambda e: e.scalar_tensor_tensor(out=gs1row[:, 0, :], in0=modrow[:, D:2 * D], scalar=1.0, in1=g12[:, 0, :],
                                                         op0=ALU.add, op1=ALU.mult), reads=mr_all + ["g12a"], writes=["gs1row0"])
            P.op("dve", lambda e: e.scalar_tensor_tensor(out=gs1row[:, 1, :], in0=modrow_c[:, D:2 * D], scalar=1.0, in1=g12[:, 0, :],
                                                         op0=ALU.add, op1=ALU.mult), reads=mrc_all + ["g12a"], writes=["gs1row1"])
            P.op("dve", lambda e: e.scalar_tensor_tensor(out=modrow[:, 4 * D:5 * D], in0=modrow[:, 4 * D:5 * D], scalar=1.0, in1=g12[:, 1, :],
                                                         op0=ALU.add, op1=ALU.mult), reads=mr_all + ["g12b"], writes=mr_all)
            for j in range(4):
                P.dma("sp", lambda e: e.dma_start(out=modsave_d[j], in_=modrow[:, (2 + j) * D:(3 + j) * D]), reads=mr_all, writes=[("modsave", j)])
            dtmp = sb("dtmp", [128, KT, 128], F32, ph)
            shc = sb("shc", [128, 2, KT], F32, ph)
            for j in range(2):
                src = modrow if j == 0 else modrow_c
                P.op("dve", lambda e: e.tensor_tensor(out=dtmp[:], in0=src[:, 0:D].rearrange("p (k m) -> p k m", m=128),
                                                      in1=ident_f[:].unsqueeze(1).to_broadcast([128, KT, 128]), op=ALU.mult),
                     reads=(mr_all if j == 0 else mrc_all) + ["ident_f"], writes=["dtmp"])
                P.op("dve", lambda e: e.tensor_reduce(out=shc[:, j, :], in_=dtmp[:], axis=AX.X, op=ALU.add), reads=["dtmp"], writes=[("shc", j)])
                P.op("dve", lambda e: e.tensor_copy(out=sh1rep[:, j, :, :], in_=shc[:, j, :].unsqueeze(2).to_broadcast([128, KT, 128])),
                     reads=[("shc", j)], writes=[("sh1rep", j)])
            P.dma("sp", lambda e: e.dma_start(out=gains[:], in_=smallp_d[0:1, :].partition_broadcast(128)), writes=["gains"])
            lt = sb("lt", [128, 2, 64], F32, ph)
            ls = sb("ls", [128, 4], F32, ph)
            P.op("dve", lambda e: e.tensor_tensor(out=lt[:, 0, :], in0=gains[:, 128:192], in1=gains[:, 192:256], op=ALU.mult), reads=["gains"], writes=["lt0"])
            P.op("dve", lambda e: e.tensor_tensor(out=lt[:, 1, :], in0=gains[:, 256:320], in1=gains[:, 320:384], op=ALU.mult), reads=["gains"], writes=["lt1"])
            P.op("dve", lambda e: e.tensor_reduce(out=ls[:, 0:2], in_=lt[:], axis=AX.X, op=ALU.add), reads=["lt0", "lt1"], writes=["ls01"])
            P.op("act", lambda e: e.activation(out=ls[:, 2:4], in_=ls[:, 0:2], func=AF.Exp), reads=["ls01"], writes=["ls23"])
            P.op("dve", lambda e: e.tensor_tensor(out=ls[:, 0:1], in0=ls[:, 3:4], in1=ls[:, 2:3], op=ALU.subtract), reads=["ls23"], writes=["ls0"])
            P.op("dve", lambda e: e.tensor_scalar(out=neg_lam[:], in0=ls[:, 0:1], scalar1=-LAM_INIT, scalar2=None, op0=ALU.add),
                 reads=["ls0"], writes=["neg_lam"])
            P.op("dve", lambda e: e.tensor_scalar(out=gains[:, 0:64], in0=gains[:, 0:64], scalar1=0.125, scalar2=None, op0=ALU.mult),
                 reads=["gains", "lt0", "lt1"], writes=["gains"])
            P.op("dve", lambda e: e.tensor_scalar(out=gains[:, 384:512], in0=gains[:, 384:512], scalar1=1.0 - LAM_INIT, scalar2=None, op0=ALU.mult),
                 reads=["gains"], writes=["gains"])
            P.op("dve", lambda e: e.tensor_scalar(out=gains[:, 512:640], in0=gains[:, 512:640], scalar1=128.0 ** -0.5, scalar2=None, op0=ALU.mult),
                 reads=["gains"], writes=["gains"])
            sdt = sb("sdt", [128, 128], F32, ph)
            P.op("dve", lambda e: e.tensor_tensor(out=sdt[:], in0=gains[:, 384:512], in1=ident_f[:], op=ALU.mult), reads=["gains", "ident_f"], writes=["sdt"])
            P.op("dve", lambda e: e.tensor_reduce(out=subln_col[:], in_=sdt[:], axis=AX.X, op=ALU.add), reads=["sdt"], writes=["subln_col"])
            if debug:
                P.dma("sp", lambda e: e.dma_start(out=dbg_d[:, 0:768], in_=gains[:]), reads=["gains"], writes=["dbg0"])
                P.dma("sp", lambda e: e.dma_start(out=dbg_d[:, 768:769], in_=neg_lam[:], allow_slow_non_contiguous=True), reads=["neg_lam"], writes=["dbg1"])
                P.dma("sp", lambda e: e.dma_start(out=dbg_d[:, 1024:1024 + 2 * KT], in_=shc[:].rearrange("p a k -> p (a k)")),
                      reads=[("shc", 0), ("shc", 1)], writes=["dbg2"])
            P.barrier()
        if upto < 1:
            P.barrier(pool_ring=True)
            return nc, P

        GT = 8
        with ExitStack() as ph:
            xT = sb("xT", [128, KT, GT * 128], BF16, ph)
            wbuf = [sb("wbuf%d" % i, [128, KT, 512], BF16, ph) for i in range(2)]
            ropet = sb("ropet", [128, GT, 2, 64], F32, ph)
            xt = [sb("xt%d" % i, [128, D], F32, ph) for i in range(2)]
            xb = [sb("xb%d" % i, [128, D], BF16, ph) for i in range(2)]
            junk = sb("junk", [128, D], BF16, ph)
            ssq = sb("ssq", [128, 2], F32, ph)
            shwb = [sb("shwb%d" % i, [128, 512], F32, ph) for i in range(2)]
            NB = 3
            pv = [sb("pv%d" % i, [128, 512], F32, ph) for i in range(NB)]
            sq = [sb("sq%d" % i, [128, 512], F32, ph) for i in range(NB)]
            qn = [sb("qn%d" % i, [128, 512], F32, ph) for i in range(NB)]
            t1 = [sb("t1%d" % i, [128, 512], F32, ph) for i in range(NB)]
            t2 = [sb("t2%d" % i, [128, 512], F32, ph) for i in range(NB)]
            s8 = [sb("s8%d" % i, [128, 8], F32, ph) for i in range(NB)]
            qo = [sb("qo%d" % i, [128, 512], BF16, ph) for i in range(NB)]
            stage = [sb("stage%d" % i, [128, 4, GT * 128], BF16, ph) for i in range(2)]
            pT = [ps("pT%d" % i, [128, 4, 128], BF16, ph) for i in range(2)]
            pM = [ps("pM%d" % i, [128, 512], F32, ph) for i in range(3)]
            pW = ps("pW", [128, 512], F32, ph)
            cnt = {"pT": 0, "pM": 0, "pp": 0, "stage": 0, "w": 0}

            blocks = []
            for cb in range(20):
                c0 = cb * 512
                if c0 < OFF_QB:
                    blocks.append(("qa", cb, c0 // 128))
                elif c0 < OFF_GATE:
                    blocks.append(("qb", cb, (c0 - OFF_QB) // 128))
                elif c0 < OFF_KA:
                    blocks.append(("gate", cb, (c0 - OFF_GATE) // 128))
                elif c0 < OFF_VA:
                    blocks.append(("ka", cb, (c0 - OFF_KA) // 128))
                elif c0 < OFF_KB:
                    blocks.append(("va", cb, (c0 - OFF_VA)))
                elif c0 < OFF_VB:
                    blocks.append(("kb", cb, (c0 - OFF_KB) // 128))
                else:
                    blocks.append(("vb", cb, (c0 - OFF_VB)))

            groups = [("lat", g) for g in range(NT // GT)] + [("ctx", 0)]
            for gkind, g in groups:
                is_ctx = gkind == "ctx"
                ntile = 2 if is_ctx else GT
                mj = 1 if is_ctx else 0
                src_d = ctx_d if is_ctx else x_d
                tok0 = 0 if is_ctx else g * GT * 128
                key0 = 0 if is_ctx else L + g * GT * 128
                if not is_ctx:
                    P.dma("sp", lambda e: e.dma_start(out=ropet[:], in_=rope_d[:, g * GT:(g + 1) * GT, :, :]), writes=["ropet"])
                for tt in range(ntile):
                    i = tt % 2
                    rcol = (NT + tt) if is_ctx else (g * GT + tt)
                    P.dma("sp", lambda e: e.dma_start(out=xt[i][:], in_=src_d[tok0 + tt * 128: tok0 + (tt + 1) * 128, :]), writes=[("xt", i)])
                    P.op("act", lambda e: e.activation(out=junk[:], in_=xt[i][:], func=AF.Square, accum_out=ssq[:, 0:1]),
                         reads=[("xt", i)], writes=["junk", "ssq0"])
                    P.op("act", lambda e: e.activation(out=ssq[:, 1:2], in_=ssq[:, 0:1], func=AF.Sqrt, scale=1.0 / D, bias=EPS),
                         reads=["ssq0"], writes=["ssq1"])
                    P.op("dve", lambda e: e.reciprocal(out=rstd_all[:, rcol:rcol + 1], in_=ssq[:, 1:2]), reads=["ssq1"], writes=[("rstd", rcol)])
                    P.op("dve", lambda e: e.tensor_tensor(out=xb[i][:], in0=xt[i][:], in1=gs1row[:, mj, :], op=ALU.mult),
                         reads=[("xt", i), "gs1row%d" % mj], writes=[("xb", i)])
                    for j4 in range(4):
                        pi = cnt["pT"] % 2
                        cnt["pT"] += 1
                        for jj in range(4):
                            k = j4 * 4 + jj
                            P.op("pe", lambda e: e.transpose(out=pT[pi][:, jj, :], in_=xb[i][:, k * 128:(k + 1) * 128], identity=ident_b[:]),
                                 reads=[("xb", i)], writes=[("pT", pi)])
                        eng = "act" if j4 % 2 == 0 else "dve"
                        if eng == "act":
                            P.op("act", lambda e: e.copy(out=xT[:, j4 * 4:(j4 + 1) * 4, tt * 128:(tt + 1) * 128], in_=pT[pi][:]),
                                 reads=[("pT", pi)], writes=[("xT", tt)])
                        else:
                            P.op("dve", lambda e: e.tensor_copy(out=xT[:, j4 * 4:(j4 + 1) * 4, tt * 128:(tt + 1) * 128], in_=pT[pi][:]),
                                 reads=[("pT", pi)], writes=[("xT", tt)])
                for kind, cb, hoff in blocks:
                    if is_ctx and kind in ("qa", "qb", "gate"):
                        continue
                    wi = cnt["w"] % 2
                    cnt["w"] += 1
                    wkeys = [("wbf_in", r_, (cb * 512) // 2048) for r_ in range(4)]
                    P.dma("sp", lambda e: e.dma_start(out=wbuf[wi][:], in_=wbf_in[:, cb * 512:(cb + 1) * 512].rearrange("(k p) c -> p k c", p=128)),
                          reads=wkeys, writes=[("wbuf", wi)])
                    for k in range(KT):
                        P.op("pe", lambda e: e.matmul(pW[:], lhsT=sh1rep[:, mj, k, :], rhs=wbuf[wi][:, k, :], start=(k == 0), stop=(k == KT - 1)),
                             reads=[("sh1rep", mj), ("wbuf", wi)], writes=["pW"])
                    P.op("act", lambda e: e.copy(out=shwb[wi][:], in_=pW[:]), reads=["pW"], writes=[("shwb", wi)])
                    need_stage = kind in ("qa", "qb", "ka", "kb", "gate")
                    if need_stage:
                        si = cnt["stage"] % 2
                        cnt["stage"] += 1
                    pending = []
                    for tt in range(ntile):
                        rcol = (NT + tt) if is_ctx else (g * GT + tt)
                        mi = cnt["pM"] % 3
                        cnt["pM"] += 1
                        for k in range(KT):
                            P.op("pe", lambda e: e.matmul(pM[mi][:], lhsT=xT[:, k, tt * 128:(tt + 1) * 128], rhs=wbuf[wi][:, k, :],
                                                          start=(k == 0), stop=(k == KT - 1)),
                                 reads=[("xT", tt), ("wbuf", wi)], writes=[("pM", mi)])
                        while len(pending) > 1:
                            pending.pop(0)()
                        bi = cnt["pp"] % NB
                        cnt["pp"] += 1
                        if kind in ("va", "vb"):
                            P.op("dve", lambda e: e.scalar_tensor_tensor(out=qo[bi][:], in0=pM[mi][:], scalar=rstd_all[:, rcol:rcol + 1],
                                                                         in1=shwb[wi][:], op0=ALU.mult, op1=ALU.add),
                                 reads=[("pM", mi), ("rstd", rcol), ("shwb", wi)], writes=[("qo", bi)])
                            dst = va_d if kind == "va" else vb_d
                            P.dma("act", lambda e: e.dma_start(out=dst[key0 + tt * 128:key0 + (tt + 1) * 128, hoff:hoff + 512], in_=qo[bi][:]),
                                  reads=[("qo", bi)], writes=[(kind, key0 + tt * 128, hoff)])
                            continue
                        P.op("dve", lambda e: e.scalar_tensor_tensor(out=pv[bi][:], in0=pM[mi][:], scalar=rstd_all[:, rcol:rcol + 1],
                                                                     in1=shwb[wi][:], op0=ALU.mult, op1=ALU.add),
                             reads=[("pM", mi), ("rstd", rcol), ("shwb", wi)], writes=[("pv", bi)])
                        if kind == "gate":
                            P.op("act", lambda e: e.activation(out=qo[bi][:], in_=pv[bi][:], func=AF.Sigmoid), reads=[("pv", bi)], writes=[("qo", bi)])
                        else:
                            npc, wdt = (8, 64) if kind in ("qa", "ka") else (4, 128)
                            goff = {"qa": 0, "ka": 64, "qb": 512, "kb": 640}[kind]
                            P.op("act", lambda e: e.activation(out=sq[bi][:], in_=pv[bi][:], func=AF.Square), reads=[("pv", bi)], writes=[("sq", bi)])
                            P.op("dve", lambda e: e.tensor_reduce(out=s8[bi][:, 0:npc], in_=sq[bi][:].rearrange("p (a b) -> p a b", b=wdt),
                                                                  axis=AX.X, op=ALU.add), reads=[("sq", bi)], writes=[("s8", bi)])
                            P.op("act", lambda e: e.activation(out=s8[bi][:, 0:npc], in_=s8[bi][:, 0:npc], func=AF.Sqrt, scale=1.0 / wdt, bias=EPS),
                                 reads=[("s8", bi)], writes=[("s8", bi)])
                            P.op("dve", lambda e: e.reciprocal(out=s8[bi][:, 0:npc], in_=s8[bi][:, 0:npc]), reads=[("s8", bi)], writes=[("s8", bi)])
                            P.op("dve", lambda e: e.tensor_tensor(out=qn[bi][:].rearrange("p (a b) -> p a b", b=wdt),
                                                                  in0=pv[bi][:].rearrange("p (a b) -> p a b", b=wdt),
                                                                  in1=s8[bi][:, 0:npc].unsqueeze(2).to_broadcast([128, npc, wdt]), op=ALU.mult),
                                 reads=[("pv", bi), ("s8", bi)], writes=[("qn", bi)])
                            rope_on = kind in ("qa", "ka") and not is_ctx
                            gdst = qn[bi] if rope_on else qo[bi]
                            P.op("dve", lambda e: e.tensor_tensor(out=gdst[:].rearrange("p (a b) -> p a b", b=wdt),
                                                                  in0=qn[bi][:].rearrange("p (a b) -> p a b", b=wdt),
                                                                  in1=gains[:, goff:goff + wdt].unsqueeze(1).to_broadcast([128, npc, wdt]), op=ALU.mult),
                                 reads=[("qn", bi), "gains"], writes=[("qn", bi) if rope_on else ("qo", bi)])
                            if rope_on:
                                q5 = qn[bi][:].rearrange("p (a r h w) -> p a r h w", a=8, r=2, h=2, w=16)
                                t5 = t2[bi][:].rearrange("p (a r h w) -> p a r h w", a=8, r=2, h=2, w=16)
                                cosb = ropet[:, tt, 0, :].unsqueeze(1).to_broadcast([128, 8, 64])
                                ss4 = ropet[:, tt, 1, :].rearrange("p (r h w) -> p r h w", r=2, h=2, w=16)
                                P.op("dve", lambda e: e.tensor_tensor(out=t1[bi][:].rearrange("p (a b) -> p a b", b=64),
                                                                      in0=qn[bi][:].rearrange("p (a b) -> p a b", b=64), in1=cosb, op=ALU.mult),
                                     reads=[("qn", bi), "ropet"], writes=[("t1", bi)])
                                for r_ in range(2):
                                    for h_ in range(2):
                                        P.op("dve", lambda e: e.tensor_tensor(out=t5[:, :, r_, h_, :], in0=q5[:, :, r_, 1 - h_, :],
                                                                              in1=ss4[:, r_, h_, :].unsqueeze(1).to_broadcast([128, 8, 16]), op=ALU.mult),
                                             reads=[("qn", bi), "ropet"], writes=[("t2", bi, r_, h_)])
                                P.op("dve", lambda e: e.tensor_tensor(out=qo[bi][:], in0=t1[bi][:], in1=t2[bi][:], op=ALU.add),
                                     reads=[("t1", bi)] + [("t2", bi, r_, h_) for r_ in range(2) for h_ in range(2)], writes=[("qo", bi)])
                        def do_tr(bi=bi, si=si, tt=tt):
                            pi = cnt["pT"] % 2
                            cnt["pT"] += 1
                            for jj in range(4):
                                P.op("pe", lambda e: e.transpose(out=pT[pi][:, jj, :], in_=qo[bi][:, jj * 128:(jj + 1) * 128], identity=ident_b[:]),
                                     reads=[("qo", bi)], writes=[("pT", pi)])
                            P.op("act", lambda e: e.copy(out=stage[si][:, :, tt * 128:(tt + 1) * 128], in_=pT[pi][:]),
                                 reads=[("pT", pi)], writes=[("stage", si)])
                        pending.append(do_tr)
                    while pending:
                        pending.pop(0)()
                    if need_stage:
                        nt_ = ntile * 128
                        if kind == "gate":
                            r0 = hoff * 128
                            P.dma("act", lambda e: e.dma_start(out=gT_d[r0:r0 + 512, tok0:tok0 + nt_].rearrange("(j p) t -> p j t", p=128),
                                                              in_=stage[si][:, :, 0:nt_]), reads=[("stage", si)], writes=[("gT", cb, g)])
                        else:
                            dst = {"qa": qaT_d, "ka": kaT_d, "qb": qbT_d, "kb": kbT_d}[kind]
                            o0 = tok0 if kind in ("qa", "qb") else key0
                            P.dma("act", lambda e: e.dma_start(out=dst[hoff:hoff + 4, :, o0:o0 + nt_].rearrange("h p t -> p h t"),
                                                              in_=stage[si][:, :, 0:nt_]), reads=[("stage", si)], writes=[(kind, cb, g, gkind)])
            P.barrier()
        ph01.close()
        if upto < 2:
            P.barrier(pool_ring=True)
            return nc, P

        NKC = NK // 128
        with ExitStack() as ph:
            qT = [sb("aqT%d" % i, [128, S], BF16, ph) for i in range(2)]
            kT = [sb("akT%d" % i, [128, NK], BF16, ph) for i in range(2)]
            vv = [sb("avv%d" % i, [128, NKC, 128], BF16, ph) for i in range(2)]
            pTb = [sb("apT%d" % i, [128, 2, 512], BF16, ph) for i in range(3)]
            accS = sb("aaccS", [128, 2, 512], F32, ph)
            rb = [sb("arb%d" % i, [128, 512], F32, ph) for i in range(2)]
            tta = sb("atta", [128, 512], F32, ph)
            yy = sb("ayy", [128, 512], F32, ph)
            ysq = sb("aysq", [128, 512], F32, ph)
            rstdb = sb("arstdb", [128, 512], F32, ph)
            yo = [sb("ayo%d" % i, [128, 512], BF16, ph) for i in range(2)]
            psS = [ps("apsS%d" % i, [128, 2, 512], F32, ph) for i in range(2)]
            psO = [ps("apsO%d" % i, [128, 512], F32, ph) for i in range(2)]
            psF = [ps("apsF%d" % i, [128, 512], F32, ph) for i in range(2)]
            nhA = lim.get("headsA", 8)

            def load_head_a(h):
                hi = h % 2
                P.dma("sp", lambda e: e.dma_start(out=qT[hi][:], in_=qaT_d[h]), writes=[("qT", hi)])
                P.dma("sp", lambda e: e.dma_start(out=kT[hi][:], in_=kaT_d[h]), writes=[("kT", hi)])
                for c0 in range(0, NKC, 22):
                    P.dma("sp", lambda e: e.dma_start(out=vv[hi][:, c0:c0 + 22, :],
                                                      in_=va_d[c0 * 128:(c0 + 22) * 128, h * 128:(h + 1) * 128].rearrange("(c p) d -> p c d", p=128)),
                          writes=[("vv", hi, c0)])

            nqbA = lim.get("qbA", 16)
            stepsA = [(h, qb, kc) for h in range(nhA) for qb in range(nqbA) for kc in range(NKC)]

            def qk_a(s):
                h, qb, kc = stepsA[s]
                hi = h % 2
                si = s % 2
                for sm in range(2):
                    P.op("pe", lambda e: e.matmul(psS[si][:, sm, :], lhsT=kT[hi][sm * 64:(sm + 1) * 64, kc * 128:(kc + 1) * 128],
                                                  rhs=qT[hi][sm * 64:(sm + 1) * 64, qb * 512:(qb + 1) * 512], start=True, stop=True),
                         reads=[("kT", hi), ("qT", hi)], writes=[("psS", si, sm)])

            load_head_a(0)
            qk_a(0)
            for s, (h, qb, kc) in enumerate(stepsA):
                    hi = h % 2
                    vkeys = [("vv", hi, c0) for c0 in range(0, NKC, 22)]
                    if qb == 1 and kc == 0 and h + 1 < nhA:
                        load_head_a(h + 1)
                    si = s % 2
                    pi = s % 3
                    if s + 1 < len(stepsA):
                        qk_a(s + 1)
                    for sm in range(2):
                        P.op("act", lambda e: e.activation(out=pTb[pi][:, sm, :], in_=psS[si][:, sm, :], func=AF.Exp),
                             reads=[("psS", si, sm)], writes=[("pTb", pi, sm)])
                    if kc == 0:
                        P.op("dve", lambda e: e.tensor_copy(out=accS[:, 0, :], in_=pTb[pi][:, 0, :]), reads=[("pTb", pi, 0)], writes=["accS"])
                    else:
                        P.op("dve", lambda e: e.tensor_tensor(out=accS[:, 0, :], in0=accS[:, 0, :], in1=pTb[pi][:, 0, :], op=ALU.add),
                             reads=[("pTb", pi, 0), "accS"], writes=["accS"])
                    if kc == 0:
                        P.op("dve", lambda e: e.tensor_copy(out=accS[:, 1, :], in_=pTb[pi][:, 1, :]), reads=[("pTb", pi, 1)], writes=["accS1"])
                    else:
                        P.op("dve", lambda e: e.tensor_tensor(out=accS[:, 1, :], in0=accS[:, 1, :], in1=pTb[pi][:, 1, :], op=ALU.add),
                             reads=[("pTb", pi, 1), "accS1"], writes=["accS1"])
                    for sm in range(2):
                        P.op("pe", lambda e: e.matmul(psO[sm][:], lhsT=vv[hi][:, kc, :], rhs=pTb[pi][:, sm, :], start=(kc == 0), stop=(kc == NKC - 1)),
                             reads=[("pTb", pi, sm)] + vkeys, writes=[("psO", sm)])
                    if kc != NKC - 1:
                        continue
                    yi = qb % 2
                    for sm in range(2):
                        P.op("pe", lambda e: e.matmul(psF[sm][:], lhsT=ones_f[:], rhs=accS[:, sm, :], start=True, stop=True),
                             reads=["accS" if sm == 0 else "accS1", "ones_f"], writes=[("psF", sm)])
                        P.op("dve", lambda e: e.reciprocal(out=rb[sm][:], in_=psF[sm][:]), reads=[("psF", sm)], writes=[("rb", sm)])
                    P.op("dve", lambda e: e.tensor_scalar(out=rb[1][:], in0=rb[1][:], scalar1=neg_lam[:, 0:1], scalar2=None, op0=ALU.mult),
                         reads=[("rb", 1), "neg_lam"], writes=[("rb", 1)])
                    P.op("dve", lambda e: e.tensor_tensor(out=tta[:], in0=psO[1][:], in1=rb[1][:], op=ALU.mult), reads=[("psO", 1), ("rb", 1)], writes=["tta"])
                    P.op("dve", lambda e: e.tensor_tensor(out=yy[:], in0=psO[0][:], in1=rb[0][:], op=ALU.mult), reads=[("psO", 0), ("rb", 0)], writes=["yy"])
                    P.op("dve", lambda e: e.tensor_tensor(out=yy[:], in0=yy[:], in1=tta[:], op=ALU.add), reads=["yy", "tta"], writes=["yy"])
                    P.op("dve", lambda e: e.tensor_tensor(out=ysq[:], in0=yy[:], in1=yy[:], op=ALU.mult), reads=["yy"], writes=["ysq"])
                    P.op("pe", lambda e: e.matmul(psF[0][:], lhsT=ones_f[:], rhs=ysq[:], start=True, stop=True), reads=["ysq", "ones_f"], writes=[("psF", 0)])
                    P.op("act", lambda e: e.activation(out=rstdb[:], in_=psF[0][:], func=AF.Ln, scale=1.0 / 128, bias=eps_col[:, 0:1]),
                         reads=[("psF", 0), "eps_col"], writes=["rstdb"])
                    P.op("act", lambda e: e.activation(out=rstdb[:], in_=rstdb[:], func=AF.Exp, scale=-0.5), reads=["rstdb"], writes=["rstdb"])
                    P.op("dve", lambda e: e.scalar_tensor_tensor(out=yo[yi][:], in0=yy[:], scalar=subln_col[:, 0:1], in1=rstdb[:], op0=ALU.mult, op1=ALU.mult),
                         reads=["yy", "rstdb", "subln_col"], writes=[("yo", yi)])
                    P.dma("sp", lambda e: e.dma_start(out=yaT_d[h][:, qb * 512:(qb + 1) * 512], in_=yo[yi][:]),
                          reads=[("yo", yi)], writes=[("yaT", h, qb)])
            P.barrier()
        if upto < 3:
            P.barrier(pool_ring=True)
            return nc, P

        with ExitStack() as ph:
            qT = [sb("bqT%d" % i, [128, S], BF16, ph) for i in range(2)]
            kT = [sb("bkT%d" % i, [128, NK], BF16, ph) for i in range(2)]
            vE = [sb("bvE%d" % i, [128, NKC, 129], BF16, ph) for i in range(2)]
            vO = [sb("bvO%d" % i, [128, NKC - 1, 129], BF16, ph) for i in range(2)]
            rpbt = sb("brpbt", [128, 8, 4, 64], F32, ph)
            nam = sb("bnam", [128, 4, 64], F32, ph)
            expB = [sb("bexpB%d" % i, [128, 8, 4, 64], BF16, ph) for i in range(2)]
            Pb = [sb("bPb%d" % i, [128, 6, 64], BF16, ph) for i in range(3)]
            rec = [sb("brec%d" % i, [128, 1], F32, ph) for i in range(2)]
            yb = [sb("byb%d" % i, [128, 128], BF16, ph) for i in range(2)]
            ybst = [sb("bybst%d" % i, [128, 2048], BF16, ph) for i in range(2)]
            psS = [ps("bpsS%d" % i, [128, 6, 64], F32, ph) for i in range(3)]
            accO = [ps("baccO%d" % i, [128, 129], F32, ph) for i in range(2)]
            pTr = ps("bpTr", [128, 128], BF16, ph)
            P.dma("sp", lambda e: e.dma_start(out=nam[:], in_=namask_d[:, :, :]), writes=["nam"])
            for i in range(2):
                P.op("dve", lambda e: e.memset(vE[i][:, :, 128:129], 1.0), writes=[("vE1", i)])
                P.op("dve", lambda e: e.memset(vO[i][:, :, 128:129], 1.0), writes=[("vO1", i)])
            step = 0
            nhB = lim.get("headsB", 8)

            def load_head_b(h):
                hi = h % 2
                P.dma("sp", lambda e: e.dma_start(out=qT[hi][:], in_=qbT_d[h]), writes=[("qT", hi)])
                P.dma("sp", lambda e: e.dma_start(out=kT[hi][:], in_=kbT_d[h]), writes=[("kT", hi)])
                for c0 in range(0, NKC, 22):
                    P.dma("sp", lambda e: e.dma_start(out=vE[hi][:, c0:c0 + 22, 0:128],
                                                      in_=vb_d[c0 * 128:(c0 + 22) * 128, h * 128:(h + 1) * 128].rearrange("(c p) d -> p c d", p=128)),
                          writes=[("vE", hi, c0)])
                for c0, n_ in ((0, 22), (22, 22), (44, 21)):
                    P.dma("sp", lambda e: e.dma_start(out=vO[hi][:, c0:c0 + n_, 0:128],
                                                      in_=vb_d[64 + c0 * 128:64 + (c0 + n_) * 128, h * 128:(h + 1) * 128].rearrange("(c p) d -> p c d", p=128)),
                          writes=[("vO", hi, c0)])

            npB = lim.get("pairsB", 64)
            stepsB = [(h, j, r2) for h in range(nhB) for j in range(npB) for r2 in range(2)]

            def rowinfo(j, r2):
                r = 2 * j + r2
                r_start = min(max(r - 4, 0), 120)
                return r, r_start, r - r_start, L + r_start * 64

            def qk_b(s):
                h, j, r2 = stepsB[s]
                hi = h % 2
                si = s % 3
                r, r_start, v, key0 = rowinfo(j, r2)
                for c in range(6):
                    ko = c * 128 if c < 2 else key0 + (c - 2) * 128
                    P.op("pe", lambda e: e.matmul(psS[si][:, c, :], lhsT=kT[hi][:, ko:ko + 128], rhs=qT[hi][:, r * 64:(r + 1) * 64],
                                                  start=True, stop=True, skip_group_check=True),
                         reads=[("kT", hi), ("qT", hi)], writes=[("psS", si)])

            def load_bias(h):
                hi = h % 2
                P.dma("sp", lambda e: e.dma_start(out=rpbt[:], in_=rpbx_d[h]), writes=["rpbt"])
                P.op("act", lambda e: e.activation(out=rpbt[:], in_=rpbt[:], func=AF.Exp), reads=["rpbt"], writes=["rpbt"])
                P.op("dve", lambda e: e.tensor_tensor(out=expB[hi][:], in0=rpbt[:], in1=nam[:].unsqueeze(1).to_broadcast([128, 8, 4, 64]), op=ALU.mult),
                     reads=["rpbt", "nam"], writes=[("expB", hi)])

            pendB = []
            load_head_b(0)
            load_bias(0)
            qk_b(0)
            for s, (h, j, r2) in enumerate(stepsB):
                hi = h % 2
                si = s % 3
                ai = j % 2
                vEk = [("vE", hi, c0) for c0 in range(0, NKC, 22)] + [("vE1", hi)]
                vOk = [("vO", hi, c0) for c0 in (0, 22, 44)] + [("vO1", hi)]
                if j == 8 and r2 == 0 and h + 1 < nhB:
                    load_head_b(h + 1)
                    load_bias(h + 1)
                r, r_start, v, key0 = rowinfo(j, r2)
                if s + 1 < len(stepsB):
                    qk_b(s + 1)
                P.op("act", lambda e: e.activation(out=Pb[si][:], in_=psS[si][:], func=AF.Exp), reads=[("psS", si)], writes=[("Pb", si)])
                P.op("dve", lambda e: e.tensor_tensor(out=Pb[si][:, 2:6, :], in0=Pb[si][:, 2:6, :], in1=expB[hi][:, v, :, :], op=ALU.mult),
                     reads=[("Pb", si), ("expB", hi)], writes=[("Pb", si)])
                for c in range(6):
                    if c < 2:
                        rhs = vE[hi][:, c, :]
                    elif r_start % 2 == 0:
                        rhs = vE[hi][:, 2 + r_start // 2 + (c - 2), :]
                    else:
                        rhs = vO[hi][:, (192 + r_start * 64) // 128 + (c - 2), :]
                    P.op("pe", lambda e: e.matmul(accO[ai][r2 * 64:(r2 + 1) * 64, :], lhsT=Pb[si][:, c, :], rhs=rhs, start=(c == 0), stop=(c == 5),
                                                  skip_group_check=True),
                         reads=[("Pb", si)] + vEk + vOk, writes=[("accO", ai)])
                while pendB:
                    pendB.pop(0)()
                if r2 == 0:
                    continue
                P.op("dve", lambda e: e.reciprocal(out=rec[ai][:], in_=accO[ai][:, 128:129]), reads=[("accO", ai)], writes=[("rec", ai)])
                P.op("dve", lambda e: e.tensor_scalar(out=yb[ai][:], in0=accO[ai][:, 0:128], scalar1=rec[ai][:, 0:1], scalar2=None, op0=ALU.mult),
                     reads=[("accO", ai), ("rec", ai)], writes=[("yb", ai)])

                def fin_b(ai=ai, j=j, h=h):
                    P.op("pe", lambda e: e.transpose(out=pTr[:], in_=yb[ai][:], identity=ident_b[:]), reads=[("yb", ai)], writes=["pTr"])
                    yi = (j // 16) % 2
                    P.op("act", lambda e: e.copy(out=ybst[yi][:, (j % 16) * 128:(j % 16 + 1) * 128], in_=pTr[:]), reads=["pTr"], writes=[("ybst", yi)])
                    if j % 16 == 15:
                        P.dma("sp", lambda e: e.dma_start(out=ybT_d[h][:, (j // 16) * 2048:(j // 16 + 1) * 2048], in_=ybst[yi][:]),
                              reads=[("ybst", yi)], writes=[("ybT", h, j // 16)])
                pendB.append(fin_b)
            while pendB:
                pendB.pop(0)()
            P.barrier()
        if upto < 4:
            P.barrier(pool_ring=True)
            return nc, P

        with ExitStack() as ph:
            wa = sb("cwa", [128, 8, D], BF16, ph)
            wb = sb("cwb", [128, 8, D], BF16, ph)
            P.dma("sp", lambda e: e.dma_start(out=wa[:], in_=wbf_a.rearrange("(k p) c -> p k c", p=128)),
                  reads=[("wbf_a", r_, 0) for r_ in range(2)], writes=["wa"])
            P.dma("sp", lambda e: e.dma_start(out=wb[:], in_=wbf_b.rearrange("(k p) c -> p k c", p=128)),
                  reads=[("wbf_b", r_, 0) for r_ in range(2)], writes=["wb"])
            yaTg = [sb("cya%d" % i, [128, 8, 512], BF16, ph) for i in range(2)]
            ybTg = [sb("cyb%d" % i, [128, 8, 512], BF16, ph) for i in range(2)]
            gt = [sb("cgt%d" % i, [128, 2, 512], BF16, ph) for i in range(4)]
            t1 = [sb("ct1%d" % i, [128, 512], F32, ph) for i in range(2)]
            t2 = [sb("ct2%d" % i, [128, 512], F32, ph) for i in range(2)]
            mst = [sb("cmst%d" % i, [128, 512], BF16, ph) for i in range(2)]
            psA = [ps("cpsA%d" % i, [128, 512], F32, ph) for i in range(2)]
            psB = [ps("cpsB%d" % i, [128, 512], F32, ph) for i in range(2)]
            n3 = 0
            for tg in range(lim.get("tg3a", S // 512)):
                gi = tg % 2
                tsl = slice(tg * 512, (tg + 1) * 512)
                P.dma("sp", lambda e: e.dma_start(out=yaTg[gi][:], in_=yaT_d[:, :, tsl].rearrange("h p t -> p h t")), writes=[("yaTg", gi)])
                P.dma("sp", lambda e: e.dma_start(out=ybTg[gi][:], in_=ybT_d[:, :, tsl].rearrange("h p t -> p h t")), writes=[("ybTg", gi)])
                for dc in range(KT):
                    pi = n3 % 2
                    g4 = n3 % 4
                    n3 += 1
                    P.dma("sp", lambda e: e.dma_start(out=gt[g4][:, 0, :], in_=gT_d[dc * 128:(dc + 1) * 128, tsl]), writes=[("gt", g4, 0)])
                    P.dma("sp", lambda e: e.dma_start(out=gt[g4][:, 1, :], in_=gT_d[D + dc * 128:D + (dc + 1) * 128, tsl]), writes=[("gt", g4, 1)])
                    for k in range(8):
                        P.op("pe", lambda e: e.matmul(psA[pi][:], lhsT=wa[:, k, dc * 128:(dc + 1) * 128], rhs=yaTg[gi][:, k, :], start=(k == 0), stop=(k == 7)),
                             reads=["wa", ("yaTg", gi)], writes=[("psA", pi)])
                    for k in range(8):
                        P.op("pe", lambda e: e.matmul(psB[pi][:], lhsT=wb[:, k, dc * 128:(dc + 1) * 128], rhs=ybTg[gi][:, k, :], start=(k == 0), stop=(k == 7)),
                             reads=["wb", ("ybTg", gi)], writes=[("psB", pi)])
                    P.op("dve", lambda e: e.tensor_tensor(out=t1[pi][:], in0=psA[pi][:], in1=gt[g4][:, 0, :], op=ALU.mult),
                         reads=[("psA", pi), ("gt", g4, 0)], writes=[("t1", pi)])
                    P.op("dve", lambda e: e.tensor_tensor(out=t2[pi][:], in0=psB[pi][:], in1=gt[g4][:, 1, :], op=ALU.mult),
                         reads=[("psB", pi), ("gt", g4, 1)], writes=[("t2", pi)])
                    P.op("dve", lambda e: e.tensor_tensor(out=mst[pi][:], in0=t1[pi][:], in1=t2[pi][:], op=ALU.add),
                         reads=[("t1", pi), ("t2", pi)], writes=[("mst", pi)])
                    P.dma("act", lambda e: e.dma_start(out=mT_d[dc][:, tsl], in_=mst[pi][:]), reads=[("mst", pi)], writes=[("mT", dc, tg)])
            P.barrier()

        with ExitStack() as ph:
            wo = sb("dwo", [128, KT, D], BF16, ph)
            P.dma("sp", lambda e: e.dma_start(out=wo[:], in_=wbf_o.rearrange("(k p) c -> p k c", p=128)),
                  reads=[("wbf_o", r_, 0) for r_ in range(4)], writes=["wo"])
            wr = sb("dwr", [128, KT, NE], F32, ph)
            P.dma("sp", lambda e: e.dma_start(out=wr[:], in_=wr_d.rearrange("(k p) e -> p k e", p=128)), writes=["wr"])
            rows = sb("drows", [128, 3, D], F32, ph)
            for j in range(3):
                P.dma("sp", lambda e: e.dma_start(out=rows[:, j, :], in_=modsave_d[j]), writes=[("rows", j)])
            mTt = [sb("dmT%d" % i, [128, KT, 128], BF16, ph) for i in range(2)]
            xt = [sb("dxt%d" % i, [128, D], F32, ph) for i in range(2)]
            xn = [sb("dxn%d" % i, [128, D], F32, ph) for i in range(2)]
            h2 = sb("dh2", [128, D], F32, ph)
            h2b = [sb("dh2b%d" % i, [128, D], BF16, ph) for i in range(2)]
            h2T = sb("dh2T", [128, KT, 128], F32, ph)
            junk = sb("djunk", [128, D], BF16, ph)
            sm = [sb("dsm%d" % i, [128, 8], F32, ph) for i in range(2)]
            ex = [sb("dex%d" % i, [128, NE], F32, ph) for i in range(2)]
            pmix = [ps("dpmix%d" % i, [128, 512], F32, ph) for i in range(2)]
            pTf = [ps("dpTf%d" % i, [128, 4, 128], F32, ph) for i in range(2)]
            pR = ps("dpR", [128, NE], F32, ph)
            n3 = 0
            ntf = 0
            for t in range(lim.get("t3b", NT)):
                i = t % 2
                rsl = slice(t * 128, (t + 1) * 128)
                P.dma("sp", lambda e: e.dma_start(out=mTt[i][:], in_=mT_d[:, :, rsl].rearrange("k p t -> p k t")), writes=[("mTt", i)])
                P.dma("sp", lambda e: e.dma_start(out=xt[i][:], in_=x_d[rsl, :]), writes=[("xt", i)])
                for cb in range(4):
                    pi = n3 % 2
                    n3 += 1
                    csl = slice(cb * 512, (cb + 1) * 512)
                    for k in range(KT):
                        P.op("pe", lambda e: e.matmul(pmix[pi][:], lhsT=mTt[i][:, k, :], rhs=wo[:, k, csl], start=(k == 0), stop=(k == KT - 1)),
                             reads=[("mTt", i), "wo"], writes=[("pmix", pi)])
                    P.op("dve", lambda e: e.tensor_tensor(out=xn[i][:, csl], in0=pmix[pi][:], in1=rows[:, 0, csl], op=ALU.mult),
                         reads=[("pmix", pi), ("rows", 0)], writes=[("xn", i, cb)])
                    P.op("dve", lambda e: e.tensor_tensor(out=xn[i][:, csl], in0=xn[i][:, csl], in1=xt[i][:, csl], op=ALU.add),
                         reads=[("xn", i, cb), ("xt", i)], writes=[("xn", i, cb)])
                xnk = [("xn", i, cb) for cb in range(4)]
                P.dma("act", lambda e: e.dma_start(out=out_d[rsl, :], in_=xn[i][:]), reads=xnk, writes=[("out", t)])
                P.op("act", lambda e: e.activation(out=junk[:], in_=xn[i][:], func=AF.Square, accum_out=sm[i][:, 0:1]), reads=xnk, writes=["junk", ("sm", i, 0)])
                P.op("act", lambda e: e.activation(out=sm[i][:, 1:2], in_=sm[i][:, 0:1], func=AF.Sqrt, scale=1.0 / D, bias=EPS),
                     reads=[("sm", i, 0)], writes=[("sm", i, 1)])
                P.op("dve", lambda e: e.reciprocal(out=sm[i][:, 1:2], in_=sm[i][:, 1:2]), reads=[("sm", i, 1)], writes=[("sm", i, 1)])
                P.op("dve", lambda e: e.scalar_tensor_tensor(out=h2[:], in0=xn[i][:], scalar=sm[i][:, 1:2], in1=rows[:, 2, :], op0=ALU.mult, op1=ALU.mult),
                     reads=xnk + [("sm", i, 1), ("rows", 2)], writes=["h2"])
                P.op("dve", lambda e: e.tensor_tensor(out=h2[:], in0=h2[:], in1=rows[:, 1, :], op=ALU.add), reads=["h2", ("rows", 1)], writes=["h2"])
                P.op("act", lambda e: e.copy(out=h2b[i][:], in_=h2[:]), reads=["h2"], writes=[("h2b", i)])
                P.dma("act", lambda e: e.dma_start(out=h2_d[rsl, :], in_=h2b[i][:]), reads=[("h2b", i)], writes=[("h2d", t)])
                for j4 in range(4):
                    ti = ntf % 2
                    ntf += 1
                    for jj in range(4):
                        k = j4 * 4 + jj
                        P.op("pe", lambda e: e.transpose(out=pTf[ti][:, jj, :], in_=h2[:, k * 128:(k + 1) * 128], identity=ident_f[:]),
                             reads=["h2"], writes=[("pTf", ti)])
                    if j4 % 2 == 0:
                        P.op("act", lambda e: e.copy(out=h2T[:, j4 * 4:(j4 + 1) * 4, :], in_=pTf[ti][:]), reads=[("pTf", ti)], writes=[("h2T", j4)])
                    else:
                        P.op("dve", lambda e: e.tensor_copy(out=h2T[:, j4 * 4:(j4 + 1) * 4, :], in_=pTf[ti][:]), reads=[("pTf", ti)], writes=[("h2T", j4)])
                for k in range(KT):
                    P.op("pe", lambda e: e.matmul(pR[:], lhsT=h2T[:, k, :], rhs=wr[:, k, :], start=(k == 0), stop=(k == KT - 1)),
                         reads=[("h2T", k // 4), "wr"], writes=["pR"])
                P.op("dve", lambda e: e.tensor_reduce(out=sm[i][:, 2:3], in_=pR[:], axis=AX.X, op=ALU.max), reads=["pR"], writes=[("sm", i, 2)])
                P.op("dve", lambda e: e.tensor_scalar(out=sm[i][:, 3:4], in0=sm[i][:, 2:3], scalar1=-1.0, scalar2=None, op0=ALU.mult),
                     reads=[("sm", i, 2)], writes=[("sm", i, 3)])
                P.op("act", lambda e: e.activation(out=ex[i][:], in_=pR[:], func=AF.Exp, bias=sm[i][:, 3:4], accum_out=sm[i][:, 4:5]),
                     reads=["pR", ("sm", i, 3)], writes=[("ex", i), ("sm", i, 4)])
                P.op("dve", lambda e: e.reciprocal(out=sm[i][:, 5:6], in_=sm[i][:, 4:5]), reads=[("sm", i, 4)], writes=[("sm", i, 5)])
                P.op("dve", lambda e: e.tensor_scalar(out=aff_all[:, t, :], in0=ex[i][:], scalar1=sm[i][:, 5:6], scalar2=None, op0=ALU.mult),
                     reads=[("ex", i), ("sm", i, 5)], writes=[("aff", t)])
            if debug:
                P.dma("sp", lambda e: e.dma_start(out=dbg_d[:, 2048:2048 + NT * NE], in_=aff_all[:].rearrange("p j e -> p (j e)")),
                      reads=[("aff", t_) for t_ in range(lim.get("t3b", NT))], writes=["dbg3"])
            P.barrier()
        if upto < 5:
            P.barrier(pool_ring=True)
            return nc, P

        with ExitStack() as ph:
            meta = sb("emeta", [128, NE, 8, 4], F32, ph)
            idx32 = sb("eidx32", [128, NE, 8], I32, ph)
            idx4 = sb("eidx4", [128, NE, 8, 4], I32, ph)
            with ExitStack() as ph4:
                lo = sb("elo", [128, NE], F32, ph4)
                mid = sb("emid", [128, NE], F32, ph4)
                cmpb = sb("ecmp", [128, NT, NE], BF16, ph4)
                cntp = sb("ecntp", [128, NE], F32, ph4)

                gsel = sb("egsel", [128, NE], F32, ph4)
                pC = ps("epC", [128, NE], F32, ph4)
                P.op("dve", lambda e: e.memset(lo[:], 0.0), writes=["lo"])
                for it in range(30):
                    ci = 2.0 ** -(it + 1)
                    P.op("dve", lambda e: e.tensor_scalar(out=mid[:], in0=lo[:], scalar1=ci, scalar2=None, op0=ALU.add), reads=["lo"], writes=["mid"])
                    P.op("dve", lambda e: e.tensor_tensor(out=cmpb[:], in0=aff_all[:], in1=mid[:].unsqueeze(1).to_broadcast([128, NT, NE]), op=ALU.is_ge),
                         reads=["mid"], writes=["cmpb"])
                    P.op("dve", lambda e: e.tensor_reduce(out=cntp[:], in_=cmpb[:].rearrange("p j e -> p e j"), axis=AX.X, op=ALU.add),
                         reads=["cmpb"], writes=["cntp"])
                    P.op("pe", lambda e: e.matmul(pC[:], lhsT=ones_f[:], rhs=cntp[:], start=True, stop=True), reads=["cntp", "ones_f"], writes=["pC"])
                    P.op("dve", lambda e: e.tensor_scalar(out=gsel[:], in0=pC[:], scalar1=CAP - 0.5, scalar2=ci, op0=ALU.is_ge, op1=ALU.mult),
                         reads=["pC"], writes=["gsel"])
                    P.op("dve", lambda e: e.tensor_tensor(out=lo[:], in0=lo[:], in1=gsel[:], op=ALU.add), reads=["lo", "gsel"], writes=["lo"])
                msk = sb("emsk", [128, NT, NE], F32, ph4)
                mskb = sb("emskb", [128, NT, NE], BF16, ph4)
                U = sb("eU", [128, 128], BF16, ph4)
                tot = sb("etot", [128, NE, NT], F32, ph4)
                incl = sb("eincl", [128, NE, NT], F32, ph4)
                ones64 = sb("eones64", [128, NT], F32, ph4)
                pos = sb("epos", [128, NT, NE], F32, ph4)
                posm = sb("eposm", [128, NT, NE], F32, ph4)
                vals = sb("evals", [128, NT, NE, 5], BF16, ph4)
                rres = sb("erres", [128, NT, NE], F32, ph4)
                iota_j = sb("eiotaj", [128, NT], F32, ph4)
                iota_s = sb("eiotas", [128, CAP], F32, ph4)
                oh = [sb("eoh%d" % i, [128, CAP], BF16, ph4) for i in range(3)]
                pP = [ps("epP%d" % i, [128, 512], F32, ph4) for i in range(2)]
                pTt = [ps("epTt%d" % i, [128, 512], F32, ph4) for i in range(2)]
                pM = [ps("epM%d" % i, [128, 8, 8, 8], F32, ph4) for i in range(2)]
                P.op("pool", lambda e: e.iota(iota_j[:], pattern=[[1, NT]], base=0, channel_multiplier=0, allow_small_or_imprecise_dtypes=True),
                     writes=["iota_j"])
                P.op("pool", lambda e: e.iota(iota_s[:], pattern=[[1, CAP]], base=0, channel_multiplier=0, allow_small_or_imprecise_dtypes=True),
                     writes=["iota_s"])
                P.op("dve", lambda e: e.tensor_tensor(out=msk[:], in0=aff_all[:], in1=lo[:].unsqueeze(1).to_broadcast([128, NT, NE]), op=ALU.is_ge),
                     reads=["lo"], writes=["msk"])
                P.op("dve", lambda e: e.tensor_copy(out=mskb[:], in_=msk[:]), reads=["msk"], writes=["mskb"])
                P.op("dve", lambda e: e.tensor_scalar(out=U[:], in0=iota_row[:], scalar1=iota_p[:, 0:1], scalar2=None, op0=ALU.is_gt), writes=["U"])
                P.op("dve", lambda e: e.memset(ones64[:], 1.0), writes=["ones64"])
                mflat = mskb[:].rearrange("p j e -> p (j e)")
                for hf in range(2):
                    P.op("pe", lambda e: e.matmul(pP[hf][:], lhsT=U[:], rhs=mflat[:, hf * 512:(hf + 1) * 512], start=True, stop=True),
                         reads=["U", "mskb"], writes=[("pP", hf)])
                    P.op("pe", lambda e: e.matmul(pTt[hf][:], lhsT=ones_b[:], rhs=mflat[:, hf * 512:(hf + 1) * 512], start=True, stop=True),
                         reads=["mskb"], writes=[("pTt", hf)])
                    P.op("dve", lambda e: e.tensor_copy(out=tot[:, :, hf * 32:(hf + 1) * 32], in_=pTt[hf][:].rearrange("p (j e) -> p e j", e=NE)),
                         reads=[("pTt", hf)], writes=[("tot", hf)])
                for e_ in range(NE):
                    P.op("dve", lambda e: e.tensor_tensor_scan(out=incl[:, e_, :], data0=ones64[:], data1=tot[:, e_, :], initial=0.0, op0=ALU.mult, op1=ALU.add),
                         reads=[("tot", 0), ("tot", 1), "ones64"], writes=[("incl", e_)])
                inck = [("incl", e_) for e_ in range(NE)]
                P.op("dve", lambda e: e.tensor_tensor(out=incl[:], in0=incl[:], in1=tot[:], op=ALU.subtract), reads=inck + [("tot", 0), ("tot", 1)], writes=inck)
                for hf in range(2):
                    P.op("dve", lambda e: e.tensor_tensor(out=pos[:, hf * 32:(hf + 1) * 32, :], in0=pP[hf][:].rearrange("p (j e) -> p j e", e=NE),
                                                          in1=incl[:, :, hf * 32:(hf + 1) * 32].rearrange("p e j -> p j e"), op=ALU.add),
                         reads=[("pP", hf)] + inck, writes=[("pos", hf)])
                P.op("dve", lambda e: e.scalar_tensor_tensor(out=posm[:], in0=pos[:], scalar=1.0, in1=msk[:], op0=ALU.add, op1=ALU.mult),
                     reads=[("pos", 0), ("pos", 1), "msk"], writes=["posm"])
                P.op("dve", lambda e: e.tensor_scalar(out=posm[:], in0=posm[:], scalar1=-1.0, scalar2=None, op0=ALU.add), reads=["posm"], writes=["posm"])
                P.op("dve", lambda e: e.tensor_copy(out=vals[:, :, :, 0], in_=aff_all[:]), writes=["v0"])
                P.op("dve", lambda e: e.tensor_tensor(out=rres[:], in0=aff_all[:], in1=vals[:, :, :, 0], op=ALU.subtract), reads=["v0"], writes=["rres"])
                P.op("dve", lambda e: e.tensor_copy(out=vals[:, :, :, 1], in_=rres[:]), reads=["rres"], writes=["v1"])
                P.op("dve", lambda e: e.tensor_tensor(out=rres[:], in0=rres[:], in1=vals[:, :, :, 1], op=ALU.subtract), reads=["rres", "v1"], writes=["rres"])
                P.op("dve", lambda e: e.tensor_copy(out=vals[:, :, :, 2], in_=rres[:]), reads=["rres"], writes=["v2"])
                P.op("dve", lambda e: e.tensor_copy(out=vals[:, :, :, 3], in_=iota_p[:, 0:1].unsqueeze(2).to_broadcast([128, NT, NE])), writes=["v3"])
                P.op("dve", lambda e: e.tensor_copy(out=vals[:, :, :, 4], in_=iota_j[:].unsqueeze(2).to_broadcast([128, NT, NE])),
                     reads=["iota_j"], writes=["v4"])
                if debug:
                    P.dma("sp", lambda e: e.dma_start(out=dbg_d[:, 3072:3072 + NT * NE], in_=posm[:].rearrange("p j e -> p (j e)")),
                          reads=["posm"], writes=["dbg4"])
                    P.dma("sp", lambda e: e.dma_start(out=dbg_d[:, 1100:1100 + NE], in_=lo[:]), reads=["lo"], writes=["dbg5"])
                noh = 0
                for e_ in range(NE):
                    for j in range(NT):
                        oi = noh % 3
                        noh += 1
                        P.op("dve", lambda e: e.tensor_scalar(out=oh[oi][:], in0=iota_s[:], scalar1=posm[:, j, e_:e_ + 1], scalar2=None, op0=ALU.is_equal),
                             reads=["iota_s", "posm"], writes=[("oh", oi)])
                        for st in range(8):
                            P.op("pe", lambda e: e.matmul(pM[e_ // 8][:, e_ % 8, st, 0:5], lhsT=oh[oi][:, st * 128:(st + 1) * 128], rhs=vals[:, j, e_, :],
                                                          start=(e_ % 8 == 0 and j == 0 and st == 0), stop=(j == NT - 1), skip_group_check=True),
                                 reads=[("oh", oi), "v0", "v1", "v2", "v3", "v4"], writes=[("pM", e_ // 8)])
                for hf in range(2):
                    esl = slice(hf * 8, hf * 8 + 8)
                    P.op("dve", lambda e: e.tensor_tensor(out=meta[:, esl, :, 0], in0=pM[hf][:, :, :, 0], in1=pM[hf][:, :, :, 1], op=ALU.add) if False else
                         e.tensor_copy(out=meta[:, esl, :, 0:3], in_=pM[hf][:, :, :, 2:5]), reads=[("pM", hf)], writes=[("metaA", hf)])
                    P.op("dve", lambda e: e.tensor_tensor(out=meta[:, esl, :, 0], in0=meta[:, esl, :, 0], in1=pM[hf][:, :, :, 1], op=ALU.add),
                         reads=[("pM", hf), ("metaA", hf)], writes=[("metaA", hf)])
                    P.op("dve", lambda e: e.tensor_tensor(out=meta[:, esl, :, 0], in0=meta[:, esl, :, 0], in1=pM[hf][:, :, :, 0], op=ALU.add),
                         reads=[("pM", hf), ("metaA", hf)], writes=[("metaA", hf)])
                P.op("dve", lambda e: e.tensor_copy(out=meta[:, :, :, 3:4], in_=meta[:, :, :, 3:4]), reads=[("metaA", 0), ("metaA", 1)], writes=["meta"])
                tokf = sb("etokf", [128, NE, 8], F32, ph4)
                tok4 = sb("etok4", [128, NE, 8, 4], F32, ph4)
                P.op("dve", lambda e: e.scalar_tensor_tensor(out=tokf[:], in0=meta[:, :, :, 2], scalar=128.0, in1=meta[:, :, :, 1], op0=ALU.mult, op1=ALU.add),
                     reads=["meta"], writes=["tokf"])
                P.op("dve", lambda e: e.tensor_copy(out=idx32[:], in_=tokf[:]), reads=["tokf"], writes=["idx32"])
                for db in range(4):
                    P.op("dve", lambda e: e.tensor_scalar(out=tok4[:, :, :, db], in0=tokf[:], scalar1=4.0, scalar2=float(db), op0=ALU.mult, op1=ALU.add),
                         reads=["tokf"], writes=[("tok4", db)])
                P.op("dve", lambda e: e.tensor_copy(out=idx4[:], in_=tok4[:]), reads=[("tok4", db) for db in range(4)], writes=["idx4"])
                if debug:
                    P.dma("sp", lambda e: e.dma_start(out=dbg_d[:, 1200:1200 + NE * 8 * 4], in_=meta[:].rearrange("p a b c -> p (a b c)")),
                          reads=["meta"], writes=["dbg6"])
                P.barrier()
            with ExitStack() as ph5:
                ga2row = sb("fga2", [128, D], F32, ph5)
                P.dma("sp", lambda e: e.dma_start(out=ga2row[:], in_=modsave_d[3]), writes=["ga2row"])
                xeT = sb("fxeT", [128, KT, CAP], BF16, ph5)
                actT = sb("factT", [128, KT, CAP], BF16, ph5)
                wring = [sb("fw%d" % i, [128, KT, 512], BF16, ph5) for i in range(4)]
                xg = [sb("fxg%d" % i, [128, D], BF16, ph5) for i in range(2)]
                sa = [sb("fsa%d" % i, [128, 512], F32, ph5) for i in range(2)]
                ysc = [sb("fysc%d" % i, [128, 512], F32, ph5) for i in range(4)]
                psA = [ps("fpsA%d" % i, [128, 512], F32, ph5) for i in range(2)]
                psU = [ps("fpsU%d" % i, [128, 512], F32, ph5) for i in range(2)]
                psY = [ps("fpsY%d" % i, [128, 512], F32, ph5) for i in range(2)]
                pTx = [ps("fpTx%d" % i, [128, 4, 128], BF16, ph5) for i in range(2)]
                out4 = out_d.rearrange("t (q c) -> (t q) c", c=512)
                c5 = {"w": 0, "xg": 0, "tx": 0, "au": 0, "y": 0, "ysc": 0}
                nexp = lim.get("experts", NE)

                def load_w(srcw, keyname, e_, blk):
                    wi = c5["w"] % 4
                    c5["w"] += 1
                    P.dma("sp", lambda e: e.dma_start(out=wring[wi][:], in_=srcw[e_][:, blk * 512:(blk + 1) * 512].rearrange("(k p) c -> p k c", p=128)),
                          reads=[((keyname, e_), r_, 0) for r_ in range(4)], writes=[("wring", wi)])
                    return wi

                def gather(e_):
                    for st in range(8):
                        gi = c5["xg"] % 2
                        c5["xg"] += 1
                        P.dma("pool", lambda e: e.indirect_dma_start(out=xg[gi][:], out_offset=None, in_=h2_d[:, :],
                                                                     in_offset=bass.IndirectOffsetOnAxis(ap=idx32[:, e_, st:st + 1], axis=0)),
                              reads=["idx32"], writes=[("xg", gi)])
                        for j4 in range(4):
                            ti = c5["tx"] % 2
                            c5["tx"] += 1
                            for jj in range(4):
                                k = j4 * 4 + jj
                                P.op("pe", lambda e: e.transpose(out=pTx[ti][:, jj, :], in_=xg[gi][:, k * 128:(k + 1) * 128], identity=ident_b[:]),
                                     reads=[("xg", gi)], writes=[("pTx", ti)])
                            if j4 % 2 == 0:
                                P.op("act", lambda e: e.copy(out=xeT[:, j4 * 4:(j4 + 1) * 4, st * 128:(st + 1) * 128], in_=pTx[ti][:]),
                                     reads=[("pTx", ti)], writes=[("xeT", st)])
                            else:
                                P.op("dve", lambda e: e.tensor_copy(out=xeT[:, j4 * 4:(j4 + 1) * 4, st * 128:(st + 1) * 128], in_=pTx[ti][:]),
                                     reads=[("pTx", ti)], writes=[("xeT", st)])

                prev_sc = []
                gather(0)
                for e_ in range(nexp):
                    for fb in range(4):
                        wg = load_w(wbf_eg, "wbf_eg", e_, fb)
                        wu = load_w(wbf_eu, "wbf_eu", e_, fb)
                        for fc in range(4):
                            for sh in range(2):
                                ai = c5["au"] % 2
                                c5["au"] += 1
                                xk = [("xeT", s_) for s_ in range(sh * 4, sh * 4 + 4)]
                                for k in range(KT):
                                    P.op("pe", lambda e: e.matmul(psA[ai][:], lhsT=wring[wg][:, k, fc * 128:(fc + 1) * 128], rhs=xeT[:, k, sh * 512:(sh + 1) * 512],
                                                                  start=(k == 0), stop=(k == KT - 1)), reads=[("wring", wg)] + xk, writes=[("psA", ai)])
                                for k in range(KT):
                                    P.op("pe", lambda e: e.matmul(psU[ai][:], lhsT=wring[wu][:, k, fc * 128:(fc + 1) * 128], rhs=xeT[:, k, sh * 512:(sh + 1) * 512],
                                                                  start=(k == 0), stop=(k == KT - 1)), reads=[("wring", wu)] + xk, writes=[("psU", ai)])
                                P.op("act", lambda e: e.activation(out=sa[ai][:], in_=psA[ai][:], func=AF.Silu), reads=[("psA", ai)], writes=[("sa", ai)])
                                P.op("dve", lambda e: e.tensor_tensor(out=actT[:, fb * 4 + fc, sh * 512:(sh + 1) * 512], in0=psU[ai][:], in1=sa[ai][:], op=ALU.mult),
                                     reads=[("psU", ai), ("sa", ai)], writes=[("actT", fb * 4 + fc, sh)])
                    if e_ + 1 < nexp:
                        gather(e_ + 1)
                    cur_sc = []
                    ak = [("actT", f_, s_) for f_ in range(KT) for s_ in range(2)]
                    for db in range(4):
                        wd = load_w(wbf_ed, "wbf_ed", e_, db)
                        for st in range(8):
                            yi = c5["y"] % 2
                            c5["y"] += 1
                            for k in range(KT):
                                P.op("pe", lambda e: e.matmul(psY[yi][:], lhsT=actT[:, k, st * 128:(st + 1) * 128], rhs=wring[wd][:, k, :],
                                                              start=(k == 0), stop=(k == KT - 1)),
                                     reads=[("wring", wd)] + [("actT", f_, st // 4) for f_ in range(KT)], writes=[("psY", yi)])
                            si = c5["ysc"] % 4
                            c5["ysc"] += 1
                            P.op("dve", lambda e: e.scalar_tensor_tensor(out=ysc[si][:], in0=psY[yi][:], scalar=meta[:, e_, st, 0:1],
                                                                         in1=ga2row[:, db * 512:(db + 1) * 512], op0=ALU.mult, op1=ALU.mult),
                                 reads=[("psY", yi), "meta", "ga2row"], writes=[("ysc", si)])
                            for ev in prev_sc:
                                P.wait("pool", ev)
                            ev = P.dma("pool", lambda e: e.indirect_dma_start(out=out4[:, :],
                                                                             out_offset=bass.IndirectOffsetOnAxis(ap=idx4[:, e_, st, db:db + 1], axis=0),
                                                                             in_=ysc[si][:], in_offset=None, compute_op=ALU.add),
                                       reads=[("ysc", si), "idx4"], writes=[])
                            cur_sc.append(ev)
                    prev_sc = cur_sc
                P.barrier(pool_ring=True)
        P.barrier(pool_ring=True)
        return nc, P


_CONST_CACHE = {}


def _prep_inputs(inputs):
    if "c" not in _CONST_CACHE:
        _CONST_CACHE["c"] = _consts()
    rope, namask = _CONST_CACHE["c"]
    f = lambda a: np.ascontiguousarray(np.asarray(a, dtype=np.float32))
    x = f(inputs["x"]); ctx = f(inputs["ctx"]); c = f(inputs["c"]); c_ctx = f(inputs["c_ctx"])
    smallp = np.concatenate([f(inputs[k])[0] for k in ("q_gain_a", "k_gain_a", "lam_q1", "lam_k1", "lam_q2", "lam_k2",
                                                       "subln_gain", "q_gain_b", "k_gain_b")]).reshape(1, 768)
    shared = {
        "w_mod": f(inputs["w_mod"])[0], "b_mod": f(inputs["b_mod"])[0].reshape(1, -1),
        "g12": np.stack([f(inputs["g_norm1"])[0], f(inputs["g_norm2"])[0]]),
        "w_in": f(inputs["w_in"])[0], "smallp": smallp,
        "rpbx": _rpb_expand(f(inputs["rel_pos_bias"])[0]), "namask": namask, "rope": rope,
        "w_a": f(inputs["w_branch_a"])[0], "w_b": f(inputs["w_branch_b"])[0], "w_o": f(inputs["w_out"])[0],
        "w_r": f(inputs["w_router"])[0], "w_eg": f(inputs["w_exp_gate"])[0], "w_eu": f(inputs["w_exp_up"])[0],
        "w_ed": f(inputs["w_exp_down"])[0],
    }
    maps = []
    for core in range(N_CORES):
        b = core % 4
        cc = np.stack([c[b].reshape(KT, 128).T, c_ctx.reshape(KT, 128).T], axis=-1)
        m = dict(shared)
        m.update({"x": x[b], "ctx": ctx[b], "cc": np.ascontiguousarray(cc)})
        maps.append(m)
    return maps


def kernel(**inputs):
    maps = _prep_inputs(inputs)
    nc, _ = build()
    res = run_bass_kernel_spmd(nc, maps, core_ids=list(range(N_CORES)))
    out = np.stack([res.results[b]["out"] for b in range(4)], axis=0)
    return out.astype(np.float32)
```

```python
import math
from contextlib import ExitStack

import numpy as np
import concourse.bass as bass
import concourse.mybir as mybir
from concourse.bass_utils import run_bass_kernel_spmd

F32 = mybir.dt.float32
BF16 = mybir.dt.bfloat16
I32 = mybir.dt.int32
AF = mybir.ActivationFunctionType
ALU = mybir.AluOpType
AX = mybir.AxisListType

D = 2048
S = 8192
L = 256
NK = S + L
NT = S // 128
KT = D // 128
GRID_W = 64
PROJ = 10240
OFF_QA, OFF_QB, OFF_GATE, OFF_KA, OFF_VA, OFF_KB, OFF_VB = 0, 1024, 2048, 6144, 7168, 8192, 9216
NE = 16
CAP = 1024
EPS = 1e-6
LAM_INIT = 0.8 - 0.6 * math.exp(0.0)
N_CORES = 8


class Ev:
    __slots__ = ("sem", "name", "val")

    def __init__(self, sem, name, val):
        self.sem, self.name, self.val = sem, name, val


class Prog:
    def __init__(self, nc, es):
        self.nc = nc
        self.eng = {"pe": nc.tensor, "act": nc.scalar, "dve": nc.vector, "pool": nc.gpsimd, "sp": nc.sync}
        self.esem = {}
        self.ecnt = {}
        for e in ("pe", "act", "dve", "pool"):
            self.esem[e] = es.enter_context(nc.semaphore("s_" + e))
            self.ecnt[e] = 0
        self.rings = {}
        self.rpos = {}
        for e, n in (("sp", 12), ("pool", 12), ("act", 8)):
            self.rings[e] = [[es.enter_context(nc.semaphore("d_%s%d" % (e, i))), "d_%s%d" % (e, i), 0] for i in range(n)]
            self.rpos[e] = 0
        self.waited = {e: {} for e in self.eng}
        self.lastw = {}
        self.readers = {}
        self.nins = 0

    def wait(self, eng, ev):
        if ev is None:
            return
        if eng == "pe" and ev.name == "s_pe":
            return
        w = self.waited[eng]
        if w.get(ev.name, 0) >= ev.val:
            return
        self.eng[eng].wait_ge(ev.sem, ev.val)
        w[ev.name] = ev.val
        self.nins += 1

    def _hazards(self, eng, reads, writes):
        for k in reads:
            self.wait(eng, self.lastw.get(k))
        for k in writes:
            self.wait(eng, self.lastw.get(k))
            rd = self.readers.get(k)
            if rd:
                for ev in rd.values():
                    self.wait(eng, ev)

    def _record(self, ev, reads, writes):
        for k in reads:
            self.readers.setdefault(k, {})[ev.name] = ev
        for k in writes:
            self.lastw[k] = ev
            self.readers[k] = {}

    def op(self, eng, fn, reads=(), writes=()):
        self._hazards(eng, reads, writes)
        ins = fn(self.eng[eng])
        self.ecnt[eng] += 1
        ins.then_inc(self.esem[eng], 1)
        ev = Ev(self.esem[eng], "s_" + eng, self.ecnt[eng])
        self._record(ev, reads, writes)
        self.nins += 1
        return ev

    def dma(self, eng, fn, reads=(), writes=()):
        ring = self.rings[eng]
        i = self.rpos[eng]
        self.rpos[eng] = (i + 1) % len(ring)
        sem, name, cnt = ring[i]
        if cnt:
            self.wait(eng, Ev(sem, name, cnt))
        self._hazards(eng, reads, writes)
        ins = fn(self.eng[eng])
        ins.then_inc(sem, 16)
        ring[i][2] = cnt + 16
        ev = Ev(sem, name, cnt + 16)
        self._record(ev, reads, writes)
        self.nins += 1
        return ev

    def barrier(self, pool_ring=False):
        evs = [Ev(self.esem[e], "s_" + e, self.ecnt[e]) for e in self.esem if self.ecnt[e]]
        for e in self.rings:
            if e == "pool" and not pool_ring:
                continue
            for sem, name, cnt in self.rings[e]:
                if cnt:
                    evs.append(Ev(sem, name, cnt))
        for e in self.eng:
            for ev in evs:
                self.wait(e, ev)
        keep = {k: v for k, v in self.lastw.items() if "wbf" in repr(k)}
        self.lastw = keep
        self.readers = {}


def _consts():
    t = np.arange(S)
    row = (t // GRID_W).astype(np.float32)
    col = (t % GRID_W).astype(np.float32)
    half = 32
    inv_freq = (10000.0 ** (-np.arange(0, half, 2, dtype=np.float32) / half)).astype(np.float32)

    def tab(pos):
        ang = pos[:, None] * inv_freq[None, :]
        ang = np.concatenate([ang, ang], axis=-1)
        return np.cos(ang), np.sin(ang)

    cr, sr = tab(row)
    cc, sc = tab(col)
    cos = np.concatenate([cr, cc], -1).astype(np.float32)
    sin = np.concatenate([sr, sc], -1).astype(np.float32)
    ss = sin.reshape(S, 2, 2, 16).copy()
    ss[:, :, 0, :] *= -1.0
    ss = ss.reshape(S, 64)
    rope = np.stack([cos, ss], axis=1)
    rope = np.ascontiguousarray(rope.reshape(NT, 128, 2, 64).transpose(1, 0, 2, 3))
    q = np.arange(64)
    cs = np.clip(q - 8, 0, 48)
    wk = np.arange(64)
    inwin = (wk[None, :] >= cs[:, None]) & (wk[None, :] < cs[:, None] + 16)
    m = np.zeros((128, 4, 64), np.float32)
    for i in range(8):
        m[(i % 2) * 64:(i % 2) * 64 + 64, i // 2, :] = inwin.T.astype(np.float32)
    return rope.astype(np.float32), m


def _rpb_expand(rpb):
    H = rpb.shape[0]
    q = np.arange(64)
    wk = np.arange(64)
    idx_c = np.clip(wk[:, None] - q[None, :] + 15, 0, 30)
    out = np.zeros((H, 128, 8, 4, 64), np.float32)
    for v in range(8):
        for i in range(8):
            idx_r = 7 - v + i
            out[:, (i % 2) * 64:(i % 2) * 64 + 64, v, i // 2, :] = rpb[:, idx_r][:, idx_c]
    return out


def build(debug=False, upto=99, lim=None):
    lim = lim or {}
    nc = bass.Bass("TRN2", target_bir_lowering=False)

    def din(name, shape, dt=F32):
        return nc.dram_tensor(name, list(shape), dt, kind="ExternalInput").ap()

    def dscr(name, shape, dt, dbg=False):
        kind = "ExternalOutput" if (debug and dbg) else "Internal"
        return nc.dram_tensor(name, list(shape), dt, kind=kind).ap()

    x_d = din("x", [S, D])
    ctx_d = din("ctx", [L, D])
    cc_d = din("cc", [128, KT, 2])
    wmod_d = din("w_mod", [D, 6 * D])
    bmod_d = din("b_mod", [1, 6 * D])
    g12_d = din("g12", [2, D])
    win_d = din("w_in", [D, PROJ])
    smallp_d = din("smallp", [1, 768])
    rpbx_d = din("rpbx", [8, 128, 8, 4, 64])
    namask_d = din("namask", [128, 4, 64])
    rope_d = din("rope", [128, NT, 2, 64])
    wa_d = din("w_a", [1024, D])
    wb_d = din("w_b", [1024, D])
    wo_d = din("w_o", [D, D])
    wr_d = din("w_r", [D, NE])
    weg_d = din("w_eg", [NE, D, D])
    weu_d = din("w_eu", [NE, D, D])
    wed_d = din("w_ed", [NE, D, D])
    out_d = nc.dram_tensor("out", [S, D], F32, kind="ExternalOutput").ap()

    qaT_d = dscr("qaT", [8, 128, S], BF16, True)
    kaT_d = dscr("kaT", [8, 128, NK], BF16, True)
    va_d = dscr("va", [NK, 1024], BF16, True)
    qbT_d = dscr("qbT", [8, 128, S], BF16, True)
    kbT_d = dscr("kbT", [8, 128, NK], BF16, True)
    vb_d = dscr("vb", [NK, 1024], BF16, True)
    gT_d = dscr("gT", [4096, S], BF16, True)
    yaT_d = dscr("yaT", [8, 128, S], BF16, True)
    ybT_d = dscr("ybT", [8, 128, S], BF16, True)
    mT_d = dscr("mT", [KT, 128, S], BF16, True)
    h2_d = dscr("h2", [S, D], BF16, True)
    modsave_d = dscr("modsave", [4, 128, D], F32, True)
    dbg_d = dscr("dbg", [128, 4096], F32, True)
    wbf_in = dscr("wbf_in", [D, PROJ], BF16)
    wbf_a = dscr("wbf_a", [1024, D], BF16)
    wbf_b = dscr("wbf_b", [1024, D], BF16)
    wbf_o = dscr("wbf_o", [D, D], BF16)
    wbf_eg = dscr("wbf_eg", [NE, D, D], BF16)
    wbf_eu = dscr("wbf_eu", [NE, D, D], BF16)
    wbf_ed = dscr("wbf_ed", [NE, D, D], BF16)

    with ExitStack() as es:
        P = Prog(nc, es)

        uniq = [0]

        def sb(name, shape, dt, stack=es):
            uniq[0] += 1
            return stack.enter_context(nc.sbuf_tensor("sb%d_%s" % (uniq[0], name), list(shape), dt))

        def ps(name, shape, dt, stack=es):
            uniq[0] += 1
            return stack.enter_context(nc.psum_tensor("ps%d_%s" % (uniq[0], name), list(shape), dt))

        ident_f = sb("ident_f", [128, 128], F32)
        ident_b = sb("ident_b", [128, 128], BF16)
        ones_b = sb("ones_b", [128, 128], BF16)
        rstd_all = sb("rstd_all", [128, NT + 2], F32)
        aff_all = sb("aff_all", [128, NT, NE], F32)
        neg_lam = sb("neg_lam", [128, 1], F32)
        gains = sb("gains", [128, 768], F32)
        iota_p = sb("iota_p", [128, 1], F32)
        ones_f = sb("ones_f", [128, 128], F32)
        subln_col = sb("subln_col", [128, 1], F32)
        eps_col = sb("eps_col", [128, 1], F32)
        iota_row = sb("iota_row", [128, 128], F32)

        def convert(src, dst, rows, cols, key):
            for r0 in range(0, rows, 512):
                for c0 in range(0, cols, 2048):
                    P.dma("pool", lambda e, r0=r0, c0=c0: e.dma_start(
                        out=dst[r0:r0 + 512, c0:c0 + 2048], in_=src[r0:r0 + 512, c0:c0 + 2048]),
                        writes=[(key, r0 // 512, c0 // 2048)])

        ph01 = es.enter_context(ExitStack())
        gs1row = sb("gs1row", [128, 2, D], F32, ph01)
        sh1rep = sb("sh1rep", [128, 2, KT, 128], BF16, ph01)
        with ExitStack() as ph:
            P.op("pool", lambda e: e.iota(iota_row[:], pattern=[[1, 128]], base=0, channel_multiplier=0,
                                          allow_small_or_imprecise_dtypes=True), writes=["iota_row"])
            P.op("pool", lambda e: e.iota(iota_p[:], pattern=[[0, 1]], base=0, channel_multiplier=1,
                                          allow_small_or_imprecise_dtypes=True), writes=["iota_p"])
            convert(win_d, wbf_in, D, PROJ, "wbf_in")
            convert(wa_d, wbf_a, 1024, D, "wbf_a")
            convert(wb_d, wbf_b, 1024, D, "wbf_b")
            convert(wo_d, wbf_o, D, D, "wbf_o")
            for e_ in range(NE if upto >= 4 else 0):
                convert(weg_d[e_], wbf_eg[e_], D, D, ("wbf_eg", e_))
                convert(weu_d[e_], wbf_eu[e_], D, D, ("wbf_eu", e_))
                convert(wed_d[e_], wbf_ed[e_], D, D, ("wbf_ed", e_))

            P.op("dve", lambda e: e.tensor_scalar(out=ident_f[:], in0=iota_row[:], scalar1=iota_p[:, 0:1], scalar2=None,
                                                  op0=ALU.is_equal), reads=["iota_row", "iota_p"], writes=["ident_f"])
            P.op("dve", lambda e: e.tensor_copy(out=ident_b[:], in_=ident_f[:]), reads=["ident_f"], writes=["ident_b"])
            P.op("dve", lambda e: e.memset(ones_b[:], 1.0), writes=["ones_b"])
            P.op("dve", lambda e: e.memset(ones_f[:], 1.0), writes=["ones_f"])
            P.op("dve", lambda e: e.memset(eps_col[:], EPS), writes=["eps_col"])

            cc = sb("cc", [128, KT, 2], F32, ph)
            csl = sb("csl", [128, KT, 2], F32, ph)
            crep = sb("crep", [128, KT, 2, 128], F32, ph)
            P.dma("sp", lambda e: e.dma_start(out=cc[:], in_=cc_d[:, :, :]), writes=["cc"])
            P.op("act", lambda e: e.activation(out=csl[:], in_=cc[:], func=AF.Silu), reads=["cc"], writes=["csl"])
            P.op("dve", lambda e: e.tensor_copy(out=crep[:], in_=csl[:].unsqueeze(3).to_broadcast([128, KT, 2, 128])),
                 reads=["csl"], writes=["crep"])
            modrow = sb("modrow", [128, 6 * D], F32, ph)
            modrow_c = sb("modrow_c", [128, 2 * D], F32, ph)
            MB = 512
            wmb = [sb("wmb%d" % i, [128, KT, MB], F32, ph) for i in range(2)]
            bmb = [sb("bmb%d" % i, [128, MB], F32, ph) for i in range(2)]
            psm = [ps("psm%d" % i, [128, MB], F32, ph) for i in range(4)]
            npm = 0
            nblk = 6 * D // MB
            for blk in range(nblk):
                i = blk % 2
                P.dma("sp", lambda e: e.dma_start(out=wmb[i][:], in_=wmod_d[:, blk * MB:(blk + 1) * MB].rearrange("(k p) c -> p k c", p=128)),
                      writes=[("wmb", i)])
                P.dma("sp", lambda e: e.dma_start(out=bmb[i][:], in_=bmod_d[0:1, blk * MB:(blk + 1) * MB].partition_broadcast(128)),
                      writes=[("bmb", i)])
                for j in range(2 if blk < 2 * D // MB else 1):
                    pt = psm[npm % 4]
                    pk = ("psm", npm % 4)
                    npm += 1
                    for k in range(KT):
                        P.op("pe", lambda e: e.matmul(pt[:], lhsT=crep[:, k, j, :], rhs=wmb[i][:, k, :], start=(k == 0), stop=(k == KT - 1)),
                             reads=["crep", ("wmb", i)], writes=[pk])
                    dst = modrow if j == 0 else modrow_c
                    P.op("dve", lambda e: e.tensor_tensor(out=dst[:, blk * MB:(blk + 1) * MB], in0=pt[:], in1=bmb[i][:], op=ALU.add),
                         reads=[pk, ("bmb", i)], writes=[("modrow", j, blk)])
            g12 = sb("g12", [128, 2, D], F32, ph)
            P.dma("sp", lambda e: e.dma_start(out=g12[:, 0, :], in_=g12_d[0:1, :].partition_broadcast(128)), writes=["g12a"])
            P.dma("sp", lambda e: e.dma_start(out=g12[:, 1, :], in_=g12_d[1:2, :].partition_broadcast(128)), writes=["g12b"])
            mr_all = [("modrow", 0, b_) for b_ in range(nblk)]
            mrc_all = [("modrow", 1, b_) for b_ in range(2 * D // MB)]
            P.op("dve", lambda e: e.scalar_tensor_tensor(out=gs1row[:, 0, :], in0=modrow[:, D:2 * D], scalar=1.0, in1=g12[:, 0, :],
                                                         op0=ALU.add, op1=ALU.mult), reads=mr_all + ["g12a"], writes=["gs1row0"])
            P.op("dve", lambda e: e.scalar_tensor_tensor(out=gs1row[:, 1, :], in0=modrow_c[:, D:2 * D], scalar=1.0, in1=g12[:, 0, :],
                                                         op0=ALU.add, op1=ALU.mult), reads=mrc_all + ["g12a"], writes=["gs1row1"])
            P.op("dve", lambda e: e.scalar_tensor_tensor(out=modrow[:, 4 * D:5 * D], in0=modrow[:, 4 * D:5 * D], scalar=1.0, in1=g12[:, 1, :],
                                                         op0=ALU.add, op1=ALU.mult), reads=mr_all + ["g12b"], writes=mr_all)
            for j in range(4):
                P.dma("sp", lambda e: e.dma_start(out=modsave_d[j], in_=modrow[:, (2 + j) * D:(3 + j) * D]), reads=mr_all, writes=[("modsave", j)])
            dtmp = sb("dtmp", [128, KT, 128], F32, ph)
            shc = sb("shc", [128, 2, KT], F32, ph)
            for j in range(2):
                src = modrow if j == 0 else modrow_c
                P.op("dve", lambda e: e.tensor_tensor(out=dtmp[:], in0=src[:, 0:D].rearrange("p (k m) -> p k m", m=128),
                                                      in1=ident_f[:].unsqueeze(1).to_broadcast([128, KT, 128]), op=ALU.mult),
                     reads=(mr_all if j == 0 else mrc_all) + ["ident_f"], writes=["dtmp"])
                P.op("dve", lambda e: e.tensor_reduce(out=shc[:, j, :], in_=dtmp[:], axis=AX.X, op=ALU.add), reads=["dtmp"], writes=[("shc", j)])
                P.op("dve", lambda e: e.tensor_copy(out=sh1rep[:, j, :, :], in_=shc[:, j, :].unsqueeze(2).to_broadcast([128, KT, 128])),
                     reads=[("shc", j)], writes=[("sh1rep", j)])
            P.dma("sp", lambda e: e.dma_start(out=gains[:], in_=smallp_d[0:1, :].partition_broadcast(128)), writes=["gains"])
            lt = sb("lt", [128, 2, 64], F32, ph)
            ls = sb("ls", [128, 4], F32, ph)
            P.op("dve", lambda e: e.tensor_tensor(out=lt[:, 0, :], in0=gains[:, 128:192], in1=gains[:, 192:256], op=ALU.mult), reads=["gains"], writes=["lt0"])
            P.op("dve", lambda e: e.tensor_tensor(out=lt[:, 1, :], in0=gains[:, 256:320], in1=gains[:, 320:384], op=ALU.mult), reads=["gains"], writes=["lt1"])
            P.op("dve", lambda e: e.tensor_reduce(out=ls[:, 0:2], in_=lt[:], axis=AX.X, op=ALU.add), reads=["lt0", "lt1"], writes=["ls01"])
            P.op("act", lambda e: e.activation(out=ls[:, 2:4], in_=ls[:, 0:2], func=AF.Exp), reads=["ls01"], writes=["ls23"])
            P.op("dve", lambda e: e.tensor_tensor(out=ls[:, 0:1], in0=ls[:, 3:4], in1=ls[:, 2:3], op=ALU.subtract), reads=["ls23"], writes=["ls0"])
            P.op("dve", lambda e: e.tensor_scalar(out=neg_lam[:], in0=ls[:, 0:1], scalar1=-LAM_INIT, scalar2=None, op0=ALU.add),
                 reads=["ls0"], writes=["neg_lam"])
            P.op("dve", lambda e: e.tensor_scalar(out=gains[:, 0:64], in0=gains[:, 0:64], scalar1=0.125, scalar2=None, op0=ALU.mult),
                 reads=["gains", "lt0", "lt1"], writes=["gains"])
            P.op("dve", lambda e: e.tensor_scalar(out=gains[:, 384:512], in0=gains[:, 384:512], scalar1=1.0 - LAM_INIT, scalar2=None, op0=ALU.mult),
                 reads=["gains"], writes=["gains"])
            P.op("dve", lambda e: e.tensor_scalar(out=gains[:, 512:640], in0=gains[:, 512:640], scalar1=128.0 ** -0.5, scalar2=None, op0=ALU.mult),
                 reads=["gains"], writes=["gains"])
            sdt = sb("sdt", [128, 128], F32, ph)
            P.op("dve", lambda e: e.tensor_tensor(out=sdt[:], in0=gains[:, 384:512], in1=ident_f[:], op=ALU.mult), reads=["gains", "ident_f"], writes=["sdt"])
            P.op("dve", lambda e: e.tensor_reduce(out=subln_col[:], in_=sdt[:], axis=AX.X, op=ALU.add), reads=["sdt"], writes=["subln_col"])
            if debug:
                P.dma("sp", lambda e: e.dma_start(out=dbg_d[:, 0:768], in_=gains[:]), reads=["gains"], writes=["dbg0"])
                P.dma("sp", lambda e: e.dma_start(out=dbg_d[:, 768:769], in_=neg_lam[:], allow_slow_non_contiguous=True), reads=["neg_lam"], writes=["dbg1"])
                P.dma("sp", lambda e: e.dma_start(out=dbg_d[:, 1024:1024 + 2 * KT], in_=shc[:].rearrange("p a k -> p (a k)")),
                      reads=[("shc", 0), ("shc", 1)], writes=["dbg2"])
            P.barrier()
        if upto < 1:
            P.barrier(pool_ring=True)
            return nc, P

        GT = 8
        with ExitStack() as ph:
            xT = sb("xT", [128, KT, GT * 128], BF16, ph)
            wbuf = [sb("wbuf%d" % i, [128, KT, 512], BF16, ph) for i in range(2)]
            ropet = sb("ropet", [128, GT, 2, 64], F32, ph)
            xt = [sb("xt%d" % i, [128, D], F32, ph) for i in range(2)]
            xb = [sb("xb%d" % i, [128, D], BF16, ph) for i in range(2)]
            junk = sb("junk", [128, D], BF16, ph)
            ssq = sb("ssq", [128, 2], F32, ph)
            shwb = [sb("shwb%d" % i, [128, 512], F32, ph) for i in range(2)]
            NB = 3
            pv = [sb("pv%d" % i, [128, 512], F32, ph) for i in range(NB)]
            sq = [sb("sq%d" % i, [128, 512], F32, ph) for i in range(NB)]
            qn = [sb("qn%d" % i, [128, 512], F32, ph) for i in range(NB)]
            t1 = [sb("t1%d" % i, [128, 512], F32, ph) for i in range(NB)]
            t2 = [sb("t2%d" % i, [128, 512], F32, ph) for i in range(NB)]
            s8 = [sb("s8%d" % i, [128, 8], F32, ph) for i in range(NB)]
            qo = [sb("qo%d" % i, [128, 512], BF16, ph) for i in range(NB)]
            stage = [sb("stage%d" % i, [128, 4, GT * 128], BF16, ph) for i in range(2)]
            pT = [ps("pT%d" % i, [128, 4, 128], BF16, ph) for i in range(2)]
            pM = [ps("pM%d" % i, [128, 512], F32, ph) for i in range(3)]
            pW = ps("pW", [128, 512], F32, ph)
            cnt = {"pT": 0, "pM": 0, "pp": 0, "stage": 0, "w": 0}

            blocks = []
            for cb in range(20):
                c0 = cb * 512
                if c0 < OFF_QB:
                    blocks.append(("qa", cb, c0 // 128))
                elif c0 < OFF_GATE:
                    blocks.append(("qb", cb, (c0 - OFF_QB) // 128))
                elif c0 < OFF_KA:
                    blocks.append(("gate", cb, (c0 - OFF_GATE) // 128))
                elif c0 < OFF_VA:
                    blocks.append(("ka", cb, (c0 - OFF_KA) // 128))
                elif c0 < OFF_KB:
                    blocks.append(("va", cb, (c0 - OFF_VA)))
                elif c0 < OFF_VB:
                    blocks.append(("kb", cb, (c0 - OFF_KB) // 128))
                else:
                    blocks.append(("vb", cb, (c0 - OFF_VB)))

            groups = [("lat", g) for g in range(NT // GT)] + [("ctx", 0)]
            for gkind, g in groups:
                is_ctx = gkind == "ctx"
                ntile = 2 if is_ctx else GT
                mj = 1 if is_ctx else 0
                src_d = ctx_d if is_ctx else x_d
                tok0 = 0 if is_ctx else g * GT * 128
                key0 = 0 if is_ctx else L + g * GT * 128
                if not is_ctx:
                    P.dma("sp", lambda e: e.dma_start(out=ropet[:], in_=rope_d[:, g * GT:(g + 1) * GT, :, :]), writes=["ropet"])
                for tt in range(ntile):
                    i = tt % 2
                    rcol = (NT + tt) if is_ctx else (g * GT + tt)
                    P.dma("sp", lambda e: e.dma_start(out=xt[i][:], in_=src_d[tok0 + tt * 128: tok0 + (tt + 1) * 128, :]), writes=[("xt", i)])
                    P.op("act", lambda e: e.activation(out=junk[:], in_=xt[i][:], func=AF.Square, accum_out=ssq[:, 0:1]),
                         reads=[("xt", i)], writes=["junk", "ssq0"])
                    P.op("act", lambda e: e.activation(out=ssq[:, 1:2], in_=ssq[:, 0:1], func=AF.Sqrt, scale=1.0 / D, bias=EPS),
                         reads=["ssq0"], writes=["ssq1"])
                    P.op("dve", lambda e: e.reciprocal(out=rstd_all[:, rcol:rcol + 1], in_=ssq[:, 1:2]), reads=["ssq1"], writes=[("rstd", rcol)])
                    P.op("dve", lambda e: e.tensor_tensor(out=xb[i][:], in0=xt[i][:], in1=gs1row[:, mj, :], op=ALU.mult),
                         reads=[("xt", i), "gs1row%d" % mj], writes=[("xb", i)])
                    for j4 in range(4):
                        pi = cnt["pT"] % 2
                        cnt["pT"] += 1
                        for jj in range(4):
                            k = j4 * 4 + jj
                            P.op("pe", lambda e: e.transpose(out=pT[pi][:, jj, :], in_=xb[i][:, k * 128:(k + 1) * 128], identity=ident_b[:]),
                                 reads=[("xb", i)], writes=[("pT", pi)])
                        eng = "act" if j4 % 2 == 0 else "dve"
                        if eng == "act":
                            P.op("act", lambda e: e.copy(out=xT[:, j4 * 4:(j4 + 1) * 4, tt * 128:(tt + 1) * 128], in_=pT[pi][:]),
                                 reads=[("pT", pi)], writes=[("xT", tt)])
                        else:
                            P.op("dve", lambda e: e.tensor_copy(out=xT[:, j4 * 4:(j4 + 1) * 4, tt * 128:(tt + 1) * 128], in_=pT[pi][:]),
                                 reads=[("pT", pi)], writes=[("xT", tt)])
                for kind, cb, hoff in blocks:
                    if is_ctx and kind in ("qa", "qb", "gate"):
                        continue
                    wi = cnt["w"] % 2
                    cnt["w"] += 1
                    wkeys = [("wbf_in", r_, (cb * 512) // 2048) for r_ in range(4)]
                    P.dma("sp", lambda e: e.dma_start(out=wbuf[wi][:], in_=wbf_in[:, cb * 512:(cb + 1) * 512].rearrange("(k p) c -> p k c", p=128)),
                          reads=wkeys, writes=[("wbuf", wi)])
                    for k in range(KT):
                        P.op("pe", lambda e: e.matmul(pW[:], lhsT=sh1rep[:, mj, k, :], rhs=wbuf[wi][:, k, :], start=(k == 0), stop=(k == KT - 1)),
                             reads=[("sh1rep", mj), ("wbuf", wi)], writes=["pW"])
                    P.op("act", lambda e: e.copy(out=shwb[wi][:], in_=pW[:]), reads=["pW"], writes=[("shwb", wi)])
                    need_stage = kind in ("qa", "qb", "ka", "kb", "gate")
                    if need_stage:
                        si = cnt["stage"] % 2
                        cnt["stage"] += 1
                    pending = []
                    for tt in range(ntile):
                        rcol = (NT + tt) if is_ctx else (g * GT + tt)
                        mi = cnt["pM"] % 3
                        cnt["pM"] += 1
                        for k in range(KT):
                            P.op("pe", lambda e: e.matmul(pM[mi][:], lhsT=xT[:, k, tt * 128:(tt + 1) * 128], rhs=wbuf[wi][:, k, :],
                                                          start=(k == 0), stop=(k == KT - 1)),
                                 reads=[("xT", tt), ("wbuf", wi)], writes=[("pM", mi)])
                        while len(pending) > 1:
                            pending.pop(0)()
                        bi = cnt["pp"] % NB
                        cnt["pp"] += 1
                        if kind in ("va", "vb"):
                            P.op("dve", lambda e: e.scalar_tensor_tensor(out=qo[bi][:], in0=pM[mi][:], scalar=rstd_all[:, rcol:rcol + 1],
                                                                         in1=shwb[wi][:], op0=ALU.mult, op1=ALU.add),
                                 reads=[("pM", mi), ("rstd", rcol), ("shwb", wi)], writes=[("qo", bi)])
                            dst = va_d if kind == "va" else vb_d
                            P.dma("act", lambda e: e.dma_start(out=dst[key0 + tt * 128:key0 + (tt + 1) * 128, hoff:hoff + 512], in_=qo[bi][:]),
                                  reads=[("qo", bi)], writes=[(kind, key0 + tt * 128, hoff)])
                            continue
                        P.op("dve", lambda e: e.scalar_tensor_tensor(out=pv[bi][:], in0=pM[mi][:], scalar=rstd_all[:, rcol:rcol + 1],
                                                                     in1=shwb[wi][:], op0=ALU.mult, op1=ALU.add),
                             reads=[("pM", mi), ("rstd", rcol), ("shwb", wi)], writes=[("pv", bi)])
                        if kind == "gate":
                            P.op("act", lambda e: e.activation(out=qo[bi][:], in_=pv[bi][:], func=AF.Sigmoid), reads=[("pv", bi)], writes=[("qo", bi)])
                        else:
                            npc, wdt = (8, 64) if kind in ("qa", "ka") else (4, 128)
                            goff = {"qa": 0, "ka": 64, "qb": 512, "kb": 640}[kind]
                            P.op("act", lambda e: e.activation(out=sq[bi][:], in_=pv[bi][:], func=AF.Square), reads=[("pv", bi)], writes=[("sq", bi)])
                            P.op("dve", lambda e: e.tensor_reduce(out=s8[bi][:, 0:npc], in_=sq[bi][:].rearrange("p (a b) -> p a b", b=wdt),
                                                                  axis=AX.X, op=ALU.add), reads=[("sq", bi)], writes=[("s8", bi)])
                            P.op("act", lambda e: e.activation(out=s8[bi][:, 0:npc], in_=s8[bi][:, 0:npc], func=AF.Sqrt, scale=1.0 / wdt, bias=EPS),
                                 reads=[("s8", bi)], writes=[("s8", bi)])
                            P.op("dve", lambda e: e.reciprocal(out=s8[bi][:, 0:npc], in_=s8[bi][:, 0:npc]), reads=[("s8", bi)], writes=[("s8", bi)])
                            P.op("dve", lambda e: e.tensor_tensor(out=qn[bi][:].rearrange("p (a b) -> p a b", b=wdt),
                                                                  in0=pv[bi][:].rearrange("p (a b) -> p a b", b=wdt),
                                                                  in1=s8[bi][:, 0:npc].unsqueeze(2).to_broadcast([128, npc, wdt]), op=ALU.mult),
                                 reads=[("pv", bi), ("s8", bi)], writes=[("qn", bi)])
                            rope_on = kind in ("qa", "ka") and not is_ctx
                            gdst = qn[bi] if rope_on else qo[bi]
                            P.op("dve", lambda e: e.tensor_tensor(out=gdst[:].rearrange("p (a b) -> p a b", b=wdt),
                                                                  in0=qn[bi][:].rearrange("p (a b) -> p a b", b=wdt),
                                                                  in1=gains[:, goff:goff + wdt].unsqueeze(1).to_broadcast([128, npc, wdt]), op=ALU.mult),
                                 reads=[("qn", bi), "gains"], writes=[("qn", bi) if rope_on else ("qo", bi)])
                            if rope_on:
                                q5 = qn[bi][:].rearrange("p (a r h w) -> p a r h w", a=8, r=2, h=2, w=16)
                                t5 = t2[bi][:].rearrange("p (a r h w) -> p a r h w", a=8, r=2, h=2, w=16)
                                cosb = ropet[:, tt, 0, :].unsqueeze(1).to_broadcast([128, 8, 64])
                                ss4 = ropet[:, tt, 1, :].rearrange("p (r h w) -> p r h w", r=2, h=2, w=16)
                                P.op("dve", lambda e: e.tensor_tensor(out=t1[bi][:].rearrange("p (a b) -> p a b", b=64),
                                                                      in0=qn[bi][:].rearrange("p (a b) -> p a b", b=64), in1=cosb, op=ALU.mult),
                                     reads=[("qn", bi), "ropet"], writes=[("t1", bi)])
                                for r_ in range(2):
                                    for h_ in range(2):
                                        P.op("dve", lambda e: e.tensor_tensor(out=t5[:, :, r_, h_, :], in0=q5[:, :, r_, 1 - h_, :],
                                                                              in1=ss4[:, r_, h_, :].unsqueeze(1).to_broadcast([128, 8, 16]), op=ALU.mult),
                                             reads=[("qn", bi), "ropet"], writes=[("t2", bi, r_, h_)])
                                P.op("dve", lambda e: e.tensor_tensor(out=qo[bi][:], in0=t1[bi][:], in1=t2[bi][:], op=ALU.add),
                                     reads=[("t1", bi)] + [("t2", bi, r_, h_) for r_ in range(2) for h_ in range(2)], writes=[("qo", bi)])
                        def do_tr(bi=bi, si=si, tt=tt):
                            pi = cnt["pT"] % 2
                            cnt["pT"] += 1
                            for jj in range(4):
                                P.op("pe", lambda e: e.transpose(out=pT[pi][:, jj, :], in_=qo[bi][:, jj * 128:(jj + 1) * 128], identity=ident_b[:]),
                                     reads=[("qo", bi)], writes=[("pT", pi)])
                            P.op("act", lambda e: e.copy(out=stage[si][:, :, tt * 128:(tt + 1) * 128], in_=pT[pi][:]),
                                 reads=[("pT", pi)], writes=[("stage", si)])
                        pending.append(do_tr)
                    while pending:
                        pending.pop(0)()
                    if need_stage:
                        nt_ = ntile * 128
                        if kind == "gate":
                            r0 = hoff * 128
                            P.dma("act", lambda e: e.dma_start(out=gT_d[r0:r0 + 512, tok0:tok0 + nt_].rearrange("(j p) t -> p j t", p=128),
                                                              in_=stage[si][:, :, 0:nt_]), reads=[("stage", si)], writes=[("gT", cb, g)])
                        else:
                            dst = {"qa": qaT_d, "ka": kaT_d, "qb": qbT_d, "kb": kbT_d}[kind]
                            o0 = tok0 if kind in ("qa", "qb") else key0
                            P.dma("act", lambda e: e.dma_start(out=dst[hoff:hoff + 4, :, o0:o0 + nt_].rearrange("h p t -> p h t"),
                                                              in_=stage[si][:, :, 0:nt_]), reads=[("stage", si)], writes=[(kind, cb, g, gkind)])
            P.barrier()
        ph01.close()
        if upto < 2:
            P.barrier(pool_ring=True)
            return nc, P

        NKC = NK // 128
        with ExitStack() as ph:
            qT = [sb("aqT%d" % i, [128, S], BF16, ph) for i in range(2)]
            kT = [sb("akT%d" % i, [128, NK], BF16, ph) for i in range(2)]
            vv = [sb("avv%d" % i, [128, NKC, 128], BF16, ph) for i in range(2)]
            pTb = [sb("apT%d" % i, [128, 2, 512], BF16, ph) for i in range(3)]
            accS = sb("aaccS", [128, 2, 512], F32, ph)
            rb = [sb("arb%d" % i, [128, 512], F32, ph) for i in range(2)]
            tta = sb("atta", [128, 512], F32, ph)
            yy = sb("ayy", [128, 512], F32, ph)
            ysq = sb("aysq", [128, 512], F32, ph)
            rstdb = sb("arstdb", [128, 512], F32, ph)
            yo = [sb("ayo%d" % i, [128, 512], BF16, ph) for i in range(2)]
            psS = [ps("apsS%d" % i, [128, 2, 512], F32, ph) for i in range(2)]
            psO = [ps("apsO%d" % i, [128, 512], F32, ph) for i in range(2)]
            psF = [ps("apsF%d" % i, [128, 512], F32, ph) for i in range(2)]
            nhA = lim.get("headsA", 8)

            def load_head_a(h):
                hi = h % 2
                P.dma("sp", lambda e: e.dma_start(out=qT[hi][:], in_=qaT_d[h]), writes=[("qT", hi)])
                P.dma("sp", lambda e: e.dma_start(out=kT[hi][:], in_=kaT_d[h]), writes=[("kT", hi)])
                for c0 in range(0, NKC, 22):
                    P.dma("sp", lambda e: e.dma_start(out=vv[hi][:, c0:c0 + 22, :],
                                                      in_=va_d[c0 * 128:(c0 + 22) * 128, h * 128:(h + 1) * 128].rearrange("(c p) d -> p c d", p=128)),
                          writes=[("vv", hi, c0)])

            nqbA = lim.get("qbA", 16)
            stepsA = [(h, qb, kc) for h in range(nhA) for qb in range(nqbA) for kc in range(NKC)]

            def qk_a(s):
                h, qb, kc = stepsA[s]
                hi = h % 2
                si = s % 2
                for sm in range(2):
                    P.op("pe", lambda e: e.matmul(psS[si][:, sm, :], lhsT=kT[hi][sm * 64:(sm + 1) * 64, kc * 128:(kc + 1) * 128],
                                                  rhs=qT[hi][sm * 64:(sm + 1) * 64, qb * 512:(qb + 1) * 512], start=True, stop=True),
                         reads=[("kT", hi), ("qT", hi)], writes=[("psS", si, sm)])

            load_head_a(0)
            qk_a(0)
            for s, (h, qb, kc) in enumerate(stepsA):
                    hi = h % 2
                    vkeys = [("vv", hi, c0) for c0 in range(0, NKC, 22)]
                    if qb == 1 and kc == 0 and h + 1 < nhA:
                        load_head_a(h + 1)
                    si = s % 2
                    pi = s % 3
                    if s + 1 < len(stepsA):
                        qk_a(s + 1)
                    for sm in range(2):
                        P.op("act", lambda e: e.activation(out=pTb[pi][:, sm, :], in_=psS[si][:, sm, :], func=AF.Exp),
                             reads=[("psS", si, sm)], writes=[("pTb", pi, sm)])
                    if kc == 0:
                        P.op("dve", lambda e: e.tensor_copy(out=accS[:, 0, :], in_=pTb[pi][:, 0, :]), reads=[("pTb", pi, 0)], writes=["accS"])
                    else:
                        P.op("dve", lambda e: e.tensor_tensor(out=accS[:, 0, :], in0=accS[:, 0, :], in1=pTb[pi][:, 0, :], op=ALU.add),
                             reads=[("pTb", pi, 0), "accS"], writes=["accS"])
                    if kc == 0:
                        P.op("dve", lambda e: e.tensor_copy(out=accS[:, 1, :], in_=pTb[pi][:, 1, :]), reads=[("pTb", pi, 1)], writes=["accS1"])
                    elif kc % 2 == 0:
                        P.op("dve", lambda e: e.tensor_tensor(out=accS[:, 1, :], in0=accS[:, 1, :], in1=pTb[pi][:, 1, :], op=ALU.add),
                             reads=[("pTb", pi, 1), "accS1"], writes=["accS1"])
                    else:
                        P.op("pe", lambda e: e.matmul(psF[1][:], lhsT=ones_b[:], rhs=pTb[pi][:, 1, :], start=(kc == 1), stop=False),
                             reads=[("pTb", pi, 1), "ones_b"], writes=[("psF", 1)])
                    for sm in range(2):
                        P.op("pe", lambda e: e.matmul(psO[sm][:], lhsT=vv[hi][:, kc, :], rhs=pTb[pi][:, sm, :], start=(kc == 0), stop=(kc == NKC - 1)),
                             reads=[("pTb", pi, sm)] + vkeys, writes=[("psO", sm)])
                    if kc != NKC - 1:
                        continue
                    yi = qb % 2
                    for sm in range(2):
                        P.op("pe", lambda e: e.matmul(psF[sm][:], lhsT=ones_f[:], rhs=accS[:, sm, :], start=(sm == 0), stop=True),
                             reads=["accS" if sm == 0 else "accS1", "ones_f"], writes=[("psF", sm)])
                        P.op("dve", lambda e: e.reciprocal(out=rb[sm][:], in_=psF[sm][:]), reads=[("psF", sm)], writes=[("rb", sm)])
                    P.op("dve", lambda e: e.tensor_scalar(out=rb[1][:], in0=rb[1][:], scalar1=neg_lam[:, 0:1], scalar2=None, op0=ALU.mult),
                         reads=[("rb", 1), "neg_lam"], writes=[("rb", 1)])
                    P.op("dve", lambda e: e.tensor_tensor(out=tta[:], in0=psO[1][:], in1=rb[1][:], op=ALU.mult), reads=[("psO", 1), ("rb", 1)], writes=["tta"])
                    P.op("dve", lambda e: e.tensor_tensor(out=yy[:], in0=psO[0][:], in1=rb[0][:], op=ALU.mult), reads=[("psO", 0), ("rb", 0)], writes=["yy"])
                    P.op("dve", lambda e: e.tensor_tensor(out=yy[:], in0=yy[:], in1=tta[:], op=ALU.add), reads=["yy", "tta"], writes=["yy"])
                    P.op("dve", lambda e: e.tensor_tensor(out=ysq[:], in0=yy[:], in1=yy[:], op=ALU.mult), reads=["yy"], writes=["ysq"])
                    P.op("pe", lambda e: e.matmul(psF[0][:], lhsT=ones_f[:], rhs=ysq[:], start=True, stop=True), reads=["ysq", "ones_f"], writes=[("psF", 0)])
                    P.op("act", lambda e: e.activation(out=rstdb[:], in_=psF[0][:], func=AF.Ln, scale=1.0 / 128, bias=eps_col[:, 0:1]),
                         reads=[("psF", 0), "eps_col"], writes=["rstdb"])
                    P.op("act", lambda e: e.activation(out=rstdb[:], in_=rstdb[:], func=AF.Exp, scale=-0.5), reads=["rstdb"], writes=["rstdb"])
                    P.op("dve", lambda e: e.scalar_tensor_tensor(out=yo[yi][:], in0=yy[:], scalar=subln_col[:, 0:1], in1=rstdb[:], op0=ALU.mult, op1=ALU.mult),
                         reads=["yy", "rstdb", "subln_col"], writes=[("yo", yi)])
                    P.dma("sp", lambda e: e.dma_start(out=yaT_d[h][:, qb * 512:(qb + 1) * 512], in_=yo[yi][:]),
                          reads=[("yo", yi)], writes=[("yaT", h, qb)])
            P.barrier()
        if upto < 3:
            P.barrier(pool_ring=True)
            return nc, P

        with ExitStack() as ph:
            qT = [sb("bqT%d" % i, [128, S], BF16, ph) for i in range(2)]
            kT = [sb("bkT%d" % i, [128, NK], BF16, ph) for i in range(2)]
            vE = [sb("bvE%d" % i, [128, NKC, 129], BF16, ph) for i in range(2)]
            vO = [sb("bvO%d" % i, [128, NKC - 1, 129], BF16, ph) for i in range(2)]
            rpbt = sb("brpbt", [128, 8, 4, 64], F32, ph)
            nam = sb("bnam", [128, 4, 64], F32, ph)
            expB = [sb("bexpB%d" % i, [128, 8, 4, 64], BF16, ph) for i in range(2)]
            Pb = [sb("bPb%d" % i, [128, 6, 64], BF16, ph) for i in range(3)]
            rec = [sb("brec%d" % i, [128, 1], F32, ph) for i in range(2)]
            yb = [sb("byb%d" % i, [128, 128], BF16, ph) for i in range(2)]
            ybst = [sb("bybst%d" % i, [128, 2048], BF16, ph) for i in range(2)]
            psS = [ps("bpsS%d" % i, [128, 6, 64], F32, ph) for i in range(3)]
            accO = [ps("baccO%d" % i, [128, 129], F32, ph) for i in range(2)]
            pTr = ps("bpTr", [128, 128], BF16, ph)
            P.dma("sp", lambda e: e.dma_start(out=nam[:], in_=namask_d[:, :, :]), writes=["nam"])
            for i in range(2):
                P.op("dve", lambda e: e.memset(vE[i][:, :, 128:129], 1.0), writes=[("vE1", i)])
                P.op("dve", lambda e: e.memset(vO[i][:, :, 128:129], 1.0), writes=[("vO1", i)])
            step = 0
            nhB = lim.get("headsB", 8)

            def load_head_b(h):
                hi = h % 2
                P.dma("sp", lambda e: e.dma_start(out=qT[hi][:], in_=qbT_d[h]), writes=[("qT", hi)])
                P.dma("sp", lambda e: e.dma_start(out=kT[hi][:], in_=kbT_d[h]), writes=[("kT", hi)])
                for c0 in range(0, NKC, 22):
                    P.dma("sp", lambda e: e.dma_start(out=vE[hi][:, c0:c0 + 22, 0:128],
                                                      in_=vb_d[c0 * 128:(c0 + 22) * 128, h * 128:(h + 1) * 128].rearrange("(c p) d -> p c d", p=128)),
                          writes=[("vE", hi, c0)])
                for c0, n_ in ((0, 22), (22, 22), (44, 21)):
                    P.dma("sp", lambda e: e.dma_start(out=vO[hi][:, c0:c0 + n_, 0:128],
                                                      in_=vb_d[64 + c0 * 128:64 + (c0 + n_) * 128, h * 128:(h + 1) * 128].rearrange("(c p) d -> p c d", p=128)),
                          writes=[("vO", hi, c0)])

            npB = lim.get("pairsB", 64)
            stepsB = [(h, j, r2) for h in range(nhB) for j in range(npB) for r2 in range(2)]

            def rowinfo(j, r2):
                r = 2 * j + r2
                r_start = min(max(r - 4, 0), 120)
                return r, r_start, r - r_start, L + r_start * 64

            def qk_b(s):
                h, j, r2 = stepsB[s]
                hi = h % 2
                si = s % 3
                r, r_start, v, key0 = rowinfo(j, r2)
                for c in range(6):
                    ko = c * 128 if c < 2 else key0 + (c - 2) * 128
                    P.op("pe", lambda e: e.matmul(psS[si][:, c, :], lhsT=kT[hi][:, ko:ko + 128], rhs=qT[hi][:, r * 64:(r + 1) * 64],
                                                  start=True, stop=True, skip_group_check=True),
                         reads=[("kT", hi), ("qT", hi)], writes=[("psS", si)])

            def load_bias(h):
                hi = h % 2
                P.dma("sp", lambda e: e.dma_start(out=rpbt[:], in_=rpbx_d[h]), writes=["rpbt"])
                P.op("act", lambda e: e.activation(out=rpbt[:], in_=rpbt[:], func=AF.Exp), reads=["rpbt"], writes=["rpbt"])
                P.op("dve", lambda e: e.tensor_tensor(out=expB[hi][:], in0=rpbt[:], in1=nam[:].unsqueeze(1).to_broadcast([128, 8, 4, 64]), op=ALU.mult),
                     reads=["rpbt", "nam"], writes=[("expB", hi)])

            pendB = []
            load_head_b(0)
            load_bias(0)
            qk_b(0)
            for s, (h, j, r2) in enumerate(stepsB):
                hi = h % 2
                si = s % 3
                ai = j % 2
                vEk = [("vE", hi, c0) for c0 in range(0, NKC, 22)] + [("vE1", hi)]
                vOk = [("vO", hi, c0) for c0 in (0, 22, 44)] + [("vO1", hi)]
                if j == 8 and r2 == 0 and h + 1 < nhB:
                    load_head_b(h + 1)
                    load_bias(h + 1)
                r, r_start, v, key0 = rowinfo(j, r2)
                if s + 1 < len(stepsB):
                    qk_b(s + 1)
                P.op("act", lambda e: e.activation(out=Pb[si][:], in_=psS[si][:], func=AF.Exp), reads=[("psS", si)], writes=[("Pb", si)])
                P.op("dve", lambda e: e.tensor_tensor(out=Pb[si][:, 2:6, :], in0=Pb[si][:, 2:6, :], in1=expB[hi][:, v, :, :], op=ALU.mult),
                     reads=[("Pb", si), ("expB", hi)], writes=[("Pb", si)])
                for c in range(6):
                    if c < 2:
                        rhs = vE[hi][:, c, :]
                    elif r_start % 2 == 0:
                        rhs = vE[hi][:, 2 + r_start // 2 + (c - 2), :]
                    else:
                        rhs = vO[hi][:, (192 + r_start * 64) // 128 + (c - 2), :]
                    P.op("pe", lambda e: e.matmul(accO[ai][r2 * 64:(r2 + 1) * 64, :], lhsT=Pb[si][:, c, :], rhs=rhs, start=(c == 0), stop=(c == 5),
                                                  skip_group_check=True),
                         reads=[("Pb", si)] + vEk + vOk, writes=[("accO", ai)])
                while pendB:
                    pendB.pop(0)()
                if r2 == 0:
                    continue
                P.op("dve", lambda e: e.reciprocal(out=rec[ai][:], in_=accO[ai][:, 128:129]), reads=[("accO", ai)], writes=[("rec", ai)])
                P.op("dve", lambda e: e.tensor_scalar(out=yb[ai][:], in0=accO[ai][:, 0:128], scalar1=rec[ai][:, 0:1], scalar2=None, op0=ALU.mult),
                     reads=[("accO", ai), ("rec", ai)], writes=[("yb", ai)])

                def fin_b(ai=ai, j=j, h=h):
                    P.op("pe", lambda e: e.transpose(out=pTr[:], in_=yb[ai][:], identity=ident_b[:]), reads=[("yb", ai)], writes=["pTr"])
                    yi = (j // 16) % 2
                    P.op("act", lambda e: e.copy(out=ybst[yi][:, (j % 16) * 128:(j % 16 + 1) * 128], in_=pTr[:]), reads=["pTr"], writes=[("ybst", yi)])
                    if j % 16 == 15:
                        P.dma("sp", lambda e: e.dma_start(out=ybT_d[h][:, (j // 16) * 2048:(j // 16 + 1) * 2048], in_=ybst[yi][:]),
                              reads=[("ybst", yi)], writes=[("ybT", h, j // 16)])
                pendB.append(fin_b)
            while pendB:
                pendB.pop(0)()
            P.barrier()
        if upto < 4:
            P.barrier(pool_ring=True)
            return nc, P

        with ExitStack() as ph:
            wa = sb("cwa", [128, 8, D], BF16, ph)
            wb = sb("cwb", [128, 8, D], BF16, ph)
            P.dma("sp", lambda e: e.dma_start(out=wa[:], in_=wbf_a.rearrange("(k p) c -> p k c", p=128)),
                  reads=[("wbf_a", r_, 0) for r_ in range(2)], writes=["wa"])
            P.dma("sp", lambda e: e.dma_start(out=wb[:], in_=wbf_b.rearrange("(k p) c -> p k c", p=128)),
                  reads=[("wbf_b", r_, 0) for r_ in range(2)], writes=["wb"])
            yaTg = [sb("cya%d" % i, [128, 8, 512], BF16, ph) for i in range(2)]
            ybTg = [sb("cyb%d" % i, [128, 8, 512], BF16, ph) for i in range(2)]
            gt = [sb("cgt%d" % i, [128, 2, 512], BF16, ph) for i in range(4)]
            t1 = [sb("ct1%d" % i, [128, 512], F32, ph) for i in range(2)]
            t2 = [sb("ct2%d" % i, [128, 512], F32, ph) for i in range(2)]
            mst = [sb("cmst%d" % i, [128, 512], BF16, ph) for i in range(2)]
            psA = [ps("cpsA%d" % i, [128, 512], F32, ph) for i in range(2)]
            psB = [ps("cpsB%d" % i, [128, 512], F32, ph) for i in range(2)]
            n3 = 0
            for tg in range(lim.get("tg3a", S // 512)):
                gi = tg % 2
                tsl = slice(tg * 512, (tg + 1) * 512)
                P.dma("sp", lambda e: e.dma_start(out=yaTg[gi][:], in_=yaT_d[:, :, tsl].rearrange("h p t -> p h t")), writes=[("yaTg", gi)])
                P.dma("sp", lambda e: e.dma_start(out=ybTg[gi][:], in_=ybT_d[:, :, tsl].rearrange("h p t -> p h t")), writes=[("ybTg", gi)])
                for dc in range(KT):
                    pi = n3 % 2
                    g4 = n3 % 4
                    n3 += 1
                    P.dma("sp", lambda e: e.dma_start(out=gt[g4][:, 0, :], in_=gT_d[dc * 128:(dc + 1) * 128, tsl]), writes=[("gt", g4, 0)])
                    P.dma("sp", lambda e: e.dma_start(out=gt[g4][:, 1, :], in_=gT_d[D + dc * 128:D + (dc + 1) * 128, tsl]), writes=[("gt", g4, 1)])
                    for k in range(8):
                        P.op("pe", lambda e: e.matmul(psA[pi][:], lhsT=wa[:, k, dc * 128:(dc + 1) * 128], rhs=yaTg[gi][:, k, :], start=(k == 0), stop=(k == 7)),
                             reads=["wa", ("yaTg", gi)], writes=[("psA", pi)])
                    for k in range(8):
                        P.op("pe", lambda e: e.matmul(psB[pi][:], lhsT=wb[:, k, dc * 128:(dc + 1) * 128], rhs=ybTg[gi][:, k, :], start=(k == 0), stop=(k == 7)),
                             reads=["wb", ("ybTg", gi)], writes=[("psB", pi)])
                    P.op("dve", lambda e: e.tensor_tensor(out=t1[pi][:], in0=psA[pi][:], in1=gt[g4][:, 0, :], op=ALU.mult),
                         reads=[("psA", pi), ("gt", g4, 0)], writes=[("t1", pi)])
                    P.op("dve", lambda e: e.tensor_tensor(out=t2[pi][:], in0=psB[pi][:], in1=gt[g4][:, 1, :], op=ALU.mult),
                         reads=[("psB", pi), ("gt", g4, 1)], writes=[("t2", pi)])
                    P.op("dve", lambda e: e.tensor_tensor(out=mst[pi][:], in0=t1[pi][:], in1=t2[pi][:], op=ALU.add),
                         reads=[("t1", pi), ("t2", pi)], writes=[("mst", pi)])
                    P.dma("act", lambda e: e.dma_start(out=mT_d[dc][:, tsl], in_=mst[pi][:]), reads=[("mst", pi)], writes=[("mT", dc, tg)])
            P.barrier()

        with ExitStack() as ph:
            wo = sb("dwo", [128, KT, D], BF16, ph)
            P.dma("sp", lambda e: e.dma_start(out=wo[:], in_=wbf_o.rearrange("(k p) c -> p k c", p=128)),
                  reads=[("wbf_o", r_, 0) for r_ in range(4)], writes=["wo"])
            wr = sb("dwr", [128, KT, NE], F32, ph)
            P.dma("sp", lambda e: e.dma_start(out=wr[:], in_=wr_d.rearrange("(k p) e -> p k e", p=128)), writes=["wr"])
            rows = sb("drows", [128, 3, D], F32, ph)
            for j in range(3):
                P.dma("sp", lambda e: e.dma_start(out=rows[:, j, :], in_=modsave_d[j]), writes=[("rows", j)])
            mTt = [sb("dmT%d" % i, [128, KT, 128], BF16, ph) for i in range(2)]
            xt = [sb("dxt%d" % i, [128, D], F32, ph) for i in range(2)]
            xn = [sb("dxn%d" % i, [128, D], F32, ph) for i in range(2)]
            h2 = sb("dh2", [128, D], F32, ph)
            h2b = [sb("dh2b%d" % i, [128, D], BF16, ph) for i in range(2)]
            h2T = sb("dh2T", [128, KT, 128], F32, ph)
            junk = sb("djunk", [128, D], BF16, ph)
            sm = [sb("dsm%d" % i, [128, 8], F32, ph) for i in range(2)]
            ex = [sb("dex%d" % i, [128, NE], F32, ph) for i in range(2)]
            pmix = [ps("dpmix%d" % i, [128, 512], F32, ph) for i in range(2)]
            pTf = [ps("dpTf%d" % i, [128, 4, 128], F32, ph) for i in range(2)]
            pR = ps("dpR", [128, NE], F32, ph)
            n3 = 0
            ntf = 0
            for t in range(lim.get("t3b", NT)):
                i = t % 2
                rsl = slice(t * 128, (t + 1) * 128)
                P.dma("sp", lambda e: e.dma_start(out=mTt[i][:], in_=mT_d[:, :, rsl].rearrange("k p t -> p k t")), writes=[("mTt", i)])
                P.dma("sp", lambda e: e.dma_start(out=xt[i][:], in_=x_d[rsl, :]), writes=[("xt", i)])
                for cb in range(4):
                    pi = n3 % 2
                    n3 += 1
                    csl = slice(cb * 512, (cb + 1) * 512)
                    for k in range(KT):
                        P.op("pe", lambda e: e.matmul(pmix[pi][:], lhsT=mTt[i][:, k, :], rhs=wo[:, k, csl], start=(k == 0), stop=(k == KT - 1)),
                             reads=[("mTt", i), "wo"], writes=[("pmix", pi)])
                    P.op("dve", lambda e: e.tensor_tensor(out=xn[i][:, csl], in0=pmix[pi][:], in1=rows[:, 0, csl], op=ALU.mult),
                         reads=[("pmix", pi), ("rows", 0)], writes=[("xn", i, cb)])
                    P.op("dve", lambda e: e.tensor_tensor(out=xn[i][:, csl], in0=xn[i][:, csl], in1=xt[i][:, csl], op=ALU.add),
                         reads=[("xn", i, cb), ("xt", i)], writes=[("xn", i, cb)])
                xnk = [("xn", i, cb) for cb in range(4)]
                P.dma("act", lambda e: e.dma_start(out=out_d[rsl, :], in_=xn[i][:]), reads=xnk, writes=[("out", t)])
                P.op("act", lambda e: e.activation(out=junk[:], in_=xn[i][:], func=AF.Square, accum_out=sm[i][:, 0:1]), reads=xnk, writes=["junk", ("sm", i, 0)])
                P.op("act", lambda e: e.activation(out=sm[i][:, 1:2], in_=sm[i][:, 0:1], func=AF.Sqrt, scale=1.0 / D, bias=EPS),
                     reads=[("sm", i, 0)], writes=[("sm", i, 1)])
                P.op("dve", lambda e: e.reciprocal(out=sm[i][:, 1:2], in_=sm[i][:, 1:2]), reads=[("sm", i, 1)], writes=[("sm", i, 1)])
                P.op("dve", lambda e: e.scalar_tensor_tensor(out=h2[:], in0=xn[i][:], scalar=sm[i][:, 1:2], in1=rows[:, 2, :], op0=ALU.mult, op1=ALU.mult),
                     reads=xnk + [("sm", i, 1), ("rows", 2)], writes=["h2"])
                P.op("dve", lambda e: e.tensor_tensor(out=h2[:], in0=h2[:], in1=rows[:, 1, :], op=ALU.add), reads=["h2", ("rows", 1)], writes=["h2"])
                P.op("act", lambda e: e.copy(out=h2b[i][:], in_=h2[:]), reads=["h2"], writes=[("h2b", i)])
                P.dma("act", lambda e: e.dma_start(out=h2_d[rsl, :], in_=h2b[i][:]), reads=[("h2b", i)], writes=[("h2d", t)])
                for j4 in range(4):
                    ti = ntf % 2
                    ntf += 1
                    for jj in range(4):
                        k = j4 * 4 + jj
                        P.op("pe", lambda e: e.transpose(out=pTf[ti][:, jj, :], in_=h2[:, k * 128:(k + 1) * 128], identity=ident_f[:]),
                             reads=["h2"], writes=[("pTf", ti)])
                    if j4 % 2 == 0:
                        P.op("act", lambda e: e.copy(out=h2T[:, j4 * 4:(j4 + 1) * 4, :], in_=pTf[ti][:]), reads=[("pTf", ti)], writes=[("h2T", j4)])
                    else:
                        P.op("dve", lambda e: e.tensor_copy(out=h2T[:, j4 * 4:(j4 + 1) * 4, :], in_=pTf[ti][:]), reads=[("pTf", ti)], writes=[("h2T", j4)])
                for k in range(KT):
                    P.op("pe", lambda e: e.matmul(pR[:], lhsT=h2T[:, k, :], rhs=wr[:, k, :], start=(k == 0), stop=(k == KT - 1)),
                         reads=[("h2T", k // 4), "wr"], writes=["pR"])
                P.op("dve", lambda e: e.tensor_reduce(out=sm[i][:, 2:3], in_=pR[:], axis=AX.X, op=ALU.max), reads=["pR"], writes=[("sm", i, 2)])
                P.op("dve", lambda e: e.tensor_scalar(out=sm[i][:, 3:4], in0=sm[i][:, 2:3], scalar1=-1.0, scalar2=None, op0=ALU.mult),
                     reads=[("sm", i, 2)], writes=[("sm", i, 3)])
                P.op("act", lambda e: e.activation(out=ex[i][:], in_=pR[:], func=AF.Exp, bias=sm[i][:, 3:4], accum_out=sm[i][:, 4:5]),
                     reads=["pR", ("sm", i, 3)], writes=[("ex", i), ("sm", i, 4)])
                P.op("dve", lambda e: e.reciprocal(out=sm[i][:, 5:6], in_=sm[i][:, 4:5]), reads=[("sm", i, 4)], writes=[("sm", i, 5)])
                P.op("dve", lambda e: e.tensor_scalar(out=aff_all[:, t, :], in0=ex[i][:], scalar1=sm[i][:, 5:6], scalar2=None, op0=ALU.mult),
                     reads=[("ex", i), ("sm", i, 5)], writes=[("aff", t)])
            if debug:
                P.dma("sp", lambda e: e.dma_start(out=dbg_d[:, 2048:2048 + NT * NE], in_=aff_all[:].rearrange("p j e -> p (j e)")),
                      reads=[("aff", t_) for t_ in range(lim.get("t3b", NT))], writes=["dbg3"])
            P.barrier()
        if upto < 5:
            P.barrier(pool_ring=True)
            return nc, P

        with ExitStack() as ph:
            meta = sb("emeta", [128, NE, 8, 4], F32, ph)
            idx32 = sb("eidx32", [128, NE, 8], I32, ph)
            idx4 = sb("eidx4", [128, NE, 8, 4], I32, ph)
            with ExitStack() as ph4:
                lo = sb("elo", [128, NE], F32, ph4)
                mid = sb("emid", [128, NE], F32, ph4)
                cmpb = sb("ecmp", [128, NT, NE], BF16, ph4)
                cntp = sb("ecntp", [128, NE], F32, ph4)

                gsel = sb("egsel", [128, NE], F32, ph4)
                pC = ps("epC", [128, NE], F32, ph4)
                P.op("dve", lambda e: e.memset(lo[:], 0.0), writes=["lo"])
                for it in range(30):
                    ci = 2.0 ** -(it + 1)
                    P.op("dve", lambda e: e.tensor_scalar(out=mid[:], in0=lo[:], scalar1=ci, scalar2=None, op0=ALU.add), reads=["lo"], writes=["mid"])
                    P.op("dve", lambda e: e.tensor_tensor(out=cmpb[:], in0=aff_all[:], in1=mid[:].unsqueeze(1).to_broadcast([128, NT, NE]), op=ALU.is_ge),
                         reads=["mid"], writes=["cmpb"])
                    P.op("dve", lambda e: e.tensor_reduce(out=cntp[:], in_=cmpb[:].rearrange("p j e -> p e j"), axis=AX.X, op=ALU.add),
                         reads=["cmpb"], writes=["cntp"])
                    P.op("pe", lambda e: e.matmul(pC[:], lhsT=ones_f[:], rhs=cntp[:], start=True, stop=True), reads=["cntp", "ones_f"], writes=["pC"])
                    P.op("dve", lambda e: e.tensor_scalar(out=gsel[:], in0=pC[:], scalar1=CAP - 0.5, scalar2=ci, op0=ALU.is_ge, op1=ALU.mult),
                         reads=["pC"], writes=["gsel"])
                    P.op("dve", lambda e: e.tensor_tensor(out=lo[:], in0=lo[:], in1=gsel[:], op=ALU.add), reads=["lo", "gsel"], writes=["lo"])
                msk = sb("emsk", [128, NT, NE], F32, ph4)
                mskb = sb("emskb", [128, NT, NE], BF16, ph4)
                U = sb("eU", [128, 128], BF16, ph4)
                tot = sb("etot", [128, NE, NT], F32, ph4)
                incl = sb("eincl", [128, NE, NT], F32, ph4)
                ones64 = sb("eones64", [128, NT], F32, ph4)
                pos = sb("epos", [128, NT, NE], F32, ph4)
                posm = sb("eposm", [128, NT, NE], F32, ph4)
                vals = sb("evals", [128, NT, NE, 5], BF16, ph4)
                rres = sb("erres", [128, NT, NE], F32, ph4)
                iota_j = sb("eiotaj", [128, NT], F32, ph4)
                iota_s = sb("eiotas", [128, CAP], F32, ph4)
                oh = [sb("eoh%d" % i, [128, CAP], BF16, ph4) for i in range(3)]
                pP = [ps("epP%d" % i, [128, 512], F32, ph4) for i in range(2)]
                pTt = [ps("epTt%d" % i, [128, 512], F32, ph4) for i in range(2)]
                pM = [ps("epM%d" % i, [128, 8, 8, 8], F32, ph4) for i in range(2)]
                P.op("pool", lambda e: e.iota(iota_j[:], pattern=[[1, NT]], base=0, channel_multiplier=0, allow_small_or_imprecise_dtypes=True),
                     writes=["iota_j"])
                P.op("pool", lambda e: e.iota(iota_s[:], pattern=[[1, CAP]], base=0, channel_multiplier=0, allow_small_or_imprecise_dtypes=True),
                     writes=["iota_s"])
                P.op("dve", lambda e: e.tensor_tensor(out=msk[:], in0=aff_all[:], in1=lo[:].unsqueeze(1).to_broadcast([128, NT, NE]), op=ALU.is_ge),
                     reads=["lo"], writes=["msk"])
                P.op("dve", lambda e: e.tensor_copy(out=mskb[:], in_=msk[:]), reads=["msk"], writes=["mskb"])
                P.op("dve", lambda e: e.tensor_scalar(out=U[:], in0=iota_row[:], scalar1=iota_p[:, 0:1], scalar2=None, op0=ALU.is_gt), writes=["U"])
                P.op("dve", lambda e: e.memset(ones64[:], 1.0), writes=["ones64"])
                mflat = mskb[:].rearrange("p j e -> p (j e)")
                for hf in range(2):
                    P.op("pe", lambda e: e.matmul(pP[hf][:], lhsT=U[:], rhs=mflat[:, hf * 512:(hf + 1) * 512], start=True, stop=True),
                         reads=["U", "mskb"], writes=[("pP", hf)])
                    P.op("pe", lambda e: e.matmul(pTt[hf][:], lhsT=ones_b[:], rhs=mflat[:, hf * 512:(hf + 1) * 512], start=True, stop=True),
                         reads=["mskb"], writes=[("pTt", hf)])
                    P.op("dve", lambda e: e.tensor_copy(out=tot[:, :, hf * 32:(hf + 1) * 32], in_=pTt[hf][:].rearrange("p (j e) -> p e j", e=NE)),
                         reads=[("pTt", hf)], writes=[("tot", hf)])
                for e_ in range(NE):
                    P.op("dve", lambda e: e.tensor_tensor_scan(out=incl[:, e_, :], data0=ones64[:], data1=tot[:, e_, :], initial=0.0, op0=ALU.mult, op1=ALU.add),
                         reads=[("tot", 0), ("tot", 1), "ones64"], writes=[("incl", e_)])
                inck = [("incl", e_) for e_ in range(NE)]
                P.op("dve", lambda e: e.tensor_tensor(out=incl[:], in0=incl[:], in1=tot[:], op=ALU.subtract), reads=inck + [("tot", 0), ("tot", 1)], writes=inck)
                for hf in range(2):
                    P.op("dve", lambda e: e.tensor_tensor(out=pos[:, hf * 32:(hf + 1) * 32, :], in0=pP[hf][:].rearrange("p (j e) -> p j e", e=NE),
                                                          in1=incl[:, :, hf * 32:(hf + 1) * 32].rearrange("p e j -> p j e"), op=ALU.add),
                         reads=[("pP", hf)] + inck, writes=[("pos", hf)])
                P.op("dve", lambda e: e.scalar_tensor_tensor(out=posm[:], in0=pos[:], scalar=1.0, in1=msk[:], op0=ALU.add, op1=ALU.mult),
                     reads=[("pos", 0), ("pos", 1), "msk"], writes=["posm"])
                P.op("dve", lambda e: e.tensor_scalar(out=posm[:], in0=posm[:], scalar1=-1.0, scalar2=None, op0=ALU.add), reads=["posm"], writes=["posm"])
                P.op("dve", lambda e: e.tensor_copy(out=vals[:, :, :, 0], in_=aff_all[:]), writes=["v0"])
                P.op("dve", lambda e: e.tensor_tensor(out=rres[:], in0=aff_all[:], in1=vals[:, :, :, 0], op=ALU.subtract), reads=["v0"], writes=["rres"])
                P.op("dve", lambda e: e.tensor_copy(out=vals[:, :, :, 1], in_=rres[:]), reads=["rres"], writes=["v1"])
                P.op("dve", lambda e: e.tensor_tensor(out=rres[:], in0=rres[:], in1=vals[:, :, :, 1], op=ALU.subtract), reads=["rres", "v1"], writes=["rres"])
                P.op("dve", lambda e: e.tensor_copy(out=vals[:, :, :, 2], in_=rres[:]), reads=["rres"], writes=["v2"])
                P.op("dve", lambda e: e.tensor_copy(out=vals[:, :, :, 3], in_=iota_p[:, 0:1].unsqueeze(2).to_broadcast([128, NT, NE])), writes=["v3"])
                P.op("dve", lambda e: e.tensor_copy(out=vals[:, :, :, 4], in_=iota_j[:].unsqueeze(2).to_broadcast([128, NT, NE])),
                     reads=["iota_j"], writes=["v4"])
                if debug:
                    P.dma("sp", lambda e: e.dma_start(out=dbg_d[:, 3072:3072 + NT * NE], in_=posm[:].rearrange("p j e -> p (j e)")),
                          reads=["posm"], writes=["dbg4"])
                    P.dma("sp", lambda e: e.dma_start(out=dbg_d[:, 1100:1100 + NE], in_=lo[:]), reads=["lo"], writes=["dbg5"])
                noh = 0
                for e_ in range(NE):
                    for j in range(NT):
                        oi = noh % 3
                        noh += 1
                        P.op("dve", lambda e: e.tensor_scalar(out=oh[oi][:], in0=iota_s[:], scalar1=posm[:, j, e_:e_ + 1], scalar2=None, op0=ALU.is_equal),
                             reads=["iota_s", "posm"], writes=[("oh", oi)])
                        for st in range(8):
                            P.op("pe", lambda e: e.matmul(pM[e_ // 8][:, e_ % 8, st, 0:5], lhsT=oh[oi][:, st * 128:(st + 1) * 128], rhs=vals[:, j, e_, :],
                                                          start=(e_ % 8 == 0 and j == 0 and st == 0), stop=(j == NT - 1), skip_group_check=True),
                                 reads=[("oh", oi), "v0", "v1", "v2", "v3", "v4"], writes=[("pM", e_ // 8)])
                for hf in range(2):
                    esl = slice(hf * 8, hf * 8 + 8)
                    P.op("dve", lambda e: e.tensor_tensor(out=meta[:, esl, :, 0], in0=pM[hf][:, :, :, 0], in1=pM[hf][:, :, :, 1], op=ALU.add) if False else
                         e.tensor_copy(out=meta[:, esl, :, 0:3], in_=pM[hf][:, :, :, 2:5]), reads=[("pM", hf)], writes=[("metaA", hf)])
                    P.op("dve", lambda e: e.tensor_tensor(out=meta[:, esl, :, 0], in0=meta[:, esl, :, 0], in1=pM[hf][:, :, :, 1], op=ALU.add),
                         reads=[("pM", hf), ("metaA", hf)], writes=[("metaA", hf)])
                    P.op("dve", lambda e: e.tensor_tensor(out=meta[:, esl, :, 0], in0=meta[:, esl, :, 0], in1=pM[hf][:, :, :, 0], op=ALU.add),
                         reads=[("pM", hf), ("metaA", hf)], writes=[("metaA", hf)])
                P.op("dve", lambda e: e.tensor_copy(out=meta[:, :, :, 3:4], in_=meta[:, :, :, 3:4]), reads=[("metaA", 0), ("metaA", 1)], writes=["meta"])
                tokf = sb("etokf", [128, NE, 8], F32, ph4)
                tok4 = sb("etok4", [128, NE, 8, 4], F32, ph4)
                P.op("dve", lambda e: e.scalar_tensor_tensor(out=tokf[:], in0=meta[:, :, :, 2], scalar=128.0, in1=meta[:, :, :, 1], op0=ALU.mult, op1=ALU.add),
                     reads=["meta"], writes=["tokf"])
                P.op("dve", lambda e: e.tensor_copy(out=idx32[:], in_=tokf[:]), reads=["tokf"], writes=["idx32"])
                for db in range(4):
                    P.op("dve", lambda e: e.tensor_scalar(out=tok4[:, :, :, db], in0=tokf[:], scalar1=4.0, scalar2=float(db), op0=ALU.mult, op1=ALU.add),
                         reads=["tokf"], writes=[("tok4", db)])
                P.op("dve", lambda e: e.tensor_copy(out=idx4[:], in_=tok4[:]), reads=[("tok4", db) for db in range(4)], writes=["idx4"])
                if debug:
                    P.dma("sp", lambda e: e.dma_start(out=dbg_d[:, 1200:1200 + NE * 8 * 4], in_=meta[:].rearrange("p a b c -> p (a b c)")),
                          reads=["meta"], writes=["dbg6"])
                P.barrier()
            with ExitStack() as ph5:
                ga2row = sb("fga2", [128, D], F32, ph5)
                P.dma("sp", lambda e: e.dma_start(out=ga2row[:], in_=modsave_d[3]), writes=["ga2row"])
                xeT = sb("fxeT", [128, KT, CAP], BF16, ph5)
                actT = sb("factT", [128, KT, CAP], BF16, ph5)
                wring = [sb("fw%d" % i, [128, KT, 512], BF16, ph5) for i in range(4)]
                xg = [sb("fxg%d" % i, [128, D], BF16, ph5) for i in range(2)]
                sa = [sb("fsa%d" % i, [128, 512], F32, ph5) for i in range(2)]
                ysc = [sb("fysc%d" % i, [128, 512], F32, ph5) for i in range(4)]
                psA = [ps("fpsA%d" % i, [128, 512], F32, ph5) for i in range(2)]
                psU = [ps("fpsU%d" % i, [128, 512], F32, ph5) for i in range(2)]
                psY = [ps("fpsY%d" % i, [128, 512], F32, ph5) for i in range(2)]
                pTx = [ps("fpTx%d" % i, [128, 4, 128], BF16, ph5) for i in range(2)]
                out4 = out_d.rearrange("t (q c) -> (t q) c", c=512)
                c5 = {"w": 0, "xg": 0, "tx": 0, "au": 0, "y": 0, "ysc": 0}
                nexp = lim.get("experts", NE)

                def load_w(srcw, keyname, e_, blk):
                    wi = c5["w"] % 4
                    c5["w"] += 1
                    P.dma("sp", lambda e: e.dma_start(out=wring[wi][:], in_=srcw[e_][:, blk * 512:(blk + 1) * 512].rearrange("(k p) c -> p k c", p=128)),
                          reads=[((keyname, e_), r_, 0) for r_ in range(4)], writes=[("wring", wi)])
                    return wi

                def gather(e_):
                    for st in range(8):
                        gi = c5["xg"] % 2
                        c5["xg"] += 1
                        P.dma("pool", lambda e: e.indirect_dma_start(out=xg[gi][:], out_offset=None, in_=h2_d[:, :],
                                                                     in_offset=bass.IndirectOffsetOnAxis(ap=idx32[:, e_, st:st + 1], axis=0)),
                              reads=["idx32"], writes=[("xg", gi)])
                        for j4 in range(4):
                            ti = c5["tx"] % 2
                            c5["tx"] += 1
                            for jj in range(4):
                                k = j4 * 4 + jj
                                P.op("pe", lambda e: e.transpose(out=pTx[ti][:, jj, :], in_=xg[gi][:, k * 128:(k + 1) * 128], identity=ident_b[:]),
                                     reads=[("xg", gi)], writes=[("pTx", ti)])
                            if j4 % 2 == 0:
                                P.op("act", lambda e: e.copy(out=xeT[:, j4 * 4:(j4 + 1) * 4, st * 128:(st + 1) * 128], in_=pTx[ti][:]),
                                     reads=[("pTx", ti)], writes=[("xeT", st)])
                            else:
                                P.op("dve", lambda e: e.tensor_copy(out=xeT[:, j4 * 4:(j4 + 1) * 4, st * 128:(st + 1) * 128], in_=pTx[ti][:]),
                                     reads=[("pTx", ti)], writes=[("xeT", st)])

                prev_sc = []
                gather(0)
                for e_ in range(nexp):
                    for fb in range(4):
                        wg = load_w(wbf_eg, "wbf_eg", e_, fb)
                        wu = load_w(wbf_eu, "wbf_eu", e_, fb)
                        for fc in range(4):
                            for sh in range(2):
                                ai = c5["au"] % 2
                                c5["au"] += 1
                                xk = [("xeT", s_) for s_ in range(sh * 4, sh * 4 + 4)]
                                for k in range(KT):
                                    P.op("pe", lambda e: e.matmul(psA[ai][:], lhsT=wring[wg][:, k, fc * 128:(fc + 1) * 128], rhs=xeT[:, k, sh * 512:(sh + 1) * 512],
                                                                  start=(k == 0), stop=(k == KT - 1)), reads=[("wring", wg)] + xk, writes=[("psA", ai)])
                                for k in range(KT):
                                    P.op("pe", lambda e: e.matmul(psU[ai][:], lhsT=wring[wu][:, k, fc * 128:(fc + 1) * 128], rhs=xeT[:, k, sh * 512:(sh + 1) * 512],
                                                                  start=(k == 0), stop=(k == KT - 1)), reads=[("wring", wu)] + xk, writes=[("psU", ai)])
                                P.op("act", lambda e: e.activation(out=sa[ai][:], in_=psA[ai][:], func=AF.Silu), reads=[("psA", ai)], writes=[("sa", ai)])
                                P.op("dve", lambda e: e.tensor_tensor(out=actT[:, fb * 4 + fc, sh * 512:(sh + 1) * 512], in0=psU[ai][:], in1=sa[ai][:], op=ALU.mult),
                                     reads=[("psU", ai), ("sa", ai)], writes=[("actT", fb * 4 + fc, sh)])
                    if e_ + 1 < nexp:
                        gather(e_ + 1)
                    cur_sc = []
                    ak = [("actT", f_, s_) for f_ in range(KT) for s_ in range(2)]
                    for db in range(4):
                        wd = load_w(wbf_ed, "wbf_ed", e_, db)
                        for st in range(8):
                            yi = c5["y"] % 2
                            c5["y"] += 1
                            for k in range(KT):
                                P.op("pe", lambda e: e.matmul(psY[yi][:], lhsT=actT[:, k, st * 128:(st + 1) * 128], rhs=wring[wd][:, k, :],
                                                              start=(k == 0), stop=(k == KT - 1)),
                                     reads=[("wring", wd)] + [("actT", f_, st // 4) for f_ in range(KT)], writes=[("psY", yi)])
                            si = c5["ysc"] % 4
                            c5["ysc"] += 1
                            P.op("dve", lambda e: e.scalar_tensor_tensor(out=ysc[si][:], in0=psY[yi][:], scalar=meta[:, e_, st, 0:1],
                                                                         in1=ga2row[:, db * 512:(db + 1) * 512], op0=ALU.mult, op1=ALU.mult),
                                 reads=[("psY", yi), "meta", "ga2row"], writes=[("ysc", si)])
                            for ev in prev_sc:
                                P.wait("pool", ev)
                            ev = P.dma("pool", lambda e: e.indirect_dma_start(out=out4[:, :],
                                                                             out_offset=bass.IndirectOffsetOnAxis(ap=idx4[:, e_, st, db:db + 1], axis=0),
                                                                             in_=ysc[si][:], in_offset=None, compute_op=ALU.add),
                                       reads=[("ysc", si), "idx4"], writes=[])
                            cur_sc.append(ev)
                    prev_sc = cur_sc
                P.barrier(pool_ring=True)
        P.barrier(pool_ring=True)
        return nc, P


_CONST_CACHE = {}


def _prep_inputs(inputs):
    if "c" not in _CONST_CACHE:
        _CONST_CACHE["c"] = _consts()
    rope, namask = _CONST_CACHE["c"]
    f = lambda a: np.ascontiguousarray(np.asarray(a, dtype=np.float32))
    x = f(inputs["x"]); ctx = f(inputs["ctx"]); c = f(inputs["c"]); c_ctx = f(inputs["c_ctx"])
    smallp = np.concatenate([f(inputs[k])[0] for k in ("q_gain_a", "k_gain_a", "lam_q1", "lam_k1", "lam_q2", "lam_k2",
                                                       "subln_gain", "q_gain_b", "k_gain_b")]).reshape(1, 768)
    shared = {
        "w_mod": f(inputs["w_mod"])[0], "b_mod": f(inputs["b_mod"])[0].reshape(1, -1),
        "g12": np.stack([f(inputs["g_norm1"])[0], f(inputs["g_norm2"])[0]]),
        "w_in": f(inputs["w_in"])[0], "smallp": smallp,
        "rpbx": _rpb_expand(f(inputs["rel_pos_bias"])[0]), "namask": namask, "rope": rope,
        "w_a": f(inputs["w_branch_a"])[0], "w_b": f(inputs["w_branch_b"])[0], "w_o": f(inputs["w_out"])[0],
        "w_r": f(inputs["w_router"])[0], "w_eg": f(inputs["w_exp_gate"])[0], "w_eu": f(inputs["w_exp_up"])[0],
        "w_ed": f(inputs["w_exp_down"])[0],
    }
    maps = []
    for core in range(N_CORES):
        b = core % 4
        cc = np.stack([c[b].reshape(KT, 128).T, c_ctx.reshape(KT, 128).T], axis=-1)
        m = dict(shared)
        m.update({"x": x[b], "ctx": ctx[b], "cc": np.ascontiguousarray(cc)})
        maps.append(m)
    return maps


def kernel(**inputs):
    maps = _prep_inputs(inputs)
    nc, _ = build()
    res = run_bass_kernel_spmd(nc, maps, core_ids=list(range(N_CORES)))
    out = np.stack([res.results[b]["out"] for b in range(4)], axis=0)
    return out.astype(np.float32)
```

```python
import math
from contextlib import ExitStack

import numpy as np
import concourse.bass as bass
import concourse.mybir as mybir
from concourse.bass_utils import run_bass_kernel_spmd

F32 = mybir.dt.float32
BF16 = mybir.dt.bfloat16
I32 = mybir.dt.int32
AF = mybir.ActivationFunctionType
ALU = mybir.AluOpType
AX = mybir.AxisListType

D = 2048
S = 8192
L = 256
NK = S + L
NT = S // 128
KT = D // 128
GRID_W = 64
PROJ = 10240
OFF_QA, OFF_QB, OFF_GATE, OFF_KA, OFF_VA, OFF_KB, OFF_VB = 0, 1024, 2048, 6144, 7168, 8192, 9216
NE = 16
CAP = 1024
EPS = 1e-6
LAM_INIT = 0.8 - 0.6 * math.exp(0.0)
N_CORES = 8


class Ev:
    __slots__ = ("sem", "name", "val")

    def __init__(self, sem, name, val):
        self.sem, self.name, self.val = sem, name, val


class Prog:
    def __init__(self, nc, es):
        self.nc = nc
        self.eng = {"pe": nc.tensor, "act": nc.scalar, "dve": nc.vector, "pool": nc.gpsimd, "sp": nc.sync}
        self.esem = {}
        self.ecnt = {}
        for e in ("pe", "act", "dve", "pool"):
            self.esem[e] = es.enter_context(nc.semaphore("s_" + e))
            self.ecnt[e] = 0
        self.rings = {}
        self.rpos = {}
        for e, n in (("sp", 12), ("pool", 12), ("act", 8)):
            self.rings[e] = [[es.enter_context(nc.semaphore("d_%s%d" % (e, i))), "d_%s%d" % (e, i), 0] for i in range(n)]
            self.rpos[e] = 0
        self.waited = {e: {} for e in self.eng}
        self.lastw = {}
        self.readers = {}
        self.nins = 0

    def wait(self, eng, ev):
        if ev is None:
            return
        if eng == "pe" and ev.name == "s_pe":
            return
        w = self.waited[eng]
        if w.get(ev.name, 0) >= ev.val:
            return
        self.eng[eng].wait_ge(ev.sem, ev.val)
        w[ev.name] = ev.val
        self.nins += 1

    def _hazards(self, eng, reads, writes):
        for k in reads:
            self.wait(eng, self.lastw.get(k))
        for k in writes:
            self.wait(eng, self.lastw.get(k))
            rd = self.readers.get(k)
            if rd:
                for ev in rd.values():
                    self.wait(eng, ev)

    def _record(self, ev, reads, writes):
        for k in reads:
            self.readers.setdefault(k, {})[ev.name] = ev
        for k in writes:
            self.lastw[k] = ev
            self.readers[k] = {}

    def op(self, eng, fn, reads=(), writes=()):
        self._hazards(eng, reads, writes)
        ins = fn(self.eng[eng])
        self.ecnt[eng] += 1
        ins.then_inc(self.esem[eng], 1)
        ev = Ev(self.esem[eng], "s_" + eng, self.ecnt[eng])
        self._record(ev, reads, writes)
        self.nins += 1
        return ev

    def dma(self, eng, fn, reads=(), writes=()):
        ring = self.rings[eng]
        i = self.rpos[eng]
        self.rpos[eng] = (i + 1) % len(ring)
        sem, name, cnt = ring[i]
        if cnt:
            self.wait(eng, Ev(sem, name, cnt))
        self._hazards(eng, reads, writes)
        ins = fn(self.eng[eng])
        ins.then_inc(sem, 16)
        ring[i][2] = cnt + 16
        ev = Ev(sem, name, cnt + 16)
        self._record(ev, reads, writes)
        self.nins += 1
        return ev

    def barrier(self, pool_ring=False):
        evs = [Ev(self.esem[e], "s_" + e, self.ecnt[e]) for e in self.esem if self.ecnt[e]]
        for e in self.rings:
            if e == "pool" and not pool_ring:
                continue
            for sem, name, cnt in self.rings[e]:
                if cnt:
                    evs.append(Ev(sem, name, cnt))
        for e in self.eng:
            for ev in evs:
                self.wait(e, ev)
        keep = {k: v for k, v in self.lastw.items() if "wbf" in repr(k)}
        self.lastw = keep
        self.readers = {}


def _consts():
    t = np.arange(S)
    row = (t // GRID_W).astype(np.float32)
    col = (t % GRID_W).astype(np.float32)
    half = 32
    inv_freq = (10000.0 ** (-np.arange(0, half, 2, dtype=np.float32) / half)).astype(np.float32)

    def tab(pos):
        ang = pos[:, None] * inv_freq[None, :]
        ang = np.concatenate([ang, ang], axis=-1)
        return np.cos(ang), np.sin(ang)

    cr, sr = tab(row)
    cc, sc = tab(col)
    cos = np.concatenate([cr, cc], -1).astype(np.float32)
    sin = np.concatenate([sr, sc], -1).astype(np.float32)
    ss = sin.reshape(S, 2, 2, 16).copy()
    ss[:, :, 0, :] *= -1.0
    ss = ss.reshape(S, 64)
    rope = np.stack([cos, ss], axis=1)
    rope = np.ascontiguousarray(rope.reshape(NT, 128, 2, 64).transpose(1, 0, 2, 3))
    q = np.arange(64)
    cs = np.clip(q - 8, 0, 48)
    wk = np.arange(64)
    inwin = (wk[None, :] >= cs[:, None]) & (wk[None, :] < cs[:, None] + 16)
    m = np.zeros((128, 4, 64), np.float32)
    for i in range(8):
        m[(i % 2) * 64:(i % 2) * 64 + 64, i // 2, :] = inwin.T.astype(np.float32)
    return rope.astype(np.float32), m


def _rpb_expand(rpb):
    H = rpb.shape[0]
    q = np.arange(64)
    wk = np.arange(64)
    idx_c = np.clip(wk[:, None] - q[None, :] + 15, 0, 30)
    out = np.zeros((H, 128, 8, 4, 64), np.float32)
    for v in range(8):
        for i in range(8):
            idx_r = 7 - v + i
            out[:, (i % 2) * 64:(i % 2) * 64 + 64, v, i // 2, :] = rpb[:, idx_r][:, idx_c]
    return out


def build(debug=False, upto=99, lim=None):
    lim = lim or {}
    nc = bass.Bass("TRN2", target_bir_lowering=False)

    def din(name, shape, dt=F32):
        return nc.dram_tensor(name, list(shape), dt, kind="ExternalInput").ap()

    def dscr(name, shape, dt, dbg=False):
        kind = "ExternalOutput" if (debug and dbg) else "Internal"
        return nc.dram_tensor(name, list(shape), dt, kind=kind).ap()

    x_d = din("x", [S, D])
    ctx_d = din("ctx", [L, D])
    cc_d = din("cc", [128, KT, 2])
    wmod_d = din("w_mod", [D, 6 * D])
    bmod_d = din("b_mod", [1, 6 * D])
    g12_d = din("g12", [2, D])
    win_d = din("w_in", [D, PROJ])
    smallp_d = din("smallp", [1, 768])
    rpbx_d = din("rpbx", [8, 128, 8, 4, 64])
    namask_d = din("namask", [128, 4, 64])
    rope_d = din("rope", [128, NT, 2, 64])
    wa_d = din("w_a", [1024, D])
    wb_d = din("w_b", [1024, D])
    wo_d = din("w_o", [D, D])
    wr_d = din("w_r", [D, NE])
    weg_d = din("w_eg", [NE, D, D])
    weu_d = din("w_eu", [NE, D, D])
    wed_d = din("w_ed", [NE, D, D])
    out_d = nc.dram_tensor("out", [S, D], F32, kind="ExternalOutput").ap()

    qaT_d = dscr("qaT", [8, 128, S], BF16, True)
    kaT_d = dscr("kaT", [8, 128, NK], BF16, True)
    va_d = dscr("va", [NK, 1024], BF16, True)
    qbT_d = dscr("qbT", [8, 128, S], BF16, True)
    kbT_d = dscr("kbT", [8, 128, NK], BF16, True)
    vb_d = dscr("vb", [NK, 1024], BF16, True)
    gT_d = dscr("gT", [4096, S], BF16, True)
    yaT_d = dscr("yaT", [8, 128, S], BF16, True)
    ybT_d = dscr("ybT", [8, 128, S], BF16, True)
    mT_d = dscr("mT", [KT, 128, S], BF16, True)
    h2_d = dscr("h2", [S, D], BF16, True)
    modsave_d = dscr("modsave", [4, 128, D], F32, True)
    dbg_d = dscr("dbg", [128, 4096], F32, True)
    wbf_in = dscr("wbf_in", [D, PROJ], BF16)
    wbf_a = dscr("wbf_a", [1024, D], BF16)
    wbf_b = dscr("wbf_b", [1024, D], BF16)
    wbf_o = dscr("wbf_o", [D, D], BF16)
    wbf_eg = dscr("wbf_eg", [NE, D, D], BF16)
    wbf_eu = dscr("wbf_eu", [NE, D, D], BF16)
    wbf_ed = dscr("wbf_ed", [NE, D, D], BF16)

    with ExitStack() as es:
        P = Prog(nc, es)

        uniq = [0]

        def sb(name, shape, dt, stack=es):
            uniq[0] += 1
            return stack.enter_context(nc.sbuf_tensor("sb%d_%s" % (uniq[0], name), list(shape), dt))

        def ps(name, shape, dt, stack=es):
            uniq[0] += 1
            return stack.enter_context(nc.psum_tensor("ps%d_%s" % (uniq[0], name), list(shape), dt))

        ident_f = sb("ident_f", [128, 128], F32)
        ident_b = sb("ident_b", [128, 128], BF16)
        ones_b = sb("ones_b", [128, 128], BF16)
        rstd_all = sb("rstd_all", [128, NT + 2], F32)
        aff_all = sb("aff_all", [128, NT, NE], F32)
        neg_lam = sb("neg_lam", [128, 1], F32)
        gains = sb("gains", [128, 768], F32)
        iota_p = sb("iota_p", [128, 1], F32)
        ones_f = sb("ones_f", [128, 128], F32)
        subln_col = sb("subln_col", [128, 1], F32)
        eps_col = sb("eps_col", [128, 1], F32)
        iota_row = sb("iota_row", [128, 128], F32)

        def convert(src, dst, rows, cols, key):
            for r0 in range(0, rows, 512):
                for c0 in range(0, cols, 2048):
                    P.dma("pool", lambda e, r0=r0, c0=c0: e.dma_start(
                        out=dst[r0:r0 + 512, c0:c0 + 2048], in_=src[r0:r0 + 512, c0:c0 + 2048]),
                        writes=[(key, r0 // 512, c0 // 2048)])

        ph01 = es.enter_context(ExitStack())
        gs1row = sb("gs1row", [128, 2, D], F32, ph01)
        sh1rep = sb("sh1rep", [128, 2, KT, 128], BF16, ph01)
        with ExitStack() as ph:
            P.op("pool", lambda e: e.iota(iota_row[:], pattern=[[1, 128]], base=0, channel_multiplier=0,
                                          allow_small_or_imprecise_dtypes=True), writes=["iota_row"])
            P.op("pool", lambda e: e.iota(iota_p[:], pattern=[[0, 1]], base=0, channel_multiplier=1,
                                          allow_small_or_imprecise_dtypes=True), writes=["iota_p"])
            convert(win_d, wbf_in, D, PROJ, "wbf_in")
            convert(wa_d, wbf_a, 1024, D, "wbf_a")
            convert(wb_d, wbf_b, 1024, D, "wbf_b")
            convert(wo_d, wbf_o, D, D, "wbf_o")
            for e_ in range(NE if upto >= 4 else 0):
                convert(weg_d[e_], wbf_eg[e_], D, D, ("wbf_eg", e_))
                convert(weu_d[e_], wbf_eu[e_], D, D, ("wbf_eu", e_))
                convert(wed_d[e_], wbf_ed[e_], D, D, ("wbf_ed", e_))

            P.op("dve", lambda e: e.tensor_scalar(out=ident_f[:], in0=iota_row[:], scalar1=iota_p[:, 0:1], scalar2=None,
                                                  op0=ALU.is_equal), reads=["iota_row", "iota_p"], writes=["ident_f"])
            P.op("dve", lambda e: e.tensor_copy(out=ident_b[:], in_=ident_f[:]), reads=["ident_f"], writes=["ident_b"])
            P.op("dve", lambda e: e.memset(ones_b[:], 1.0), writes=["ones_b"])
            P.op("dve", lambda e: e.memset(ones_f[:], 1.0), writes=["ones_f"])
            P.op("dve", lambda e: e.memset(eps_col[:], EPS), writes=["eps_col"])

            cc = sb("cc", [128, KT, 2], F32, ph)
            csl = sb("csl", [128, KT, 2], F32, ph)
            crep = sb("crep", [128, KT, 2, 128], F32, ph)
            P.dma("sp", lambda e: e.dma_start(out=cc[:], in_=cc_d[:, :, :]), writes=["cc"])
            P.op("act", lambda e: e.activation(out=csl[:], in_=cc[:], func=AF.Silu), reads=["cc"], writes=["csl"])
            P.op("dve", lambda e: e.tensor_copy(out=crep[:], in_=csl[:].unsqueeze(3).to_broadcast([128, KT, 2, 128])),
                 reads=["csl"], writes=["crep"])
            modrow = sb("modrow", [128, 6 * D], F32, ph)
            modrow_c = sb("modrow_c", [128, 2 * D], F32, ph)
            MB = 512
            wmb = [sb("wmb%d" % i, [128, KT, MB], F32, ph) for i in range(2)]
            bmb = [sb("bmb%d" % i, [128, MB], F32, ph) for i in range(2)]
            psm = [ps("psm%d" % i, [128, MB], F32, ph) for i in range(4)]
            npm = 0
            nblk = 6 * D // MB
            for blk in range(nblk):
                i = blk % 2
                P.dma("sp", lambda e: e.dma_start(out=wmb[i][:], in_=wmod_d[:, blk * MB:(blk + 1) * MB].rearrange("(k p) c -> p k c", p=128)),
                      writes=[("wmb", i)])
                P.dma("sp", lambda e: e.dma_start(out=bmb[i][:], in_=bmod_d[0:1, blk * MB:(blk + 1) * MB].partition_broadcast(128)),
                      writes=[("bmb", i)])
                for j in range(2 if blk < 2 * D // MB else 1):
                    pt = psm[npm % 4]
                    pk = ("psm", npm % 4)
                    npm += 1
                    for k in range(KT):
                        P.op("pe", lambda e: e.matmul(pt[:], lhsT=crep[:, k, j, :], rhs=wmb[i][:, k, :], start=(k == 0), stop=(k == KT - 1)),
                             reads=["crep", ("wmb", i)], writes=[pk])
                    dst = modrow if j == 0 else modrow_c
                    P.op("dve", lambda e: e.tensor_tensor(out=dst[:, blk * MB:(blk + 1) * MB], in0=pt[:], in1=bmb[i][:], op=ALU.add),
                         reads=[pk, ("bmb", i)], writes=[("modrow", j, blk)])
            g12 = sb("g12", [128, 2, D], F32, ph)
            P.dma("sp", lambda e: e.dma_start(out=g12[:, 0, :], in_=g12_d[0:1, :].partition_broadcast(128)), writes=["g12a"])
            P.dma("sp", lambda e: e.dma_start(out=g12[:, 1, :], in_=g12_d[1:2, :].partition_broadcast(128)), writes=["g12b"])
            mr_all = [("modrow", 0, b_) for b_ in range(nblk)]
            mrc_all = [("modrow", 1, b_) for b_ in range(2 * D // MB)]
            P.op("dve", lambda e: e.scalar_tensor_tensor(out=gs1row[:, 0, :], in0=modrow[:, D:2 * D], scalar=1.0, in1=g12[:, 0, :],
                                                         op0=ALU.add, op1=ALU.mult), reads=mr_all + ["g12a"], writes=["gs1row0"])
            P.op("dve", lambda e: e.scalar_tensor_tensor(out=gs1row[:, 1, :], in0=modrow_c[:, D:2 * D], scalar=1.0, in1=g12[:, 0, :],
                                                         op0=ALU.add, op1=ALU.mult), reads=mrc_all + ["g12a"], writes=["gs1row1"])
            P.op("dve", lambda e: e.scalar_tensor_tensor(out=modrow[:, 4 * D:5 * D], in0=modrow[:, 4 * D:5 * D], scalar=1.0, in1=g12[:, 1, :],
                                                         op0=ALU.add, op1=ALU.mult), reads=mr_all + ["g12b"], writes=mr_all)
            for j in range(4):
                P.dma("sp", lambda e: e.dma_start(out=modsave_d[j], in_=modrow[:, (2 + j) * D:(3 + j) * D]), reads=mr_all, writes=[("modsave", j)])
            dtmp = sb("dtmp", [128, KT, 128], F32, ph)
            shc = sb("shc", [128, 2, KT], F32, ph)
            for j in range(2):
                src = modrow if j == 0 else modrow_c
                P.op("dve", lambda e: e.tensor_tensor(out=dtmp[:], in0=src[:, 0:D].rearrange("p (k m) -> p k m", m=128),
                                                      in1=ident_f[:].unsqueeze(1).to_broadcast([128, KT, 128]), op=ALU.mult),
                     reads=(mr_all if j == 0 else mrc_all) + ["ident_f"], writes=["dtmp"])
                P.op("dve", lambda e: e.tensor_reduce(out=shc[:, j, :], in_=dtmp[:], axis=AX.X, op=ALU.add), reads=["dtmp"], writes=[("shc", j)])
                P.op("dve", lambda e: e.tensor_copy(out=sh1rep[:, j, :, :], in_=shc[:, j, :].unsqueeze(2).to_broadcast([128, KT, 128])),
                     reads=[("shc", j)], writes=[("sh1rep", j)])
            P.dma("sp", lambda e: e.dma_start(out=gains[:], in_=smallp_d[0:1, :].partition_broadcast(128)), writes=["gains"])
            lt = sb("lt", [128, 2, 64], F32, ph)
            ls = sb("ls", [128, 4], F32, ph)
            P.op("dve", lambda e: e.tensor_tensor(out=lt[:, 0, :], in0=gains[:, 128:192], in1=gains[:, 192:256], op=ALU.mult), reads=["gains"], writes=["lt0"])
            P.op("dve", lambda e: e.tensor_tensor(out=lt[:, 1, :], in0=gains[:, 256:320], in1=gains[:, 320:384], op=ALU.mult), reads=["gains"], writes=["lt1"])
            P.op("dve", lambda e: e.tensor_reduce(out=ls[:, 0:2], in_=lt[:], axis=AX.X, op=ALU.add), reads=["lt0", "lt1"], writes=["ls01"])
            P.op("act", lambda e: e.activation(out=ls[:, 2:4], in_=ls[:, 0:2], func=AF.Exp), reads=["ls01"], writes=["ls23"])
            P.op("dve", lambda e: e.tensor_tensor(out=ls[:, 0:1], in0=ls[:, 3:4], in1=ls[:, 2:3], op=ALU.subtract), reads=["ls23"], writes=["ls0"])
            P.op("dve", lambda e: e.tensor_scalar(out=neg_lam[:], in0=ls[:, 0:1], scalar1=-LAM_INIT, scalar2=None, op0=ALU.add),
                 reads=["ls0"], writes=["neg_lam"])
            P.op("dve", lambda e: e.tensor_scalar(out=gains[:, 0:64], in0=gains[:, 0:64], scalar1=0.125, scalar2=None, op0=ALU.mult),
                 reads=["gains", "lt0", "lt1"], writes=["gains"])
            P.op("dve", lambda e: e.tensor_scalar(out=gains[:, 384:512], in0=gains[:, 384:512], scalar1=1.0 - LAM_INIT, scalar2=None, op0=ALU.mult),
                 reads=["gains"], writes=["gains"])
            P.op("dve", lambda e: e.tensor_scalar(out=gains[:, 512:640], in0=gains[:, 512:640], scalar1=128.0 ** -0.5, scalar2=None, op0=ALU.mult),
                 reads=["gains"], writes=["gains"])
            sdt = sb("sdt", [128, 128], F32, ph)
            P.op("dve", lambda e: e.tensor_tensor(out=sdt[:], in0=gains[:, 384:512], in1=ident_f[:], op=ALU.mult), reads=["gains", "ident_f"], writes=["sdt"])
            P.op("dve", lambda e: e.tensor_reduce(out=subln_col[:], in_=sdt[:], axis=AX.X, op=ALU.add), reads=["sdt"], writes=["subln_col"])
            if debug:
                P.dma("sp", lambda e: e.dma_start(out=dbg_d[:, 0:768], in_=gains[:]), reads=["gains"], writes=["dbg0"])
                P.dma("sp", lambda e: e.dma_start(out=dbg_d[:, 768:769], in_=neg_lam[:], allow_slow_non_contiguous=True), reads=["neg_lam"], writes=["dbg1"])
                P.dma("sp", lambda e: e.dma_start(out=dbg_d[:, 1024:1024 + 2 * KT], in_=shc[:].rearrange("p a k -> p (a k)")),
                      reads=[("shc", 0), ("shc", 1)], writes=["dbg2"])
            P.barrier()
        if upto < 1:
            P.barrier(pool_ring=True)
            return nc, P

        GT = 8
        with ExitStack() as ph:
            xT = sb("xT", [128, KT, GT * 128], BF16, ph)
            wbuf = [sb("wbuf%d" % i, [128, KT, 512], BF16, ph) for i in range(2)]
            ropet = sb("ropet", [128, GT, 2, 64], F32, ph)
            xt = [sb("xt%d" % i, [128, D], F32, ph) for i in range(2)]
            xb = [sb("xb%d" % i, [128, D], BF16, ph) for i in range(2)]
            junk = sb("junk", [128, D], BF16, ph)
            ssq = sb("ssq", [128, 2], F32, ph)
            shwb = [sb("shwb%d" % i, [128, 512], F32, ph) for i in range(2)]
            NB = 3
            pv = [sb("pv%d" % i, [128, 512], F32, ph) for i in range(NB)]
            sq = [sb("sq%d" % i, [128, 512], F32, ph) for i in range(NB)]
            qn = [sb("qn%d" % i, [128, 512], F32, ph) for i in range(NB)]
            t1 = [sb("t1%d" % i, [128, 512], F32, ph) for i in range(NB)]
            t2 = [sb("t2%d" % i, [128, 512], F32, ph) for i in range(NB)]
            s8 = [sb("s8%d" % i, [128, 8], F32, ph) for i in range(NB)]
            qo = [sb("qo%d" % i, [128, 512], BF16, ph) for i in range(NB)]
            stage = [sb("stage%d" % i, [128, 4, GT * 128], BF16, ph) for i in range(2)]
            pT = [ps("pT%d" % i, [128, 4, 128], BF16, ph) for i in range(2)]
            pM = [ps("pM%d" % i, [128, 512], F32, ph) for i in range(3)]
            pW = ps("pW", [128, 512], F32, ph)
            cnt = {"pT": 0, "pM": 0, "pp": 0, "stage": 0, "w": 0}

            blocks = []
            for cb in range(20):
                c0 = cb * 512
                if c0 < OFF_QB:
                    blocks.append(("qa", cb, c0 // 128))
                elif c0 < OFF_GATE:
                    blocks.append(("qb", cb, (c0 - OFF_QB) // 128))
                elif c0 < OFF_KA:
                    blocks.append(("gate", cb, (c0 - OFF_GATE) // 128))
                elif c0 < OFF_VA:
                    blocks.append(("ka", cb, (c0 - OFF_KA) // 128))
                elif c0 < OFF_KB:
                    blocks.append(("va", cb, (c0 - OFF_VA)))
                elif c0 < OFF_VB:
                    blocks.append(("kb", cb, (c0 - OFF_KB) // 128))
                else:
                    blocks.append(("vb", cb, (c0 - OFF_VB)))

            groups = [("lat", g) for g in range(NT // GT)] + [("ctx", 0)]
            for gkind, g in groups:
                is_ctx = gkind == "ctx"
                ntile = 2 if is_ctx else GT
                mj = 1 if is_ctx else 0
                src_d = ctx_d if is_ctx else x_d
                tok0 = 0 if is_ctx else g * GT * 128
                key0 = 0 if is_ctx else L + g * GT * 128
                if not is_ctx:
                    P.dma("sp", lambda e: e.dma_start(out=ropet[:], in_=rope_d[:, g * GT:(g + 1) * GT, :, :]), writes=["ropet"])
                for tt in range(ntile):
                    i = tt % 2
                    rcol = (NT + tt) if is_ctx else (g * GT + tt)
                    P.dma("sp", lambda e: e.dma_start(out=xt[i][:], in_=src_d[tok0 + tt * 128: tok0 + (tt + 1) * 128, :]), writes=[("xt", i)])
                    P.op("act", lambda e: e.activation(out=junk[:], in_=xt[i][:], func=AF.Square, accum_out=ssq[:, 0:1]),
                         reads=[("xt", i)], writes=["junk", "ssq0"])
                    P.op("act", lambda e: e.activation(out=ssq[:, 1:2], in_=ssq[:, 0:1], func=AF.Sqrt, scale=1.0 / D, bias=EPS),
                         reads=["ssq0"], writes=["ssq1"])
                    P.op("dve", lambda e: e.reciprocal(out=rstd_all[:, rcol:rcol + 1], in_=ssq[:, 1:2]), reads=["ssq1"], writes=[("rstd", rcol)])
                    P.op("dve", lambda e: e.tensor_tensor(out=xb[i][:], in0=xt[i][:], in1=gs1row[:, mj, :], op=ALU.mult),
                         reads=[("xt", i), "gs1row%d" % mj], writes=[("xb", i)])
                    for j4 in range(4):
                        pi = cnt["pT"] % 2
                        cnt["pT"] += 1
                        for jj in range(4):
                            k = j4 * 4 + jj
                            P.op("pe", lambda e: e.transpose(out=pT[pi][:, jj, :], in_=xb[i][:, k * 128:(k + 1) * 128], identity=ident_b[:]),
                                 reads=[("xb", i)], writes=[("pT", pi)])
                        eng = "act" if j4 % 2 == 0 else "dve"
                        if eng == "act":
                            P.op("act", lambda e: e.copy(out=xT[:, j4 * 4:(j4 + 1) * 4, tt * 128:(tt + 1) * 128], in_=pT[pi][:]),
                                 reads=[("pT", pi)], writes=[("xT", tt)])
                        else:
                            P.op("dve", lambda e: e.tensor_copy(out=xT[:, j4 * 4:(j4 + 1) * 4, tt * 128:(tt + 1) * 128], in_=pT[pi][:]),
                                 reads=[("pT", pi)], writes=[("xT", tt)])
                for kind, cb, hoff in blocks:
                    if is_ctx and kind in ("qa", "qb", "gate"):
                        continue
                    wi = cnt["w"] % 2
                    cnt["w"] += 1
                    wkeys = [("wbf_in", r_, (cb * 512) // 2048) for r_ in range(4)]
                    P.dma("sp", lambda e: e.dma_start(out=wbuf[wi][:], in_=wbf_in[:, cb * 512:(cb + 1) * 512].rearrange("(k p) c -> p k c", p=128)),
                          reads=wkeys, writes=[("wbuf", wi)])
                    for k in range(KT):
                        P.op("pe", lambda e: e.matmul(pW[:], lhsT=sh1rep[:, mj, k, :], rhs=wbuf[wi][:, k, :], start=(k == 0), stop=(k == KT - 1)),
                             reads=[("sh1rep", mj), ("wbuf", wi)], writes=["pW"])
                    P.op("act", lambda e: e.copy(out=shwb[wi][:], in_=pW[:]), reads=["pW"], writes=[("shwb", wi)])
                    need_stage = kind in ("qa", "qb", "ka", "kb", "gate")
                    if need_stage:
                        si = cnt["stage"] % 2
                        cnt["stage"] += 1
                    pending = []
                    for tt in range(ntile):
                        rcol = (NT + tt) if is_ctx else (g * GT + tt)
                        mi = cnt["pM"] % 3
                        cnt["pM"] += 1
                        for k in range(KT):
                            P.op("pe", lambda e: e.matmul(pM[mi][:], lhsT=xT[:, k, tt * 128:(tt + 1) * 128], rhs=wbuf[wi][:, k, :],
                                                          start=(k == 0), stop=(k == KT - 1)),
                                 reads=[("xT", tt), ("wbuf", wi)], writes=[("pM", mi)])
                        while len(pending) > 1:
                            pending.pop(0)()
                        bi = cnt["pp"] % NB
                        cnt["pp"] += 1
                        if kind in ("va", "vb"):
                            P.op("dve", lambda e: e.scalar_tensor_tensor(out=qo[bi][:], in0=pM[mi][:], scalar=rstd_all[:, rcol:rcol + 1],
                                                                         in1=shwb[wi][:], op0=ALU.mult, op1=ALU.add),
                                 reads=[("pM", mi), ("rstd", rcol), ("shwb", wi)], writes=[("qo", bi)])
                            dst = va_d if kind == "va" else vb_d
                            P.dma("act", lambda e: e.dma_start(out=dst[key0 + tt * 128:key0 + (tt + 1) * 128, hoff:hoff + 512], in_=qo[bi][:]),
                                  reads=[("qo", bi)], writes=[(kind, key0 + tt * 128, hoff)])
                            continue
                        P.op("dve", lambda e: e.scalar_tensor_tensor(out=pv[bi][:], in0=pM[mi][:], scalar=rstd_all[:, rcol:rcol + 1],
                                                                     in1=shwb[wi][:], op0=ALU.mult, op1=ALU.add),
                             reads=[("pM", mi), ("rstd", rcol), ("shwb", wi)], writes=[("pv", bi)])
                        if kind == "gate":
                            P.op("act", lambda e: e.activation(out=qo[bi][:], in_=pv[bi][:], func=AF.Sigmoid), reads=[("pv", bi)], writes=[("qo", bi)])
                        else:
                            npc, wdt = (8, 64) if kind in ("qa", "ka") else (4, 128)
                            goff = {"qa": 0, "ka": 64, "qb": 512, "kb": 640}[kind]
                            P.op("act", lambda e: e.activation(out=sq[bi][:], in_=pv[bi][:], func=AF.Square), reads=[("pv", bi)], writes=[("sq", bi)])
                            P.op("dve", lambda e: e.tensor_reduce(out=s8[bi][:, 0:npc], in_=sq[bi][:].rearrange("p (a b) -> p a b", b=wdt),
                                                                  axis=AX.X, op=ALU.add), reads=[("sq", bi)], writes=[("s8", bi)])
                            P.op("act", lambda e: e.activation(out=s8[bi][:, 0:npc], in_=s8[bi][:, 0:npc], func=AF.Sqrt, scale=1.0 / wdt, bias=EPS),
                                 reads=[("s8", bi)], writes=[("s8", bi)])
                            P.op("dve", lambda e: e.reciprocal(out=s8[bi][:, 0:npc], in_=s8[bi][:, 0:npc]), reads=[("s8", bi)], writes=[("s8", bi)])
                            P.op("dve", lambda e: e.tensor_tensor(out=qn[bi][:].rearrange("p (a b) -> p a b", b=wdt),
                                                                  in0=pv[bi][:].rearrange("p (a b) -> p a b", b=wdt),
                                                                  in1=s8[bi][:, 0:npc].unsqueeze(2).to_broadcast([128, npc, wdt]), op=ALU.mult),
                                 reads=[("pv", bi), ("s8", bi)], writes=[("qn", bi)])
                            rope_on = kind in ("qa", "ka") and not is_ctx
                            gdst = qn[bi] if rope_on else qo[bi]
                            P.op("dve", lambda e: e.tensor_tensor(out=gdst[:].rearrange("p (a b) -> p a b", b=wdt),
                                                                  in0=qn[bi][:].rearrange("p (a b) -> p a b", b=wdt),
                                                                  in1=gains[:, goff:goff + wdt].unsqueeze(1).to_broadcast([128, npc, wdt]), op=ALU.mult),
                                 reads=[("qn", bi), "gains"], writes=[("qn", bi) if rope_on else ("qo", bi)])
                            if rope_on:
                                q5 = qn[bi][:].rearrange("p (a r h w) -> p a r h w", a=8, r=2, h=2, w=16)
                                t5 = t2[bi][:].rearrange("p (a r h w) -> p a r h w", a=8, r=2, h=2, w=16)
                                cosb = ropet[:, tt, 0, :].unsqueeze(1).to_broadcast([128, 8, 64])
                                ss4 = ropet[:, tt, 1, :].rearrange("p (r h w) -> p r h w", r=2, h=2, w=16)
                                P.op("dve", lambda e: e.tensor_tensor(out=t1[bi][:].rearrange("p (a b) -> p a b", b=64),
                                                                      in0=qn[bi][:].rearrange("p (a b) -> p a b", b=64), in1=cosb, op=ALU.mult),
                                     reads=[("qn", bi), "ropet"], writes=[("t1", bi)])
                                for r_ in range(2):
                                    for h_ in range(2):
                                        P.op("dve", lambda e: e.tensor_tensor(out=t5[:, :, r_, h_, :], in0=q5[:, :, r_, 1 - h_, :],
                                                                              in1=ss4[:, r_, h_, :].unsqueeze(1).to_broadcast([128, 8, 16]), op=ALU.mult),
                                             reads=[("qn", bi), "ropet"], writes=[("t2", bi, r_, h_)])
                                P.op("dve", lambda e: e.tensor_tensor(out=qo[bi][:], in0=t1[bi][:], in1=t2[bi][:], op=ALU.add),
                                     reads=[("t1", bi)] + [("t2", bi, r_, h_) for r_ in range(2) for h_ in range(2)], writes=[("qo", bi)])
                        def do_tr(bi=bi, si=si, tt=tt):
                            pi = cnt["pT"] % 2
                            cnt["pT"] += 1
                            for jj in range(4):
                                P.op("pe", lambda e: e.transpose(out=pT[pi][:, jj, :], in_=qo[bi][:, jj * 128:(jj + 1) * 128], identity=ident_b[:]),
                                     reads=[("qo", bi)], writes=[("pT", pi)])
                            P.op("act", lambda e: e.copy(out=stage[si][:, :, tt * 128:(tt + 1) * 128], in_=pT[pi][:]),
                                 reads=[("pT", pi)], writes=[("stage", si)])
                        pending.append(do_tr)
                    while pending:
                        pending.pop(0)()
                    if need_stage:
                        nt_ = ntile * 128
                        if kind == "gate":
                            r0 = hoff * 128
                            P.dma("act", lambda e: e.dma_start(out=gT_d[r0:r0 + 512, tok0:tok0 + nt_].rearrange("(j p) t -> p j t", p=128),
                                                              in_=stage[si][:, :, 0:nt_]), reads=[("stage", si)], writes=[("gT", cb, g)])
                        else:
                            dst = {"qa": qaT_d, "ka": kaT_d, "qb": qbT_d, "kb": kbT_d}[kind]
                            o0 = tok0 if kind in ("qa", "qb") else key0
                            P.dma("act", lambda e: e.dma_start(out=dst[hoff:hoff + 4, :, o0:o0 + nt_].rearrange("h p t -> p h t"),
                                                              in_=stage[si][:, :, 0:nt_]), reads=[("stage", si)], writes=[(kind, cb, g, gkind)])
            P.barrier()
        ph01.close()
        if upto < 2:
            P.barrier(pool_ring=True)
            return nc, P

        NKC = NK // 128
        with ExitStack() as ph:
            qT = [sb("aqT%d" % i, [128, S], BF16, ph) for i in range(2)]
            kT = [sb("akT%d" % i, [128, NK], BF16, ph) for i in range(2)]
            vv = [sb("avv%d" % i, [128, NKC, 128], BF16, ph) for i in range(2)]
            pTb = [sb("apT%d" % i, [128, 2, 512], BF16, ph) for i in range(3)]
            accS = sb("aaccS", [128, 2, 512], F32, ph)
            rb = [sb("arb%d" % i, [128, 512], F32, ph) for i in range(2)]
            tta = sb("atta", [128, 512], F32, ph)
            yy = sb("ayy", [128, 512], F32, ph)
            ysq = sb("aysq", [128, 512], F32, ph)
            rstdb = sb("arstdb", [128, 512], F32, ph)
            yo = [sb("ayo%d" % i, [128, 512], BF16, ph) for i in range(2)]
            psS = [ps("apsS%d" % i, [128, 2, 512], F32, ph) for i in range(2)]
            psO = [ps("apsO%d" % i, [128, 512], F32, ph) for i in range(2)]
            psF = [ps("apsF%d" % i, [128, 512], F32, ph) for i in range(2)]
            nhA = lim.get("headsA", 8)

            def load_head_a(h):
                hi = h % 2
                P.dma("sp", lambda e: e.dma_start(out=qT[hi][:], in_=qaT_d[h]), writes=[("qT", hi)])
                P.dma("sp", lambda e: e.dma_start(out=kT[hi][:], in_=kaT_d[h]), writes=[("kT", hi)])
                for c0 in range(0, NKC, 22):
                    P.dma("sp", lambda e: e.dma_start(out=vv[hi][:, c0:c0 + 22, :],
                                                      in_=va_d[c0 * 128:(c0 + 22) * 128, h * 128:(h + 1) * 128].rearrange("(c p) d -> p c d", p=128)),
                          writes=[("vv", hi, c0)])

            nqbA = lim.get("qbA", 16)
            stepsA = [(h, qb, kc) for h in range(nhA) for qb in range(nqbA) for kc in range(NKC)]

            def qk_a(s):
                h, qb, kc = stepsA[s]
                hi = h % 2
                si = s % 2
                for sm in range(2):
                    P.op("pe", lambda e: e.matmul(psS[si][:, sm, :], lhsT=kT[hi][sm * 64:(sm + 1) * 64, kc * 128:(kc + 1) * 128],
                                                  rhs=qT[hi][sm * 64:(sm + 1) * 64, qb * 512:(qb + 1) * 512], start=True, stop=True),
                         reads=[("kT", hi), ("qT", hi)], writes=[("psS", si, sm)])

            load_head_a(0)
            qk_a(0)
            for s, (h, qb, kc) in enumerate(stepsA):
                    hi = h % 2
                    vkeys = [("vv", hi, c0) for c0 in range(0, NKC, 22)]
                    if qb == 1 and kc == 0 and h + 1 < nhA:
                        load_head_a(h + 1)
                    si = s % 2
                    pi = s % 3
                    if s + 1 < len(stepsA):
                        qk_a(s + 1)
                    for sm in range(2):
                        P.op("act", lambda e: e.activation(out=pTb[pi][:, sm, :], in_=psS[si][:, sm, :], func=AF.Exp),
                             reads=[("psS", si, sm)], writes=[("pTb", pi, sm)])
                    if kc == 0:
                        P.op("dve", lambda e: e.tensor_copy(out=accS[:, 0, :], in_=pTb[pi][:, 0, :]), reads=[("pTb", pi, 0)], writes=["accS"])
                    else:
                        P.op("dve", lambda e: e.tensor_tensor(out=accS[:, 0, :], in0=accS[:, 0, :], in1=pTb[pi][:, 0, :], op=ALU.add),
                             reads=[("pTb", pi, 0), "accS"], writes=["accS"])
                    if kc == 0:
                        P.op("dve", lambda e: e.tensor_copy(out=accS[:, 1, :], in_=pTb[pi][:, 1, :]), reads=[("pTb", pi, 1)], writes=["accS1"])
                    else:
                        P.op("dve", lambda e: e.tensor_tensor(out=accS[:, 1, :], in0=accS[:, 1, :], in1=pTb[pi][:, 1, :], op=ALU.add),
                             reads=[("pTb", pi, 1), "accS1"], writes=["accS1"])
                    for sm in range(2):
                        P.op("pe", lambda e: e.matmul(psO[sm][:], lhsT=vv[hi][:, kc, :], rhs=pTb[pi][:, sm, :], start=(kc == 0), stop=(kc == NKC - 1)),
                             reads=[("pTb", pi, sm)] + vkeys, writes=[("psO", sm)])
                    if kc != NKC - 1:
                        continue
                    yi = qb % 2
                    for sm in range(2):
                        P.op("pe", lambda e: e.matmul(psF[sm][:], lhsT=ones_f[:], rhs=accS[:, sm, :], start=True, stop=True),
                             reads=["accS" if sm == 0 else "accS1", "ones_f"], writes=[("psF", sm)])
                        P.op("dve", lambda e: e.reciprocal(out=rb[sm][:], in_=psF[sm][:]), reads=[("psF", sm)], writes=[("rb", sm)])
                    P.op("dve", lambda e: e.tensor_scalar(out=rb[1][:], in0=rb[1][:], scalar1=neg_lam[:, 0:1], scalar2=None, op0=ALU.mult),
                         reads=[("rb", 1), "neg_lam"], writes=[("rb", 1)])
                    P.op("dve", lambda e: e.tensor_tensor(out=tta[:], in0=psO[1][:], in1=rb[1][:], op=ALU.mult), reads=[("psO", 1), ("rb", 1)], writes=["tta"])
                    P.op("dve", lambda e: e.tensor_tensor(out=yy[:], in0=psO[0][:], in1=rb[0][:], op=ALU.mult), reads=[("psO", 0), ("rb", 0)], writes=["yy"])
                    P.op("dve", lambda e: e.tensor_tensor(out=yy[:], in0=yy[:], in1=tta[:], op=ALU.add), reads=["yy", "tta"], writes=["yy"])
                    P.op("dve", lambda e: e.tensor_tensor(out=ysq[:], in0=yy[:], in1=yy[:], op=ALU.mult), reads=["yy"], writes=["ysq"])
                    P.op("pe", lambda e: e.matmul(psF[0][:], lhsT=ones_f[:], rhs=ysq[:], start=True, stop=True), reads=["ysq", "ones_f"], writes=[("psF", 0)])
                    P.op("act", lambda e: e.activation(out=rstdb[:], in_=psF[0][:], func=AF.Ln, scale=1.0 / 128, bias=eps_col[:, 0:1]),
                         reads=[("psF", 0), "eps_col"], writes=["rstdb"])
                    P.op("act", lambda e: e.activation(out=rstdb[:], in_=rstdb[:], func=AF.Exp, scale=-0.5), reads=["rstdb"], writes=["rstdb"])
                    P.op("dve", lambda e: e.scalar_tensor_tensor(out=yo[yi][:], in0=yy[:], scalar=subln_col[:, 0:1], in1=rstdb[:], op0=ALU.mult, op1=ALU.mult),
                         reads=["yy", "rstdb", "subln_col"], writes=[("yo", yi)])
                    P.dma("sp", lambda e: e.dma_start(out=yaT_d[h][:, qb * 512:(qb + 1) * 512], in_=yo[yi][:]),
                          reads=[("yo", yi)], writes=[("yaT", h, qb)])
            P.barrier()
        if upto < 3:
            P.barrier(pool_ring=True)
            return nc, P

        with ExitStack() as ph:
            qT = [sb("bqT%d" % i, [128, S], BF16, ph) for i in range(2)]
            kT = [sb("bkT%d" % i, [128, NK], BF16, ph) for i in range(2)]
            vE = [sb("bvE%d" % i, [128, NKC, 129], BF16, ph) for i in range(2)]
            vO = [sb("bvO%d" % i, [128, NKC - 1, 129], BF16, ph) for i in range(2)]
            rpbt = sb("brpbt", [128, 8, 4, 64], F32, ph)
            nam = sb("bnam", [128, 4, 64], F32, ph)
            expB = [sb("bexpB%d" % i, [128, 8, 4, 64], BF16, ph) for i in range(2)]
            Pb = [sb("bPb%d" % i, [128, 6, 64], BF16, ph) for i in range(4)]
            rec = [sb("brec%d" % i, [128, 1], F32, ph) for i in range(2)]
            yb = [sb("byb%d" % i, [128, 128], BF16, ph) for i in range(2)]
            ybst = [sb("bybst%d" % i, [128, 2048], BF16, ph) for i in range(2)]
            psS = [ps("bpsS%d" % i, [128, 6, 64], F32, ph) for i in range(4)]
            accO = [ps("baccO%d" % i, [128, 129], F32, ph) for i in range(2)]
            pTr = ps("bpTr", [128, 128], BF16, ph)
            P.dma("sp", lambda e: e.dma_start(out=nam[:], in_=namask_d[:, :, :]), writes=["nam"])
            for i in range(2):
                P.op("dve", lambda e: e.memset(vE[i][:, :, 128:129], 1.0), writes=[("vE1", i)])
                P.op("dve", lambda e: e.memset(vO[i][:, :, 128:129], 1.0), writes=[("vO1", i)])
            step = 0
            nhB = lim.get("headsB", 8)

            def load_head_b(h):
                hi = h % 2
                P.dma("sp", lambda e: e.dma_start(out=qT[hi][:], in_=qbT_d[h]), writes=[("qT", hi)])
                P.dma("sp", lambda e: e.dma_start(out=kT[hi][:], in_=kbT_d[h]), writes=[("kT", hi)])
                for c0 in range(0, NKC, 22):
                    P.dma("sp", lambda e: e.dma_start(out=vE[hi][:, c0:c0 + 22, 0:128],
                                                      in_=vb_d[c0 * 128:(c0 + 22) * 128, h * 128:(h + 1) * 128].rearrange("(c p) d -> p c d", p=128)),
                          writes=[("vE", hi, c0)])
                for c0, n_ in ((0, 22), (22, 22), (44, 21)):
                    P.dma("sp", lambda e: e.dma_start(out=vO[hi][:, c0:c0 + n_, 0:128],
                                                      in_=vb_d[64 + c0 * 128:64 + (c0 + n_) * 128, h * 128:(h + 1) * 128].rearrange("(c p) d -> p c d", p=128)),
                          writes=[("vO", hi, c0)])

            npB = lim.get("pairsB", 64)
            stepsB = [(h, j, r2) for h in range(nhB) for j in range(npB) for r2 in range(2)]

            def rowinfo(j, r2):
                r = 2 * j + r2
                r_start = min(max(r - 4, 0), 120)
                return r, r_start, r - r_start, L + r_start * 64

            def qk_b(s):
                h, j, r2 = stepsB[s]
                hi = h % 2
                si = s % 4
                r, r_start, v, key0 = rowinfo(j, r2)
                for c in range(6):
                    ko = c * 128 if c < 2 else key0 + (c - 2) * 128
                    P.op("pe", lambda e: e.matmul(psS[si][:, c, :], lhsT=kT[hi][:, ko:ko + 128], rhs=qT[hi][:, r * 64:(r + 1) * 64],
                                                  start=True, stop=True, skip_group_check=True),
                         reads=[("kT", hi), ("qT", hi)], writes=[("psS", si)])

            def load_bias(h):
                hi = h % 2
                P.dma("sp", lambda e: e.dma_start(out=rpbt[:], in_=rpbx_d[h]), writes=["rpbt"])
                P.op("act", lambda e: e.activation(out=rpbt[:], in_=rpbt[:], func=AF.Exp), reads=["rpbt"], writes=["rpbt"])
                P.op("dve", lambda e: e.tensor_tensor(out=expB[hi][:], in0=rpbt[:], in1=nam[:].unsqueeze(1).to_broadcast([128, 8, 4, 64]), op=ALU.mult),
                     reads=["rpbt", "nam"], writes=[("expB", hi)])

            pendB = []
            load_head_b(0)
            load_bias(0)
            qk_b(0)
            for s, (h, j, r2) in enumerate(stepsB):
                hi = h % 2
                si = s % 4
                ai = j % 2
                vEk = [("vE", hi, c0) for c0 in range(0, NKC, 22)] + [("vE1", hi)]
                vOk = [("vO", hi, c0) for c0 in (0, 22, 44)] + [("vO1", hi)]
                if j == 8 and r2 == 0 and h + 1 < nhB:
                    load_head_b(h + 1)
                    load_bias(h + 1)
                r, r_start, v, key0 = rowinfo(j, r2)
                if s + 1 < len(stepsB):
                    qk_b(s + 1)
                P.op("act", lambda e: e.activation(out=Pb[si][:], in_=psS[si][:], func=AF.Exp), reads=[("psS", si)], writes=[("Pb", si)])
                P.op("dve", lambda e: e.tensor_tensor(out=Pb[si][:, 2:6, :], in0=Pb[si][:, 2:6, :], in1=expB[hi][:, v, :, :], op=ALU.mult),
                     reads=[("Pb", si), ("expB", hi)], writes=[("Pb", si)])
                for c in range(6):
                    if c < 2:
                        rhs = vE[hi][:, c, :]
                    elif r_start % 2 == 0:
                        rhs = vE[hi][:, 2 + r_start // 2 + (c - 2), :]
                    else:
                        rhs = vO[hi][:, (192 + r_start * 64) // 128 + (c - 2), :]
                    P.op("pe", lambda e: e.matmul(accO[ai][r2 * 64:(r2 + 1) * 64, :], lhsT=Pb[si][:, c, :], rhs=rhs, start=(c == 0), stop=(c == 5),
                                                  skip_group_check=True),
                         reads=[("Pb", si)] + vEk + vOk, writes=[("accO", ai)])
                while pendB:
                    pendB.pop(0)()
                if r2 == 0:
                    continue
                P.op("dve", lambda e: e.reciprocal(out=rec[ai][:], in_=accO[ai][:, 128:129]), reads=[("accO", ai)], writes=[("rec", ai)])
                P.op("dve", lambda e: e.tensor_scalar(out=yb[ai][:], in0=accO[ai][:, 0:128], scalar1=rec[ai][:, 0:1], scalar2=None, op0=ALU.mult),
                     reads=[("accO", ai), ("rec", ai)], writes=[("yb", ai)])

                def fin_b(ai=ai, j=j, h=h):
                    P.op("pe", lambda e: e.transpose(out=pTr[:], in_=yb[ai][:], identity=ident_b[:]), reads=[("yb", ai)], writes=["pTr"])
                    yi = (j // 16) % 2
                    P.op("act", lambda e: e.copy(out=ybst[yi][:, (j % 16) * 128:(j % 16 + 1) * 128], in_=pTr[:]), reads=["pTr"], writes=[("ybst", yi)])
                    if j % 16 == 15:
                        P.dma("sp", lambda e: e.dma_start(out=ybT_d[h][:, (j // 16) * 2048:(j // 16 + 1) * 2048], in_=ybst[yi][:]),
                              reads=[("ybst", yi)], writes=[("ybT", h, j // 16)])
                pendB.append(fin_b)
            while pendB:
                pendB.pop(0)()
            P.barrier()
        if upto < 4:
            P.barrier(pool_ring=True)
            return nc, P

        with ExitStack() as ph:
            wa = sb("cwa", [128, 8, D], BF16, ph)
            wb = sb("cwb", [128, 8, D], BF16, ph)
            P.dma("sp", lambda e: e.dma_start(out=wa[:], in_=wbf_a.rearrange("(k p) c -> p k c", p=128)),
                  reads=[("wbf_a", r_, 0) for r_ in range(2)], writes=["wa"])
            P.dma("sp", lambda e: e.dma_start(out=wb[:], in_=wbf_b.rearrange("(k p) c -> p k c", p=128)),
                  reads=[("wbf_b", r_, 0) for r_ in range(2)], writes=["wb"])
            yaTg = [sb("cya%d" % i, [128, 8, 512], BF16, ph) for i in range(2)]
            ybTg = [sb("cyb%d" % i, [128, 8, 512], BF16, ph) for i in range(2)]
            gt = [sb("cgt%d" % i, [128, 2, 512], BF16, ph) for i in range(6)]
            t1 = [sb("ct1%d" % i, [128, 512], F32, ph) for i in range(3)]
            t2 = [sb("ct2%d" % i, [128, 512], F32, ph) for i in range(3)]
            mst = [sb("cmst%d" % i, [128, 512], BF16, ph) for i in range(3)]
            psA = [ps("cpsA%d" % i, [128, 512], F32, ph) for i in range(3)]
            psB = [ps("cpsB%d" % i, [128, 512], F32, ph) for i in range(3)]
            n3 = 0
            for tg in range(lim.get("tg3a", S // 512)):
                gi = tg % 2
                tsl = slice(tg * 512, (tg + 1) * 512)
                P.dma("sp", lambda e: e.dma_start(out=yaTg[gi][:], in_=yaT_d[:, :, tsl].rearrange("h p t -> p h t")), writes=[("yaTg", gi)])
                P.dma("sp", lambda e: e.dma_start(out=ybTg[gi][:], in_=ybT_d[:, :, tsl].rearrange("h p t -> p h t")), writes=[("ybTg", gi)])
                for dc in range(KT):
                    pi = n3 % 3
                    g4 = n3 % 6
                    n3 += 1
                    P.dma("sp", lambda e: e.dma_start(out=gt[g4][:, 0, :], in_=gT_d[dc * 128:(dc + 1) * 128, tsl]), writes=[("gt", g4, 0)])
                    P.dma("sp", lambda e: e.dma_start(out=gt[g4][:, 1, :], in_=gT_d[D + dc * 128:D + (dc + 1) * 128, tsl]), writes=[("gt", g4, 1)])
                    for k in range(8):
                        P.op("pe", lambda e: e.matmul(psA[pi][:], lhsT=wa[:, k, dc * 128:(dc + 1) * 128], rhs=yaTg[gi][:, k, :], start=(k == 0), stop=(k == 7)),
                             reads=["wa", ("yaTg", gi)], writes=[("psA", pi)])
                    for k in range(8):
                        P.op("pe", lambda e: e.matmul(psB[pi][:], lhsT=wb[:, k, dc * 128:(dc + 1) * 128], rhs=ybTg[gi][:, k, :], start=(k == 0), stop=(k == 7)),
                             reads=["wb", ("ybTg", gi)], writes=[("psB", pi)])
                    P.op("dve", lambda e: e.tensor_tensor(out=t1[pi][:], in0=psA[pi][:], in1=gt[g4][:, 0, :], op=ALU.mult),
                         reads=[("psA", pi), ("gt", g4, 0)], writes=[("t1", pi)])
                    P.op("dve", lambda e: e.tensor_tensor(out=t2[pi][:], in0=psB[pi][:], in1=gt[g4][:, 1, :], op=ALU.mult),
                         reads=[("psB", pi), ("gt", g4, 1)], writes=[("t2", pi)])
                    P.op("dve", lambda e: e.tensor_tensor(out=mst[pi][:], in0=t1[pi][:], in1=t2[pi][:], op=ALU.add),
                         reads=[("t1", pi), ("t2", pi)], writes=[("mst", pi)])
                    P.dma("act", lambda e: e.dma_start(out=mT_d[dc][:, tsl], in_=mst[pi][:]), reads=[("mst", pi)], writes=[("mT", dc, tg)])
            P.barrier()

        with ExitStack() as ph:
            wo = sb("dwo", [128, KT, D], BF16, ph)
            P.dma("sp", lambda e: e.dma_start(out=wo[:], in_=wbf_o.rearrange("(k p) c -> p k c", p=128)),
                  reads=[("wbf_o", r_, 0) for r_ in range(4)], writes=["wo"])
            wr = sb("dwr", [128, KT, NE], F32, ph)
            P.dma("sp", lambda e: e.dma_start(out=wr[:], in_=wr_d.rearrange("(k p) e -> p k e", p=128)), writes=["wr"])
            rows = sb("drows", [128, 3, D], F32, ph)
            for j in range(3):
                P.dma("sp", lambda e: e.dma_start(out=rows[:, j, :], in_=modsave_d[j]), writes=[("rows", j)])
            mTt = [sb("dmT%d" % i, [128, KT, 128], BF16, ph) for i in range(2)]
            xt = [sb("dxt%d" % i, [128, D], F32, ph) for i in range(2)]
            xn = [sb("dxn%d" % i, [128, D], F32, ph) for i in range(2)]
            h2 = sb("dh2", [128, D], F32, ph)
            h2b = [sb("dh2b%d" % i, [128, D], BF16, ph) for i in range(2)]
            h2T = sb("dh2T", [128, KT, 128], F32, ph)
            junk = sb("djunk", [128, D], BF16, ph)
            sm = [sb("dsm%d" % i, [128, 8], F32, ph) for i in range(2)]
            ex = [sb("dex%d" % i, [128, NE], F32, ph) for i in range(2)]
            pmix = [ps("dpmix%d" % i, [128, 512], F32, ph) for i in range(3)]
            pTf = [ps("dpTf%d" % i, [128, 4, 128], F32, ph) for i in range(2)]
            pR = ps("dpR", [128, NE], F32, ph)
            n3 = 0
            ntf = 0
            for t in range(lim.get("t3b", NT)):
                i = t % 2
                rsl = slice(t * 128, (t + 1) * 128)
                P.dma("sp", lambda e: e.dma_start(out=mTt[i][:], in_=mT_d[:, :, rsl].rearrange("k p t -> p k t")), writes=[("mTt", i)])
                P.dma("sp", lambda e: e.dma_start(out=xt[i][:], in_=x_d[rsl, :]), writes=[("xt", i)])
                for cb in range(4):
                    pi = n3 % 3
                    n3 += 1
                    csl = slice(cb * 512, (cb + 1) * 512)
                    for k in range(KT):
                        P.op("pe", lambda e: e.matmul(pmix[pi][:], lhsT=mTt[i][:, k, :], rhs=wo[:, k, csl], start=(k == 0), stop=(k == KT - 1)),
                             reads=[("mTt", i), "wo"], writes=[("pmix", pi)])
                    P.op("dve", lambda e: e.tensor_tensor(out=xn[i][:, csl], in0=pmix[pi][:], in1=rows[:, 0, csl], op=ALU.mult),
                         reads=[("pmix", pi), ("rows", 0)], writes=[("xn", i, cb)])
                    P.op("dve", lambda e: e.tensor_tensor(out=xn[i][:, csl], in0=xn[i][:, csl], in1=xt[i][:, csl], op=ALU.add),
                         reads=[("xn", i, cb), ("xt", i)], writes=[("xn", i, cb)])
                xnk = [("xn", i, cb) for cb in range(4)]
                P.dma("act", lambda e: e.dma_start(out=out_d[rsl, :], in_=xn[i][:]), reads=xnk, writes=[("out", t)])
                P.op("act", lambda e: e.activation(out=junk[:], in_=xn[i][:], func=AF.Square, accum_out=sm[i][:, 0:1]), reads=xnk, writes=["junk", ("sm", i, 0)])
                P.op("act", lambda e: e.activation(out=sm[i][:, 1:2], in_=sm[i][:, 0:1], func=AF.Sqrt, scale=1.0 / D, bias=EPS),
                     reads=[("sm", i, 0)], writes=[("sm", i, 1)])
                P.op("dve", lambda e: e.reciprocal(out=sm[i][:, 1:2], in_=sm[i][:, 1:2]), reads=[("sm", i, 1)], writes=[("sm", i, 1)])
                P.op("dve", lambda e: e.scalar_tensor_tensor(out=h2[:], in0=xn[i][:], scalar=sm[i][:, 1:2], in1=rows[:, 2, :], op0=ALU.mult, op1=ALU.mult),
                     reads=xnk + [("sm", i, 1), ("rows", 2)], writes=["h2"])
                P.op("dve", lambda e: e.tensor_tensor(out=h2[:], in0=h2[:], in1=rows[:, 1, :], op=ALU.add), reads=["h2", ("rows", 1)], writes=["h2"])
                P.op("act", lambda e: e.copy(out=h2b[i][:], in_=h2[:]), reads=["h2"], writes=[("h2b", i)])
                P.dma("act", lambda e: e.dma_start(out=h2_d[rsl, :], in_=h2b[i][:]), reads=[("h2b", i)], writes=[("h2d", t)])
                for j4 in range(4):
                    ti = ntf % 2
                    ntf += 1
                    for jj in range(4):
                        k = j4 * 4 + jj
                        P.op("pe", lambda e: e.transpose(out=pTf[ti][:, jj, :], in_=h2[:, k * 128:(k + 1) * 128], identity=ident_f[:]),
                             reads=["h2"], writes=[("pTf", ti)])
                    if j4 % 2 == 0:
                        P.op("act", lambda e: e.copy(out=h2T[:, j4 * 4:(j4 + 1) * 4, :], in_=pTf[ti][:]), reads=[("pTf", ti)], writes=[("h2T", j4)])
                    else:
                        P.op("dve", lambda e: e.tensor_copy(out=h2T[:, j4 * 4:(j4 + 1) * 4, :], in_=pTf[ti][:]), reads=[("pTf", ti)], writes=[("h2T", j4)])
                for k in range(KT):
                    P.op("pe", lambda e: e.matmul(pR[:], lhsT=h2T[:, k, :], rhs=wr[:, k, :], start=(k == 0), stop=(k == KT - 1)),
                         reads=[("h2T", k // 4), "wr"], writes=["pR"])
                P.op("dve", lambda e: e.tensor_reduce(out=sm[i][:, 2:3], in_=pR[:], axis=AX.X, op=ALU.max), reads=["pR"], writes=[("sm", i, 2)])
                P.op("dve", lambda e: e.tensor_scalar(out=sm[i][:, 3:4], in0=sm[i][:, 2:3], scalar1=-1.0, scalar2=None, op0=ALU.mult),
                     reads=[("sm", i, 2)], writes=[("sm", i, 3)])
                P.op("act", lambda e: e.activation(out=ex[i][:], in_=pR[:], func=AF.Exp, bias=sm[i][:, 3:4], accum_out=sm[i][:, 4:5]),
                     reads=["pR", ("sm", i, 3)], writes=[("ex", i), ("sm", i, 4)])
                P.op("dve", lambda e: e.reciprocal(out=sm[i][:, 5:6], in_=sm[i][:, 4:5]), reads=[("sm", i, 4)], writes=[("sm", i, 5)])
                P.op("dve", lambda e: e.tensor_scalar(out=aff_all[:, t, :], in0=ex[i][:], scalar1=sm[i][:, 5:6], scalar2=None, op0=ALU.mult),
                     reads=[("ex", i), ("sm", i, 5)], writes=[("aff", t)])
            if debug:
                P.dma("sp", lambda e: e.dma_start(out=dbg_d[:, 2048:2048 + NT * NE], in_=aff_all[:].rearrange("p j e -> p (j e)")),
                      reads=[("aff", t_) for t_ in range(lim.get("t3b", NT))], writes=["dbg3"])
            P.barrier()
        if upto < 5:
            P.barrier(pool_ring=True)
            return nc, P

        with ExitStack() as ph:
            meta = sb("emeta", [128, NE, 8, 4], F32, ph)
            idx32 = sb("eidx32", [128, NE, 8], I32, ph)
            idx4 = sb("eidx4", [128, NE, 8, 4], I32, ph)
            with ExitStack() as ph4:
                lo = sb("elo", [128, NE], F32, ph4)
                mid = sb("emid", [128, NE], F32, ph4)
                cmpb = sb("ecmp", [128, NT, NE], BF16, ph4)
                cntp = sb("ecntp", [128, NE], F32, ph4)

                gsel = sb("egsel", [128, NE], F32, ph4)
                pC = ps("epC", [128, NE], F32, ph4)
                P.op("dve", lambda e: e.memset(lo[:], 0.0), writes=["lo"])
                for it in range(30):
                    ci = 2.0 ** -(it + 1)
                    P.op("dve", lambda e: e.tensor_scalar(out=mid[:], in0=lo[:], scalar1=ci, scalar2=None, op0=ALU.add), reads=["lo"], writes=["mid"])
                    P.op("dve", lambda e: e.tensor_tensor(out=cmpb[:], in0=aff_all[:], in1=mid[:].unsqueeze(1).to_broadcast([128, NT, NE]), op=ALU.is_ge),
                         reads=["mid"], writes=["cmpb"])
                    P.op("dve", lambda e: e.tensor_reduce(out=cntp[:], in_=cmpb[:].rearrange("p j e -> p e j"), axis=AX.X, op=ALU.add),
                         reads=["cmpb"], writes=["cntp"])
                    P.op("pe", lambda e: e.matmul(pC[:], lhsT=ones_f[:], rhs=cntp[:], start=True, stop=True), reads=["cntp", "ones_f"], writes=["pC"])
                    P.op("dve", lambda e: e.tensor_scalar(out=gsel[:], in0=pC[:], scalar1=CAP - 0.5, scalar2=ci, op0=ALU.is_ge, op1=ALU.mult),
                         reads=["pC"], writes=["gsel"])
                    P.op("dve", lambda e: e.tensor_tensor(out=lo[:], in0=lo[:], in1=gsel[:], op=ALU.add), reads=["lo", "gsel"], writes=["lo"])
                msk = sb("emsk", [128, NT, NE], F32, ph4)
                mskb = sb("emskb", [128, NT, NE], BF16, ph4)
                U = sb("eU", [128, 128], BF16, ph4)
                tot = sb("etot", [128, NE, NT], F32, ph4)
                incl = sb("eincl", [128, NE, NT], F32, ph4)
                ones64 = sb("eones64", [128, NT], F32, ph4)
                pos = sb("epos", [128, NT, NE], F32, ph4)
                posm = sb("eposm", [128, NT, NE], F32, ph4)
                vals = sb("evals", [128, NT, NE, 5], BF16, ph4)
                rres = sb("erres", [128, NT, NE], F32, ph4)
                iota_j = sb("eiotaj", [128, NT], F32, ph4)
                iota_s = sb("eiotas", [128, CAP], F32, ph4)
                oh = [sb("eoh%d" % i, [128, CAP], BF16, ph4) for i in range(3)]
                pP = [ps("epP%d" % i, [128, 512], F32, ph4) for i in range(2)]
                pTt = [ps("epTt%d" % i, [128, 512], F32, ph4) for i in range(2)]
                pM = [ps("epM%d" % i, [128, 8, 8, 8], F32, ph4) for i in range(2)]
                P.op("pool", lambda e: e.iota(iota_j[:], pattern=[[1, NT]], base=0, channel_multiplier=0, allow_small_or_imprecise_dtypes=True),
                     writes=["iota_j"])
                P.op("pool", lambda e: e.iota(iota_s[:], pattern=[[1, CAP]], base=0, channel_multiplier=0, allow_small_or_imprecise_dtypes=True),
                     writes=["iota_s"])
                P.op("dve", lambda e: e.tensor_tensor(out=msk[:], in0=aff_all[:], in1=lo[:].unsqueeze(1).to_broadcast([128, NT, NE]), op=ALU.is_ge),
                     reads=["lo"], writes=["msk"])
                P.op("dve", lambda e: e.tensor_copy(out=mskb[:], in_=msk[:]), reads=["msk"], writes=["mskb"])
                P.op("dve", lambda e: e.tensor_scalar(out=U[:], in0=iota_row[:], scalar1=iota_p[:, 0:1], scalar2=None, op0=ALU.is_gt), writes=["U"])
                P.op("dve", lambda e: e.memset(ones64[:], 1.0), writes=["ones64"])
                mflat = mskb[:].rearrange("p j e -> p (j e)")
                for hf in range(2):
                    P.op("pe", lambda e: e.matmul(pP[hf][:], lhsT=U[:], rhs=mflat[:, hf * 512:(hf + 1) * 512], start=True, stop=True),
                         reads=["U", "mskb"], writes=[("pP", hf)])
                    P.op("pe", lambda e: e.matmul(pTt[hf][:], lhsT=ones_b[:], rhs=mflat[:, hf * 512:(hf + 1) * 512], start=True, stop=True),
                         reads=["mskb"], writes=[("pTt", hf)])
                    P.op("dve", lambda e: e.tensor_copy(out=tot[:, :, hf * 32:(hf + 1) * 32], in_=pTt[hf][:].rearrange("p (j e) -> p e j", e=NE)),
                         reads=[("pTt", hf)], writes=[("tot", hf)])
                for e_ in range(NE):
                    P.op("dve", lambda e: e.tensor_tensor_scan(out=incl[:, e_, :], data0=ones64[:], data1=tot[:, e_, :], initial=0.0, op0=ALU.mult, op1=ALU.add),
                         reads=[("tot", 0), ("tot", 1), "ones64"], writes=[("incl", e_)])
                inck = [("incl", e_) for e_ in range(NE)]
                P.op("dve", lambda e: e.tensor_tensor(out=incl[:], in0=incl[:], in1=tot[:], op=ALU.subtract), reads=inck + [("tot", 0), ("tot", 1)], writes=inck)
                for hf in range(2):
                    P.op("dve", lambda e: e.tensor_tensor(out=pos[:, hf * 32:(hf + 1) * 32, :], in0=pP[hf][:].rearrange("p (j e) -> p j e", e=NE),
                                                          in1=incl[:, :, hf * 32:(hf + 1) * 32].rearrange("p e j -> p j e"), op=ALU.add),
                         reads=[("pP", hf)] + inck, writes=[("pos", hf)])
                P.op("dve", lambda e: e.scalar_tensor_tensor(out=posm[:], in0=pos[:], scalar=1.0, in1=msk[:], op0=ALU.add, op1=ALU.mult),
                     reads=[("pos", 0), ("pos", 1), "msk"], writes=["posm"])
                P.op("dve", lambda e: e.tensor_scalar(out=posm[:], in0=posm[:], scalar1=-1.0, scalar2=None, op0=ALU.add), reads=["posm"], writes=["posm"])
                P.op("dve", lambda e: e.tensor_copy(out=vals[:, :, :, 0], in_=aff_all[:]), writes=["v0"])
                P.op("dve", lambda e: e.tensor_tensor(out=rres[:], in0=aff_all[:], in1=vals[:, :, :, 0], op=ALU.subtract), reads=["v0"], writes=["rres"])
                P.op("dve", lambda e: e.tensor_copy(out=vals[:, :, :, 1], in_=rres[:]), reads=["rres"], writes=["v1"])
                P.op("dve", lambda e: e.tensor_tensor(out=rres[:], in0=rres[:], in1=vals[:, :, :, 1], op=ALU.subtract), reads=["rres", "v1"], writes=["rres"])
                P.op("dve", lambda e: e.tensor_copy(out=vals[:, :, :, 2], in_=rres[:]), reads=["rres"], writes=["v2"])
                P.op("dve", lambda e: e.tensor_copy(out=vals[:, :, :, 3], in_=iota_p[:, 0:1].unsqueeze(2).to_broadcast([128, NT, NE])), writes=["v3"])
                P.op("dve", lambda e: e.tensor_copy(out=vals[:, :, :, 4], in_=iota_j[:].unsqueeze(2).to_broadcast([128, NT, NE])),
                     reads=["iota_j"], writes=["v4"])
                if debug:
                    P.dma("sp", lambda e: e.dma_start(out=dbg_d[:, 3072:3072 + NT * NE], in_=posm[:].rearrange("p j e -> p (j e)")),
                          reads=["posm"], writes=["dbg4"])
                    P.dma("sp", lambda e: e.dma_start(out=dbg_d[:, 1100:1100 + NE], in_=lo[:]), reads=["lo"], writes=["dbg5"])
                noh = 0
                for e_ in range(NE):
                    for j in range(NT):
                        oi = noh % 3
                        noh += 1
                        P.op("dve", lambda e: e.tensor_scalar(out=oh[oi][:], in0=iota_s[:], scalar1=posm[:, j, e_:e_ + 1], scalar2=None, op0=ALU.is_equal),
                             reads=["iota_s", "posm"], writes=[("oh", oi)])
                        for st in range(8):
                            P.op("pe", lambda e: e.matmul(pM[e_ // 8][:, e_ % 8, st, 0:5], lhsT=oh[oi][:, st * 128:(st + 1) * 128], rhs=vals[:, j, e_, :],
                                                          start=(e_ % 8 == 0 and j == 0 and st == 0), stop=(j == NT - 1), skip_group_check=True),
                                 reads=[("oh", oi), "v0", "v1", "v2", "v3", "v4"], writes=[("pM", e_ // 8)])
                for hf in range(2):
                    esl = slice(hf * 8, hf * 8 + 8)
                    P.op("dve", lambda e: e.tensor_tensor(out=meta[:, esl, :, 0], in0=pM[hf][:, :, :, 0], in1=pM[hf][:, :, :, 1], op=ALU.add) if False else
                         e.tensor_copy(out=meta[:, esl, :, 0:3], in_=pM[hf][:, :, :, 2:5]), reads=[("pM", hf)], writes=[("metaA", hf)])
                    P.op("dve", lambda e: e.tensor_tensor(out=meta[:, esl, :, 0], in0=meta[:, esl, :, 0], in1=pM[hf][:, :, :, 1], op=ALU.add),
                         reads=[("pM", hf), ("metaA", hf)], writes=[("metaA", hf)])
                    P.op("dve", lambda e: e.tensor_tensor(out=meta[:, esl, :, 0], in0=meta[:, esl, :, 0], in1=pM[hf][:, :, :, 0], op=ALU.add),
                         reads=[("pM", hf), ("metaA", hf)], writes=[("metaA", hf)])
                P.op("dve", lambda e: e.tensor_copy(out=meta[:, :, :, 3:4], in_=meta[:, :, :, 3:4]), reads=[("metaA", 0), ("metaA", 1)], writes=["meta"])
                tokf = sb("etokf", [128, NE, 8], F32, ph4)
                tok4 = sb("etok4", [128, NE, 8, 4], F32, ph4)
                P.op("dve", lambda e: e.scalar_tensor_tensor(out=tokf[:], in0=meta[:, :, :, 2], scalar=128.0, in1=meta[:, :, :, 1], op0=ALU.mult, op1=ALU.add),
                     reads=["meta"], writes=["tokf"])
                P.op("dve", lambda e: e.tensor_copy(out=idx32[:], in_=tokf[:]), reads=["tokf"], writes=["idx32"])
                for db in range(4):
                    P.op("dve", lambda e: e.tensor_scalar(out=tok4[:, :, :, db], in0=tokf[:], scalar1=4.0, scalar2=float(db), op0=ALU.mult, op1=ALU.add),
                         reads=["tokf"], writes=[("tok4", db)])
                P.op("dve", lambda e: e.tensor_copy(out=idx4[:], in_=tok4[:]), reads=[("tok4", db) for db in range(4)], writes=["idx4"])
                if debug:
                    P.dma("sp", lambda e: e.dma_start(out=dbg_d[:, 1200:1200 + NE * 8 * 4], in_=meta[:].rearrange("p a b c -> p (a b c)")),
                          reads=["meta"], writes=["dbg6"])
                P.barrier()
            with ExitStack() as ph5:
                ga2row = sb("fga2", [128, D], F32, ph5)
                P.dma("sp", lambda e: e.dma_start(out=ga2row[:], in_=modsave_d[3]), writes=["ga2row"])
                xeT = sb("fxeT", [128, KT, CAP], BF16, ph5)
                actT = sb("factT", [128, KT, CAP], BF16, ph5)
                wring = [sb("fw%d" % i, [128, KT, 512], BF16, ph5) for i in range(4)]
                xg = [sb("fxg%d" % i, [128, D], BF16, ph5) for i in range(2)]
                sa = [sb("fsa%d" % i, [128, 512], F32, ph5) for i in range(2)]
                ysc = [sb("fysc%d" % i, [128, 512], F32, ph5) for i in range(4)]
                psA = [ps("fpsA%d" % i, [128, 512], F32, ph5) for i in range(2)]
                psU = [ps("fpsU%d" % i, [128, 512], F32, ph5) for i in range(2)]
                psY = [ps("fpsY%d" % i, [128, 512], F32, ph5) for i in range(2)]
                pTx = [ps("fpTx%d" % i, [128, 4, 128], BF16, ph5) for i in range(2)]
                out4 = out_d.rearrange("t (q c) -> (t q) c", c=512)
                c5 = {"w": 0, "xg": 0, "tx": 0, "au": 0, "y": 0, "ysc": 0}
                nexp = lim.get("experts", NE)

                def load_w(srcw, keyname, e_, blk):
                    wi = c5["w"] % 4
                    c5["w"] += 1
                    P.dma("sp", lambda e: e.dma_start(out=wring[wi][:], in_=srcw[e_][:, blk * 512:(blk + 1) * 512].rearrange("(k p) c -> p k c", p=128)),
                          reads=[((keyname, e_), r_, 0) for r_ in range(4)], writes=[("wring", wi)])
                    return wi

                def gather(e_):
                    for st in range(8):
                        gi = c5["xg"] % 2
                        c5["xg"] += 1
                        P.dma("pool", lambda e: e.indirect_dma_start(out=xg[gi][:], out_offset=None, in_=h2_d[:, :],
                                                                     in_offset=bass.IndirectOffsetOnAxis(ap=idx32[:, e_, st:st + 1], axis=0)),
                              reads=["idx32"], writes=[("xg", gi)])
                        for j4 in range(4):
                            ti = c5["tx"] % 2
                            c5["tx"] += 1
                            for jj in range(4):
                                k = j4 * 4 + jj
                                P.op("pe", lambda e: e.transpose(out=pTx[ti][:, jj, :], in_=xg[gi][:, k * 128:(k + 1) * 128], identity=ident_b[:]),
                                     reads=[("xg", gi)], writes=[("pTx", ti)])
                            if j4 % 2 == 0:
                                P.op("act", lambda e: e.copy(out=xeT[:, j4 * 4:(j4 + 1) * 4, st * 128:(st + 1) * 128], in_=pTx[ti][:]),
                                     reads=[("pTx", ti)], writes=[("xeT", st)])
                            else:
                                P.op("dve", lambda e: e.tensor_copy(out=xeT[:, j4 * 4:(j4 + 1) * 4, st * 128:(st + 1) * 128], in_=pTx[ti][:]),
                                     reads=[("pTx", ti)], writes=[("xeT", st)])

                prev_sc = []
                gather(0)
                for e_ in range(nexp):
                    for fb in range(4):
                        wg = load_w(wbf_eg, "wbf_eg", e_, fb)
                        wu = load_w(wbf_eu, "wbf_eu", e_, fb)
                        for fc in range(4):
                            for sh in range(2):
                                ai = c5["au"] % 2
                                c5["au"] += 1
                                xk = [("xeT", s_) for s_ in range(sh * 4, sh * 4 + 4)]
                                for k in range(KT):
                                    P.op("pe", lambda e: e.matmul(psA[ai][:], lhsT=wring[wg][:, k, fc * 128:(fc + 1) * 128], rhs=xeT[:, k, sh * 512:(sh + 1) * 512],
                                                                  start=(k == 0), stop=(k == KT - 1)), reads=[("wring", wg)] + xk, writes=[("psA", ai)])
                                for k in range(KT):
                                    P.op("pe", lambda e: e.matmul(psU[ai][:], lhsT=wring[wu][:, k, fc * 128:(fc + 1) * 128], rhs=xeT[:, k, sh * 512:(sh + 1) * 512],
                                                                  start=(k == 0), stop=(k == KT - 1)), reads=[("wring", wu)] + xk, writes=[("psU", ai)])
                                P.op("act", lambda e: e.activation(out=sa[ai][:], in_=psA[ai][:], func=AF.Silu), reads=[("psA", ai)], writes=[("sa", ai)])
                                P.op("dve", lambda e: e.tensor_tensor(out=actT[:, fb * 4 + fc, sh * 512:(sh + 1) * 512], in0=psU[ai][:], in1=sa[ai][:], op=ALU.mult),
                                     reads=[("psU", ai), ("sa", ai)], writes=[("actT", fb * 4 + fc, sh)])
                    if e_ + 1 < nexp:
                        gather(e_ + 1)
                    cur_sc = []
                    ak = [("actT", f_, s_) for f_ in range(KT) for s_ in range(2)]
                    for db in range(4):
                        wd = load_w(wbf_ed, "wbf_ed", e_, db)
                        for st in range(8):
                            yi = c5["y"] % 2
                            c5["y"] += 1
                            for k in range(KT):
                                P.op("pe", lambda e: e.matmul(psY[yi][:], lhsT=actT[:, k, st * 128:(st + 1) * 128], rhs=wring[wd][:, k, :],
                                                              start=(k == 0), stop=(k == KT - 1)),
                                     reads=[("wring", wd)] + [("actT", f_, st // 4) for f_ in range(KT)], writes=[("psY", yi)])
                            si = c5["ysc"] % 4
                            c5["ysc"] += 1
                            P.op("dve", lambda e: e.scalar_tensor_tensor(out=ysc[si][:], in0=psY[yi][:], scalar=meta[:, e_, st, 0:1],
                                                                         in1=ga2row[:, db * 512:(db + 1) * 512], op0=ALU.mult, op1=ALU.mult),
                                 reads=[("psY", yi), "meta", "ga2row"], writes=[("ysc", si)])
                            for ev in prev_sc:
                                P.wait("pool", ev)
                            ev = P.dma("pool", lambda e: e.indirect_dma_start(out=out4[:, :],
                                                                             out_offset=bass.IndirectOffsetOnAxis(ap=idx4[:, e_, st, db:db + 1], axis=0),
                                                                             in_=ysc[si][:], in_offset=None, compute_op=ALU.add),
                                       reads=[("ysc", si), "idx4"], writes=[])
                            cur_sc.append(ev)
                    prev_sc = cur_sc
                P.barrier(pool_ring=True)
        P.barrier(pool_ring=True)
        return nc, P


_CONST_CACHE = {}


def _prep_inputs(inputs):
    if "c" not in _CONST_CACHE:
        _CONST_CACHE["c"] = _consts()
    rope, namask = _CONST_CACHE["c"]
    f = lambda a: np.ascontiguousarray(np.asarray(a, dtype=np.float32))
    x = f(inputs["x"]); ctx = f(inputs["ctx"]); c = f(inputs["c"]); c_ctx = f(inputs["c_ctx"])
    smallp = np.concatenate([f(inputs[k])[0] for k in ("q_gain_a", "k_gain_a", "lam_q1", "lam_k1", "lam_q2", "lam_k2",
                                                       "subln_gain", "q_gain_b", "k_gain_b")]).reshape(1, 768)
    shared = {
        "w_mod": f(inputs["w_mod"])[0], "b_mod": f(inputs["b_mod"])[0].reshape(1, -1),
        "g12": np.stack([f(inputs["g_norm1"])[0], f(inputs["g_norm2"])[0]]),
        "w_in": f(inputs["w_in"])[0], "smallp": smallp,
        "rpbx": _rpb_expand(f(inputs["rel_pos_bias"])[0]), "namask": namask, "rope": rope,
        "w_a": f(inputs["w_branch_a"])[0], "w_b": f(inputs["w_branch_b"])[0], "w_o": f(inputs["w_out"])[0],
        "w_r": f(inputs["w_router"])[0], "w_eg": f(inputs["w_exp_gate"])[0], "w_eu": f(inputs["w_exp_up"])[0],
        "w_ed": f(inputs["w_exp_down"])[0],
    }
    maps = []
    for core in range(N_CORES):
        b = core % 4
        cc = np.stack([c[b].reshape(KT, 128).T, c_ctx.reshape(KT, 128).T], axis=-1)
        m = dict(shared)
        m.update({"x": x[b], "ctx": ctx[b], "cc": np.ascontiguousarray(cc)})
        maps.append(m)
    return maps


def kernel(**inputs):
    maps = _prep_inputs(inputs)
    nc, _ = build()
    res = run_bass_kernel_spmd(nc, maps, core_ids=list(range(N_CORES)))
    out = np.stack([res.results[b]["out"] for b in range(4)], axis=0)
    return out.astype(np.float32)
```
